# Optimizing a Trainium2 kernel written in Bass

```python
import functools
import numpy as np
import jax
import jax.numpy as jnp
from jax import lax

D_MODEL = 2048
BATCH = 4
SEQ = 8192
DEPTH = 2

GRID_W = 64
CTX_LEN = 256
N_AB = (DEPTH + 1) // 2
N_CD = DEPTH // 2
MIX_W = D_MODEL
A_WIDTH = MIX_W // 2
A_HEAD = 64
A_HEADS = A_WIDTH // A_HEAD
A_LORA_W = max(32, int(round(A_WIDTH ** 0.5 * 1.8 / 32)) * 32)
A_LORA_A = A_LORA_W
A_LORA_G = max(32, int(round(A_WIDTH ** 0.8 * 0.6 / 32)) * 32)
A_SPLITS = (A_WIDTH, A_WIDTH, A_WIDTH, A_LORA_A, A_LORA_G, A_LORA_W, A_LORA_W)
A_COLS = sum(A_SPLITS)
B_HEADS = 8
B_DK = 64
B_WIDTH = MIX_W - A_WIDTH
B_DV = B_WIDTH // B_HEADS
B_SPLITS = (B_HEADS * B_DK, B_HEADS * B_DK, B_WIDTH, B_WIDTH)
AB_IN = A_COLS + sum(B_SPLITS)
C_WIDTH = MIX_W // 4
S5_H = 16
S5_GROUPS = C_WIDTH // S5_H
S5_P = 64
D_WIDTH = MIX_W - C_WIDTH
D_HEAD = 128
D_HEADS = D_WIDTH // D_HEAD
D_SPLITS = (D_WIDTH,) * 5
CD_IN = C_WIDTH + sum(D_SPLITS)
N_GROUPS = 4
EXP_PER_GROUP = 8
N_EXPERTS = N_GROUPS * EXP_PER_GROUP
TOP_K = 2
D_EXPERT = D_MODEL // 4
MOE_BLOCK = 128
RET_CHUNK = 128
HGRN_CHUNK = 64
ROPE_BASE = 10000.0
EPS = 1e-6
RWKV_GN_EPS = 64e-5

kernel_name = 'hybrid_rwkv7_retnet_s5_hgrn2_hmoe_dit'


def split_cols(z, sizes):
    return jnp.split(z, np.cumsum(sizes)[:-1].tolist(), axis=-1)


def to_heads(t, n_heads):
    b, l, w = t.shape
    return t.astype(jnp.float32).reshape(b, l, n_heads, w // n_heads).transpose(0, 2, 1, 3)


def rms_norm(x, w):
    xf = x.astype(jnp.float32)
    y = xf * lax.rsqrt(jnp.mean(xf * xf, axis=-1, keepdims=True) + EPS)
    return y * w.astype(jnp.float32)


def mod_norm(x, w, shift, scale):
    return rms_norm(x, w) * (1.0 + scale) + shift


def grid_shift(z, rows):
    b, t, ch = z.shape
    g = z.reshape(b, rows, GRID_W, ch // 4, 4)
    left = jnp.pad(g[:, :, :-1, :, 0], ((0, 0), (0, 0), (1, 0), (0, 0)))
    right = jnp.pad(g[:, :, 1:, :, 1], ((0, 0), (0, 0), (0, 1), (0, 0)))
    up = jnp.pad(g[:, :-1, :, :, 2], ((0, 0), (1, 0), (0, 0), (0, 0)))
    down = jnp.pad(g[:, 1:, :, :, 3], ((0, 0), (0, 1), (0, 0), (0, 0)))
    return jnp.stack([left, right, up, down], axis=-1).reshape(b, t, ch)


def seq_shift(z):
    b, l, ch = z.shape
    g = z.reshape(b, l, ch // 2, 2)
    prev = jnp.pad(g[:, :-1, :, 0], ((0, 0), (1, 0), (0, 0)))
    nxt = jnp.pad(g[:, 1:, :, 1], ((0, 0), (0, 1), (0, 0)))
    return jnp.stack([prev, nxt], axis=-1).reshape(b, l, ch)


def axial_rope(x):
    t_len, dk = x.shape[2], x.shape[3]
    half = dk // 2
    nf = half // 2
    t = jnp.arange(t_len)
    row = (t // GRID_W).astype(jnp.float32)
    col = (t % GRID_W).astype(jnp.float32)
    inv = ROPE_BASE ** (-jnp.arange(nf, dtype=jnp.float32) / nf)
    ang = jnp.concatenate([row[:, None] * inv, col[:, None] * inv], axis=-1)
    cos, sin = jnp.cos(ang), jnp.sin(ang)
    x1, x2 = x[..., :half], x[..., half:]
    return jnp.concatenate([x1 * cos - x2 * sin, x1 * sin + x2 * cos], axis=-1)


def two_way(run_f, run_b, ctx_f, ctx_b, lat_f, lat_b, s0, axis):
    rev = lambda args: tuple(jnp.flip(a, axis) for a in args)
    yc_f, sc_f = run_f(s0, *ctx_f)
    yc_b, sc_b = run_b(s0, *rev(ctx_b))
    yl_f, _ = run_f(sc_f, *lat_f)
    yl_b, _ = run_b(sc_b, *rev(lat_b))
    return yc_f + jnp.flip(yc_b, axis), yl_f + jnp.flip(yl_b, axis)


def rwkv7_scan(s0, r, w, k, v, kk, a):
    def step(s, inp):
        r_t, w_t, k_t, v_t, kk_t, a_t = inp
        sa = jnp.einsum('bhvk,bhk->bhv', s, -kk_t)
        s = s * w_t[:, :, None, :] + sa[..., None] * (kk_t * a_t)[:, :, None, :] + v_t[..., None] * k_t[:, :, None, :]
        return s, jnp.einsum('bhvk,bhk->bhv', s, r_t)
    xs = tuple(jnp.moveaxis(t, 1, 0) for t in (r, w, k, v, kk, a))
    s_fin, y = lax.scan(step, s0, xs)
    return jnp.moveaxis(y, 0, 1), s_fin


def chunk_recurrence(s0, q, k, v, log_f, chunk):
    bsz, nh, l, dk = q.shape
    dv = v.shape[-1]
    n = l // chunk
    q = q.reshape(bsz, nh, n, chunk, dk)
    k = k.reshape(bsz, nh, n, chunk, dk)
    v = v.reshape(bsz, nh, n, chunk, dv)
    lf = jnp.broadcast_to(log_f.astype(jnp.float32), (bsz, nh, l, dk)).reshape(bsz, nh, n, chunk, dk)
    b = jnp.cumsum(lf, axis=3)
    b_mid = b[:, :, :, chunk // 2:chunk // 2 + 1]
    b_last = b[:, :, :, -1:]
    tril = jnp.tril(jnp.ones((chunk, chunk), dtype=bool))
    scores = jnp.einsum('bhnid,bhnjd->bhnij', q * jnp.exp(b - b_mid), k * jnp.exp(b_mid - b))
    o = jnp.einsum('bhnij,bhnje->bhnie', jnp.where(tril, scores, 0.0), v)
    kv = jnp.einsum('bhnjd,bhnje->bhnde', k * jnp.exp(b_last - b), v)
    dec = jnp.exp(b_last[:, :, :, 0])

    def step(s, inp):
        d_c, kv_c = inp
        return d_c[..., None] * s + kv_c, s

    s_fin, s_prev = lax.scan(step, s0, (jnp.moveaxis(dec, 2, 0), jnp.moveaxis(kv, 2, 0)))
    o = o + jnp.einsum('bhnid,nbhde->bhnie', q * jnp.exp(b), s_prev)
    return o.reshape(bsz, nh, l, dv), s_fin


def s5_discretize(lam_re, lam_im, log_dt, b_re, b_im):
    dt = jnp.exp(log_dt.astype(jnp.float32))[:, None]
    lam_re = lam_re.astype(jnp.float32)
    lam_im = lam_im.astype(jnp.float32)
    mag = jnp.exp(lam_re * dt)
    lb_re, lb_im = mag * jnp.cos(lam_im * dt), mag * jnp.sin(lam_im * dt)
    nr, ni = lb_re - 1.0, lb_im
    den = lam_re * lam_re + lam_im * lam_im
    co_re = (nr * lam_re + ni * lam_im) / den
    co_im = (ni * lam_re - nr * lam_im) / den
    b_re = b_re.astype(jnp.float32)
    b_im = b_im.astype(jnp.float32)
    bb_re = co_re[..., None] * b_re - co_im[..., None] * b_im
    bb_im = co_re[..., None] * b_im + co_im[..., None] * b_re
    return lb_re, lb_im, bb_re, bb_im


def s5_scan(s0, u, disc):
    lb_re, lb_im, bb_re, bb_im = disc
    bu_re = jnp.einsum('gph,blgh->blgp', bb_re, u)
    bu_im = jnp.einsum('gph,blgh->blgp', bb_im, u)
    x0_re, x0_im = s0
    bu_re = bu_re.at[:, 0].add(lb_re * x0_re - lb_im * x0_im)
    bu_im = bu_im.at[:, 0].add(lb_re * x0_im + lb_im * x0_re)
    l = u.shape[1]
    a_re = jnp.broadcast_to(lb_re[None, None], (1, l) + lb_re.shape)
    a_im = jnp.broadcast_to(lb_im[None, None], (1, l) + lb_im.shape)

    def combine(e1, e2):
        a1r, a1i, b1r, b1i = e1
        a2r, a2i, b2r, b2i = e2
        return (a2r * a1r - a2i * a1i, a2r * a1i + a2i * a1r,
                a2r * b1r - a2i * b1i + b2r, a2r * b1i + a2i * b1r + b2i)

    _, _, xr, xi = lax.associative_scan(combine, (a_re, a_im, bu_re, bu_im), axis=1)
    return (xr, xi), (xr[:, -1], xi[:, -1])


def rwkv7_mixer(z, zc, rows, mu, w0, w2, a0, a2, g2, k_k, k_a, r_k, ln_w, ln_b):
    def prep(zz, zs):
        zz = zz + (zs - zz) * mu
        r, k, v, a_lo, g_lo, wf_lo, wb_lo = split_cols(zz, A_SPLITS)

        def decay(lo, d):
            w = -jax.nn.softplus(-(w0[d] + jnp.tanh(lo) @ w2[d]).astype(jnp.float32)) - 0.5
            return jnp.exp(-jnp.exp(w))

        wf, wb = decay(wf_lo, 0), decay(wb_lo, 1)
        a = jax.nn.sigmoid((a0 + a_lo @ a2).astype(jnp.float32))
        g = jax.nn.sigmoid(g_lo) @ g2
        hd = lambda t: t.astype(jnp.float32).reshape(t.shape[:-1] + (A_HEADS, A_HEAD))
        r, k, v, a, wf, wb = (hd(t) for t in (r, k, v, a, wf, wb))
        kk = k * k_k.reshape(A_HEADS, A_HEAD)
        kk = kk * lax.rsqrt(jnp.sum(kk * kk, axis=-1, keepdims=True) + 1e-12)
        k = k * (1.0 + (a - 1.0) * k_a.reshape(A_HEADS, A_HEAD))
        return r, k, v, kk, a, wf, wb, g

    lat = prep(z, grid_shift(z, rows))
    ctx = prep(zc, seq_shift(zc))
    args = lambda p, w: (p[0], w, p[1], p[2], p[3], p[4])
    s0 = jnp.zeros((z.shape[0], A_HEADS, A_HEAD, A_HEAD), jnp.float32)
    yc, yl = two_way(rwkv7_scan, rwkv7_scan, args(ctx, ctx[5]), args(ctx, ctx[6]),
                     args(lat, lat[5]), args(lat, lat[6]), s0, 1)

    def finish(y, p):
        r, k, v, g = p[0], p[1], p[2], p[7]
        bsz, l = y.shape[:2]
        mean = jnp.mean(y, axis=-1, keepdims=True)
        var = jnp.mean(jnp.square(y - mean), axis=-1, keepdims=True)
        yn = ((y - mean) * lax.rsqrt(var + RWKV_GN_EPS)).reshape(bsz, l, A_WIDTH) * ln_w + ln_b
        bonus = (jnp.sum(r * k * r_k, axis=-1, keepdims=True) * v).reshape(bsz, l, A_WIDTH)
        return (yn + bonus) * g

    return finish(yl, lat), finish(yc, ctx)


def retention_mixer(z, zc, ret_exp):
    log_gamma = jnp.log1p(-jnp.exp2(ret_exp.astype(jnp.float32)))
    lg_f = log_gamma[0][None, :, None, None]
    lg_b = log_gamma[1][None, :, None, None]

    def prep(zz, use_rope):
        q, k, v, g = split_cols(zz, B_SPLITS)
        q, k, v = to_heads(q, B_HEADS), to_heads(k, B_HEADS), to_heads(v, B_HEADS)
        k = k * (B_DK ** -0.5)
        if use_rope:
            q, k = axial_rope(q), axial_rope(k)
        return q, k, v, g

    ql, kl, vl, gl = prep(z, True)
    qc, kc, vc, gc = prep(zc, False)
    run = functools.partial(chunk_recurrence, chunk=RET_CHUNK)
    s0 = jnp.zeros((z.shape[0], B_HEADS, B_DK, B_DV), jnp.float32)
    yc, yl = two_way(run, run, (qc, kc, vc, lg_f), (qc, kc, vc, lg_b),
                     (ql, kl, vl, lg_f), (ql, kl, vl, lg_b), s0, 2)

    def finish(y, g):
        mean = jnp.mean(y, axis=-1, keepdims=True)
        var = jnp.mean(jnp.square(y - mean), axis=-1, keepdims=True)
        y = (y - mean) * lax.rsqrt(var + EPS)
        bsz, nh, l, dv = y.shape
        return y.transpose(0, 2, 1, 3).reshape(bsz, l, nh * dv) * jax.nn.silu(g)

    return finish(yl, gl), finish(yc, gc)


def s5_mixer(u, uc, lam_re, lam_im, log_dt, b_re, b_im, c_re, c_im, d, glu_w):
    groups = lambda t: t.astype(jnp.float32).reshape(t.shape[0], t.shape[1], S5_GROUPS, S5_H)
    ug, ucg = groups(u), groups(uc)
    c_re = c_re.astype(jnp.float32)
    c_im = c_im.astype(jnp.float32)

    def make_run(dirn):
        disc = s5_discretize(lam_re[dirn], lam_im[dirn], log_dt[dirn], b_re, b_im)

        def run(s0, u_):
            (xr, xi), s_fin = s5_scan(s0, u_, disc)
            y = jnp.einsum('ghp,blgp->blgh', c_re, xr) - jnp.einsum('ghp,blgp->blgh', c_im, xi)
            return y, s_fin
        return run

    zero = jnp.zeros((u.shape[0], S5_GROUPS, S5_P), jnp.float32)
    yc, yl = two_way(make_run(0), make_run(1), (ucg,), (ucg,), (ug,), (ug,), (zero, zero), 1)

    def finish(y, u_):
        y = y.reshape(u_.shape) + d * u_
        a, gate = jnp.split(jax.nn.gelu(y) @ glu_w, 2, axis=-1)
        return a * jax.nn.sigmoid(gate)

    return finish(yl, u), finish(yc, uc)


def hgrn2_mixer(z, zc, lb, norm_w):
    lbh = lb.astype(jnp.float32).reshape(D_HEADS, 1, D_HEAD)

    def prep(zz):
        q, ff, fb, i, g = split_cols(zz, D_SPLITS)
        q, ff, fb, i = (to_heads(t, D_HEADS) for t in (q, ff, fb, i))
        q = jax.nn.silu(q)
        f_f = lbh + (1.0 - lbh) * jax.nn.sigmoid(ff)
        f_b = lbh + (1.0 - lbh) * jax.nn.sigmoid(fb)
        return q, 1.0 - f_f, jnp.log(f_f), 1.0 - f_b, jnp.log(f_b), i, g

    lat, ctx = prep(z), prep(zc)
    args = lambda p, dirn: (p[0], p[1 + 2 * dirn], p[5], p[2 + 2 * dirn])
    run = functools.partial(chunk_recurrence, chunk=HGRN_CHUNK)
    s0 = jnp.zeros((z.shape[0], D_HEADS, D_HEAD, D_HEAD), jnp.float32)
    yc, yl = two_way(run, run, args(ctx, 0), args(ctx, 1), args(lat, 0), args(lat, 1), s0, 2)

    def finish(y, g):
        y = y * lax.rsqrt(jnp.mean(y * y, axis=-1, keepdims=True) + EPS)
        bsz, nh, l, dv = y.shape
        return y.transpose(0, 2, 1, 3).reshape(bsz, l, nh * dv) * norm_w * jax.nn.silu(g)

    return finish(yl, lat[6]), finish(yc, ctx[6])


def hier_moe(h, wr_c, br_c, wr_f, br_f, w_gu, w_down):
    n_tok, d = h.shape
    hf = h.astype(jnp.float32)
    p_group = jax.nn.softmax(hf @ wr_c + br_c, axis=-1)
    g_val, g_idx = lax.top_k(p_group, 1)
    fine = (hf @ wr_f + br_f).reshape(n_tok, N_GROUPS, EXP_PER_GROUP)
    fine = fine[jnp.arange(n_tok), g_idx[:, 0]]
    e_val, e_idx = lax.top_k(jax.nn.softmax(fine, axis=-1), TOP_K)
    weight = (g_val * e_val / jnp.sum(e_val, axis=-1, keepdims=True)).reshape(-1)
    expert = (g_idx * EXP_PER_GROUP + e_idx).reshape(-1)
    tok = jnp.repeat(jnp.arange(n_tok, dtype=jnp.int32), TOP_K)
    m = n_tok * TOP_K
    order = jnp.argsort(expert)
    e_s, tok_s, w_s = expert[order], tok[order], weight[order]
    counts = jnp.bincount(expert, length=N_EXPERTS)
    padded = (counts + MOE_BLOCK - 1) // MOE_BLOCK * MOE_BLOCK
    pad_end = jnp.cumsum(padded)
    dest = (pad_end - padded)[e_s] + jnp.arange(m) - (jnp.cumsum(counts) - counts)[e_s]
    n_blocks = -(-m // MOE_BLOCK) + N_EXPERTS
    buf_tok = jnp.zeros(n_blocks * MOE_BLOCK, jnp.int32).at[dest].set(tok_s)
    buf_w = jnp.zeros(n_blocks * MOE_BLOCK, jnp.float32).at[dest].set(w_s)
    blk_e = jnp.minimum(jnp.searchsorted(pad_end, jnp.arange(n_blocks) * MOE_BLOCK, side='right'), N_EXPERTS - 1)

    def expert_block(args):
        toks, e = args
        gate, up = jnp.split(hf[toks] @ w_gu[e], 2, axis=-1)
        return (jax.nn.silu(gate) * up) @ w_down[e]

    out = lax.map(expert_block, (buf_tok.reshape(n_blocks, MOE_BLOCK), blk_e))
    out = out.reshape(-1, d) * buf_w[:, None]
    return jnp.zeros((n_tok, d), out.dtype).at[buf_tok].add(out)


def setup_inputs(seed: int = 0) -> dict:
    key = jax.random.key(seed)
    ks = jax.random.split(key, 48)
    nrm = lambda i, shape, std: std * jax.random.normal(ks[i], shape, jnp.float32)
    uni = lambda i, shape, lo, hi: jax.random.uniform(ks[i], shape, jnp.float32, lo, hi)
    d = D_MODEL
    ret_base = -(5.0 + jnp.arange(B_HEADS, dtype=jnp.float32))
    n_idx = jnp.arange(S5_P, dtype=jnp.float32)
    return {
        'x': nrm(0, (BATCH, SEQ, d), 1.0),
        'c': nrm(1, (BATCH, d), 1.0),
        'ctx': nrm(2, (BATCH, CTX_LEN, d), 1.0),
        'c_ctx': nrm(3, (d,), 1.0),
        'mod_w': nrm(4, (DEPTH, d, 6 * d), 0.5 * d ** -0.5),
        'mod_b': nrm(5, (DEPTH, 6 * d), 0.02),
        'norm_w': 1.0 + nrm(6, (DEPTH, 2, d), 0.02),
        'final_norm_w': 1.0 + nrm(7, (d,), 0.02),
        'ab_w_in': nrm(8, (N_AB, d, AB_IN), d ** -0.5),
        'rwkv_mu': uni(9, (N_AB, A_COLS), 0.0, 1.0),
        'rwkv_w0': uni(10, (N_AB, 2, A_WIDTH), -6.0, 1.0),
        'rwkv_w2': nrm(11, (N_AB, 2, A_LORA_W, A_WIDTH), 0.1 * A_LORA_W ** -0.5),
        'rwkv_a0': nrm(12, (N_AB, A_WIDTH), 0.1),
        'rwkv_a2': nrm(13, (N_AB, A_LORA_A, A_WIDTH), A_LORA_A ** -0.5),
        'rwkv_g2': nrm(14, (N_AB, A_LORA_G, A_WIDTH), A_LORA_G ** -0.5),
        'rwkv_k_k': 0.85 + nrm(15, (N_AB, A_WIDTH), 0.02),
        'rwkv_k_a': 1.0 + nrm(16, (N_AB, A_WIDTH), 0.02),
        'rwkv_r_k': nrm(17, (N_AB, A_HEADS, A_HEAD), 0.1),
        'rwkv_ln_w': 1.0 + nrm(18, (N_AB, A_WIDTH), 0.02),
        'rwkv_ln_b': nrm(19, (N_AB, A_WIDTH), 0.02),
        'ret_decay_exp': ret_base + nrm(20, (N_AB, 2, B_HEADS), 0.1),
        'cd_w_in': nrm(21, (N_CD, d, CD_IN), d ** -0.5),
        's5_lambda_re': -0.5 + nrm(22, (N_CD, 2, S5_GROUPS, S5_P), 0.01),
        's5_lambda_im': jnp.pi * n_idx + nrm(23, (N_CD, 2, S5_GROUPS, S5_P), 0.01),
        's5_log_dt': uni(24, (N_CD, 2, S5_GROUPS), float(np.log(1e-3)), float(np.log(1e-1))),
        's5_b_re': nrm(25, (N_CD, S5_GROUPS, S5_P, S5_H), (2 * S5_H) ** -0.5),
        's5_b_im': nrm(26, (N_CD, S5_GROUPS, S5_P, S5_H), (2 * S5_H) ** -0.5),
        's5_c_re': nrm(27, (N_CD, S5_GROUPS, S5_H, S5_P), S5_P ** -0.5),
        's5_c_im': nrm(28, (N_CD, S5_GROUPS, S5_H, S5_P), S5_P ** -0.5),
        's5_d': nrm(29, (N_CD, C_WIDTH), 1.0),
        's5_glu_w': nrm(30, (N_CD, C_WIDTH, 2 * C_WIDTH), C_WIDTH ** -0.5),
        'hgrn_lb_logits': nrm(31, (DEPTH, D_WIDTH), 0.1),
        'hgrn_norm_w': 1.0 + nrm(32, (N_CD, D_WIDTH), 0.02),
        'w_out': nrm(33, (DEPTH, MIX_W, d), MIX_W ** -0.5),
        'moe_wr_coarse': nrm(34, (DEPTH, d, N_GROUPS), d ** -0.5),
        'moe_br_coarse': nrm(35, (DEPTH, N_GROUPS), 0.01),
        'moe_wr_fine': nrm(36, (DEPTH, d, N_EXPERTS), d ** -0.5),
        'moe_br_fine': nrm(37, (DEPTH, N_EXPERTS), 0.01),
        'moe_w_gu': nrm(38, (DEPTH, N_EXPERTS, d, 2 * D_EXPERT), d ** -0.5),
        'moe_w_down': nrm(39, (DEPTH, N_EXPERTS, D_EXPERT, d), D_EXPERT ** -0.5),
    }


def reference(x, c, ctx, c_ctx, mod_w, mod_b, norm_w, final_norm_w,
              ab_w_in, rwkv_mu, rwkv_w0, rwkv_w2, rwkv_a0, rwkv_a2, rwkv_g2, rwkv_k_k, rwkv_k_a,
              rwkv_r_k, rwkv_ln_w, rwkv_ln_b, ret_decay_exp,
              cd_w_in, s5_lambda_re, s5_lambda_im, s5_log_dt, s5_b_re, s5_b_im, s5_c_re, s5_c_im,
              s5_d, s5_glu_w, hgrn_lb_logits, hgrn_norm_w,
              w_out, moe_wr_coarse, moe_br_coarse, moe_wr_fine, moe_br_fine, moe_w_gu, moe_w_down):
    bsz, t_len, d = x.shape
    l_ctx = ctx.shape[1]
    rows = t_len // GRID_W
    xc = ctx
    p_lb = jax.nn.softmax(hgrn_lb_logits.astype(jnp.float32), axis=0)
    lb_all = jnp.cumsum(p_lb, axis=0) - p_lb[0]
    for layer in range(DEPTH):
        last = layer == DEPTH - 1
        idx = layer // 2
        m = [t[:, None, :] for t in jnp.split(jax.nn.silu(c) @ mod_w[layer] + mod_b[layer], 6, axis=-1)]
        mc = jnp.split(jax.nn.silu(c_ctx) @ mod_w[layer] + mod_b[layer], 6, axis=-1)
        h = mod_norm(x, norm_w[layer, 0], m[0], m[1])
        hc = mod_norm(xc, norm_w[layer, 0], mc[0], mc[1])
        if layer % 2 == 0:
            p, pc = h @ ab_w_in[idx], hc @ ab_w_in[idx]
            ya, yac = rwkv7_mixer(p[..., :A_COLS], pc[..., :A_COLS], rows, rwkv_mu[idx], rwkv_w0[idx],
                                  rwkv_w2[idx], rwkv_a0[idx], rwkv_a2[idx], rwkv_g2[idx], rwkv_k_k[idx],
                                  rwkv_k_a[idx], rwkv_r_k[idx], rwkv_ln_w[idx], rwkv_ln_b[idx])
            yb, ybc = retention_mixer(p[..., A_COLS:], pc[..., A_COLS:], ret_decay_exp[idx])
        else:
            p, pc = h @ cd_w_in[idx], hc @ cd_w_in[idx]
            ya, yac = s5_mixer(p[..., :C_WIDTH], pc[..., :C_WIDTH], s5_lambda_re[idx], s5_lambda_im[idx],
                               s5_log_dt[idx], s5_b_re[idx], s5_b_im[idx], s5_c_re[idx], s5_c_im[idx],
                               s5_d[idx], s5_glu_w[idx])
            yb, ybc = hgrn2_mixer(p[..., C_WIDTH:], pc[..., C_WIDTH:], lb_all[layer], hgrn_norm_w[idx])
        y = jnp.concatenate([ya, yb], axis=-1)
        x = x + m[2] * (y @ w_out[layer])
        h2 = mod_norm(x, norm_w[layer, 1], m[3], m[4])
        moe_args = (moe_wr_coarse[layer], moe_br_coarse[layer], moe_wr_fine[layer], moe_br_fine[layer],
                    moe_w_gu[layer], moe_w_down[layer])
        if last:
            x = x + m[5] * hier_moe(h2.reshape(bsz * t_len, d), *moe_args).reshape(bsz, t_len, d)
        else:
            yc = jnp.concatenate([yac, ybc], axis=-1)
            xc = xc + mc[2] * (yc @ w_out[layer])
            h2c = mod_norm(xc, norm_w[layer, 1], mc[3], mc[4])
            out = hier_moe(jnp.concatenate([h2.reshape(bsz * t_len, d), h2c.reshape(bsz * l_ctx, d)], axis=0), *moe_args)
            x = x + m[5] * out[:bsz * t_len].reshape(bsz, t_len, d)
            xc = xc + mc[5] * out[bsz * t_len:].reshape(bsz, l_ctx, d)
    return rms_norm(x, final_norm_w)
```

```python
import ml_dtypes
import contextlib
import numpy as np
import concourse.bass as bass
import concourse.mybir as mybir
from concourse.bass_utils import run_bass_kernel_spmd

F32 = mybir.dt.float32
BF16 = mybir.dt.bfloat16
U32 = mybir.dt.uint32
AF = mybir.ActivationFunctionType
ALU = mybir.AluOpType
AX = mybir.AxisListType

EPOCH = 20000
NDMA_SLOTS = 6


class Prog:
    ENGS = ("pe", "act", "dve", "pool", "sp")

    def __init__(self, nc):
        self.nc = nc
        self.ops = []
        self.last_w = {}
        self.readers = {}
        self.stack = contextlib.ExitStack()
        self.arena = None
        self.arena_words = 0
        self.bump = 0
        self.base = 0
        self.psbanks = None
        self.psnext = 0

    def use_arena(self, words):
        self.arena = self.stack.enter_context(self.nc.sbuf_tensor("arena", [128, words], F32))
        self.arena_words = words
        self.psbanks = [self.stack.enter_context(self.nc.psum_tensor(f"bank{i}", [128, 512], F32)) for i in range(8)]

    def sb(self, name, shape, dt=F32):
        if self.arena is None:
            return self.stack.enter_context(self.nc.sbuf_tensor(name, list(shape), dt))
        assert dt == F32 and shape[0] <= 128
        n = int(np.prod(shape[1:]))
        assert self.bump + n <= self.arena_words, (name, self.bump, n)
        v = self.arena[0:shape[0], self.bump:self.bump + n]
        self.bump += n
        if len(shape) == 3:
            v = v.rearrange("p (a b) -> p a b", b=shape[2])
        elif len(shape) == 4:
            v = v.rearrange("p (a b c) -> p a b c", b=shape[2], c=shape[3])
        return v

    def ps(self, name, shape, dt=F32):
        if self.psbanks is None:
            return self.stack.enter_context(self.nc.psum_tensor(name, list(shape), dt))
        assert self.psnext < 8, name
        t = self.psbanks[self.psnext]
        self.psnext += 1
        return t

    def persist(self):
        self.base = self.bump

    def phase(self):
        last = {}
        recent_dma = {}
        for i, op in enumerate(self.ops):
            if op["dma"]:
                recent_dma.setdefault(op["eng"], []).append(i)
            elif op["fn"] is not None:
                last[op["eng"]] = i
        deps = set(last.values())
        for q, lst in recent_dma.items():
            deps.update(lst[-NDMA_SLOTS:])
        for e in self.ENGS:
            self.ops.append(dict(eng=e, fn=None, deps=set(deps), dma=False, marked=False))
        self.bump = self.base
        self.psnext = 0

    def dram(self, name, shape, dt=F32, kind="Internal"):
        return self.nc.dram_tensor(name, list(shape), dt, kind=kind).ap()

    def add(self, eng, fn, r=(), w=(), dma=False):
        deps = set()
        for x in r:
            if x in self.last_w:
                deps.add(self.last_w[x])
        for x in w:
            if x in self.last_w:
                deps.add(self.last_w[x])
            deps.update(self.readers.get(x, ()))
        idx = len(self.ops)
        self.ops.append(dict(eng=eng, fn=fn, deps=deps, dma=dma, marked=False))
        for x in r:
            self.readers.setdefault(x, []).append(idx)
        for x in w:
            self.last_w[x] = idx
            self.readers[x] = []
        return idx

    def dma(self, out, in_, r=(), w=(), q="sp"):
        return self.add(q, lambda e: e.dma_start(out=out, in_=in_), r, w, dma=True)

    def emit(self):
        nc = self.nc
        ops = self.ops
        for op in ops:
            for d in op["deps"]:
                ops[d]["marked"] = True
        cnt = {e: 0 for e in self.ENGS}
        dcnt = {e: 0 for e in self.ENGS}
        sem_names = set()
        for op in ops:
            e = op["eng"]
            if op["dma"]:
                slot = dcnt[e] % NDMA_SLOTS
                use = dcnt[e] // NDMA_SLOTS + 1
                dcnt[e] += 1
                nm = f"d_{e}_{slot}"
                op["sem"] = (nm, 16 * use)
                op["prev"] = (nm, 16 * (use - 1)) if use > 1 else None
                sem_names.add(nm)
            elif op["marked"]:
                cnt[e] += 1
                ep = (cnt[e] - 1) // EPOCH
                nm = f"c_{e}_{ep}"
                op["sem"] = (nm, cnt[e] - ep * EPOCH)
                sem_names.add(nm)
        sems = {nm: self.stack.enter_context(nc.semaphore(nm)) for nm in sorted(sem_names)}
        final_dma = {}
        for op in ops:
            if op["dma"]:
                nm, v = op["sem"]
                final_dma[nm] = max(final_dma.get(nm, 0), v)
        block = self.stack.enter_context(nc.Block())

        def run(eng_name, e):
            waited = {}

            def wait(nm, v):
                if waited.get(nm, 0) < v:
                    e.wait_ge(sems[nm], v)
                    waited[nm] = v

            for op in ops:
                if op["eng"] != eng_name:
                    continue
                for d in sorted(op["deps"]):
                    nm, v = ops[d]["sem"]
                    wait(nm, v)
                if op["dma"]:
                    if op["prev"] is not None:
                        wait(*op["prev"])
                    inst = op["fn"](e)
                    inst.then_inc(sems[op["sem"][0]], 16)
                elif op["fn"] is not None:
                    inst = op["fn"](e)
                    if op["marked"]:
                        inst.then_inc(sems[op["sem"][0]], 1)
            if eng_name == "sp":
                for nm, v in sorted(final_dma.items()):
                    wait(nm, v)

        @block.tensor
        def _(e):
            run("pe", e)

        @block.scalar
        def _(e):
            run("act", e)

        @block.vector
        def _(e):
            run("dve", e)

        @block.gpsimd
        def _(e):
            run("pool", e)

        @block.sync
        def _(e):
            run("sp", e)

        self.stack.close()
        return nc


D = 2048
KT = 16
NE = 32
DE = 512
EPS = 1e-6


def mm_stream_fm(P, W, K, N, rhs_fn, rhs_res, n, evac, tag, stage, ps_list, NC=256, psw="psA"):
    kt_n = K // 128
    Wv = W.rearrange("(k p) n -> p k n", p=128)
    for ci, n0 in enumerate(range(0, N, NC)):
        st, sres = stage[ci % 2]
        P.dma(st[:, 0:kt_n, 0:NC], Wv[:, :, n0:n0 + NC], w=[sres])
        for j in range(NC // 128):
            ct = (n0 // 128) + j
            pi = ct % len(ps_list)
            pst = ps_list[pi]
            pres = f"{psw}{pi}"

            def mm(e, st=st, j=j, pst=pst):
                last = None
                for k in range(kt_n):
                    last = e.matmul(pst[:, 0:n], lhsT=st[:, k, j * 128:(j + 1) * 128], rhs=rhs_fn(k),
                                    start=(k == 0), stop=(k == kt_n - 1))
                return last

            P.add("pe", mm, r=[sres] + list(rhs_res), w=[pres])
            evac(ct, pst, pres)


def build_bcd(tiles, NT, last, TT=384):
    nc = bass.Bass("TRN2", target_bir_lowering=False)
    P = Prog(nc)
    ei = lambda name, shape: nc.dram_tensor(name, list(shape), F32, kind="ExternalInput").ap()
    xT = ei("xT", [D, NT]); yT = nc.dram_tensor("yT", [D, NT], BF16, kind="ExternalInput").ap(); wout = ei("wout", [D, D])
    vecs = ei("vecs", [128, 12, KT])
    wr = ei("wr", [D, 36]); br = ei("br", [128, 36])
    wgu = ei("wgu", [NE, D, 2 * DE]); wdn = ei("wdn", [NE, DE, D])
    ident_d = ei("ident", [128, 128]); onehot_d = ei("onehot", [128, 32 * 128])
    out = nc.dram_tensor("out", [D, NT], F32, kind="ExternalOutput").ap()


    xt = P.sb("xt", [128, KT, TT]); h2 = P.sb("h2", [128, KT, TT]); acc = P.sb("acc", [128, KT, TT])
    sg = P.sb("sg", [128, 4, TT]); act = P.sb("act", [128, 4, TT]); gb = P.sb("gb", [128, TT])
    sgt = P.sb("sgt", [128, TT])
    wg = [(P.sb(f"wg{i}", [128, KT, 256]), f"wg{i}") for i in range(2)]
    wd = [(P.sb(f"wd{i}", [128, 4, 1024]), f"wd{i}") for i in range(2)]
    vec = P.sb("vec", [128, 12, KT]); weff = P.sb("weff", [128, 2, KT])
    wrs = P.sb("wrs", [128, KT, 36]); brs = P.sb("brs", [128, 36])
    ident = P.sb("ident_s", [128, 128]); onehot = P.sb("onehot_s", [128, 32 * 128])
    ones = P.sb("ones", [128, 128]); epsb = P.sb("epsb", [128, 1])
    rstd = P.sb("rstd", [128, TT])
    lg = P.sb("lg", [128, 36]); sm = P.sb("sm", [128, 64]); gd = P.sb("gd", [128, 128]); gT = P.sb("gT", [128, TT])
    psA = [P.ps(f"psA{i}", [128, 512]) for i in range(3)]
    psB = [P.ps(f"psB{i}", [128, 512]) for i in range(3)]
    psS = P.ps("psS", [128, 512]); psR = P.ps("psR", [128, 512])

    P.dma(vec[:], vecs[:, :, :], w=["vec"])
    P.dma(wrs[:], wr.rearrange("(k p) n -> p k n", p=128), w=["wrs"])
    P.dma(brs[:], br[:, :], w=["brs"])
    P.dma(ident[:], ident_d[:, :], w=["ident"])
    P.dma(onehot[:], onehot_d[:, :], w=["onehot"])
    P.add("dve", lambda e: e.memset(ones[:], 1.0), w=["ones"])
    P.add("dve", lambda e: e.memset(epsb[:], EPS), w=["epsb"])
    P.add("dve", lambda e: e.memset(gd[:], 0.0), w=["gd"])
    for c in range(2):
        P.add("dve", lambda e, c=c: e.scalar_tensor_tensor(out=weff[:, c, :], in0=vec[:, c * 4 + 2, :], scalar=1.0,
                                                          in1=vec[:, 8, :], op0=ALU.add, op1=ALU.mult),
              r=["vec"], w=[f"weff{c}"])

    def rms(src, n, srcres):
        P.add("act", lambda e: e.activation(out=h2[:, :, 0:n], in_=src[:, :, 0:n], func=AF.Square), r=[srcres], w=["h2"])

        def mm(e):
            last = None
            for k in range(KT):
                last = e.matmul(psS[:, 0:n], lhsT=ones[:], rhs=h2[:, k, 0:n], start=(k == 0), stop=(k == KT - 1))
            return last
        P.add("pe", mm, r=["h2", "ones"], w=["psS"])
        P.add("act", lambda e: e.activation(out=rstd[:, 0:n], in_=psS[:, 0:n], func=AF.Sqrt, scale=1.0 / D, bias=epsb[:, 0:1]),
              r=["psS", "epsb"], w=["rstd"])
        P.add("dve", lambda e: e.reciprocal(out=rstd[:, 0:n], in_=rstd[:, 0:n]), r=["rstd"], w=["rstd"])

    for (t0, n, cls) in tiles:
        xv = xT.rearrange("(k p) t -> p k t", p=128)
        yv = yT.rearrange("(k p) t -> p k t", p=128)
        P.dma(xt[:, :, 0:n], xv[:, :, t0:t0 + n], w=["xt"])
        P.dma(h2[:, :, 0:n], yv[:, :, t0:t0 + n], w=["h2"], q="pool")

        def ev1(ct, pst, pres, n=n, cls=cls):
            P.add("dve", lambda e: e.scalar_tensor_tensor(out=xt[:, ct, 0:n], in0=pst[:, 0:n], scalar=vec[:, cls * 4 + 0, ct:ct + 1],
                                                          in1=xt[:, ct, 0:n], op0=ALU.mult, op1=ALU.add),
                  r=[pres, "vec", "xt"], w=["xt"])
        mm_stream_fm(P, wout, D, D, lambda k, n=n: h2[:, k, 0:n], ["h2"], n, ev1, "wo", wg, psA)
        rms(xt, n, "xt")
        for k in range(KT):
            P.add("dve", lambda e, k=k, n=n: e.tensor_tensor(out=h2[:, k, 0:n], in0=xt[:, k, 0:n], in1=rstd[:, 0:n], op=ALU.mult),
                  r=["xt", "rstd"], w=["h2"])
        for k in range(KT):
            P.add("pool", lambda e, k=k, n=n, cls=cls: e.tensor_scalar(out=h2[:, k, 0:n], in0=h2[:, k, 0:n], scalar1=weff[:, cls, k:k + 1],
                                                                   scalar2=vec[:, cls * 4 + 1, k:k + 1], op0=ALU.mult, op1=ALU.add),
                  r=["h2", f"weff{cls}", "vec"], w=["h2"])
        for s in range(n // 128):
            def rmm(e, s=s):
                last = None
                for k in range(KT):
                    last = e.matmul(psR[:, 0:36], lhsT=h2[:, k, s * 128:(s + 1) * 128], rhs=wrs[:, k, :], start=(k == 0), stop=(k == KT - 1))
                return last
            P.add("pe", rmm, r=["h2", "wrs"], w=["psR"])
            P.add("dve", lambda e: e.tensor_tensor(out=lg[:], in0=psR[:, 0:36], in1=brs[:], op=ALU.add), r=["psR", "brs"], w=["lg"])
            P.add("dve", lambda e: e.tensor_reduce(out=sm[:, 0:1], in_=lg[:, 0:4], op=ALU.max, axis=AX.X), r=["lg"], w=["sm"])
            P.add("dve", lambda e: e.tensor_scalar(out=sm[:, 1:2], in0=sm[:, 0:1], scalar1=-1.0, scalar2=None, op0=ALU.mult), r=["sm"], w=["sm"])
            P.add("act", lambda e: e.activation(out=sm[:, 48:52], in_=lg[:, 0:4], func=AF.Exp, bias=sm[:, 1:2], scale=1.0, accum_out=sm[:, 2:3]),
                  r=["lg", "sm"], w=["sm"])
            P.add("dve", lambda e: e.reciprocal(out=sm[:, 3:4], in_=sm[:, 2:3]), r=["sm"], w=["sm"])
            P.add("dve", lambda e: e.tensor_scalar(out=sm[:, 4:8], in0=lg[:, 0:4], scalar1=sm[:, 0:1], scalar2=None, op0=ALU.is_equal), r=["lg", "sm"], w=["sm"])
            P.add("dve", lambda e: e.tensor_scalar(out=sm[:, 8:16], in0=lg[:, 4:12], scalar1=sm[:, 4:5], scalar2=None, op0=ALU.mult), r=["lg", "sm"], w=["sm"])
            for g in range(1, 4):
                P.add("dve", lambda e, g=g: e.scalar_tensor_tensor(out=sm[:, 8:16], in0=lg[:, 4 + 8 * g:12 + 8 * g], scalar=sm[:, 4 + g:5 + g],
                                                                  in1=sm[:, 8:16], op0=ALU.mult, op1=ALU.add), r=["lg", "sm"], w=["sm"])
            P.add("dve", lambda e: e.max(out=sm[:, 16:24], in_=sm[:, 8:16]), r=["sm"], w=["sm"])
            P.add("dve", lambda e: e.tensor_scalar(out=sm[:, 24:25], in0=sm[:, 16:17], scalar1=-1.0, scalar2=None, op0=ALU.mult), r=["sm"], w=["sm"])
            P.add("act", lambda e: e.activation(out=sm[:, 25:33], in_=sm[:, 8:16], func=AF.Exp, bias=sm[:, 24:25], scale=1.0), r=["sm"], w=["sm"])
            P.add("act", lambda e: e.activation(out=sm[:, 33:35], in_=sm[:, 16:18], func=AF.Exp, bias=sm[:, 24:25], scale=1.0, accum_out=sm[:, 35:36]),
                  r=["sm"], w=["sm"])
            P.add("dve", lambda e: e.reciprocal(out=sm[:, 36:37], in_=sm[:, 35:36]), r=["sm"], w=["sm"])
            P.add("dve", lambda e: e.tensor_tensor(out=sm[:, 36:37], in0=sm[:, 36:37], in1=sm[:, 3:4], op=ALU.mult), r=["sm"], w=["sm"])
            P.add("dve", lambda e: e.tensor_scalar(out=sm[:, 40:48], in0=sm[:, 8:16], scalar1=sm[:, 17:18], scalar2=None, op0=ALU.is_ge), r=["sm"], w=["sm"])
            P.add("dve", lambda e: e.scalar_tensor_tensor(out=sm[:, 40:48], in0=sm[:, 25:33], scalar=sm[:, 36:37], in1=sm[:, 40:48],
                                                          op0=ALU.mult, op1=ALU.mult), r=["sm"], w=["sm"])
            for g in range(4):
                P.add("dve", lambda e, g=g: e.tensor_scalar(out=gd[:, 8 * g:8 * g + 8], in0=sm[:, 40:48], scalar1=sm[:, 4 + g:5 + g], scalar2=None, op0=ALU.mult),
                      r=["sm"], w=["gd"])
            P.add("pe", lambda e: e.transpose(psR[:, 128:256], gd[:, :], ident[:]), r=["gd", "ident"], w=["psR"])
            P.add("act", lambda e, s=s: e.activation(out=gT[:, s * 128:(s + 1) * 128], in_=psR[:, 128:256], func=AF.Copy), r=["psR"], w=["gT"])

        for ex in range(NE):
            P.add("pe", lambda e, ex=ex, n=n: e.matmul(psS[:, 0:n], lhsT=onehot[:, ex * 128:(ex + 1) * 128], rhs=gT[:, 0:n], start=True, stop=True),
                  r=["onehot", "gT"], w=["psS"])
            P.add("act", lambda e, n=n: e.activation(out=gb[:, 0:n], in_=psS[:, 0:n], func=AF.Copy), r=["psS"], w=["gb"])

            def ev_gu(ct, pst, pres, n=n):
                if ct < 4:
                    P.add("act", lambda e: e.activation(out=sgt[:, 0:n], in_=pst[:, 0:n], func=AF.Silu), r=[pres], w=["sgt"])
                    P.add("dve", lambda e: e.tensor_tensor(out=sg[:, ct, 0:n], in0=sgt[:, 0:n], in1=gb[:, 0:n], op=ALU.mult), r=["sgt", "gb"], w=[f"sg{ct}"])
                else:
                    f = ct - 4
                    P.add("dve", lambda e: e.tensor_tensor(out=act[:, f, 0:n], in0=pst[:, 0:n], in1=sg[:, f, 0:n], op=ALU.mult),
                          r=[pres, f"sg{f}"], w=[f"act{f}"])
            mm_stream_fm(P, wgu[ex], D, 2 * DE, lambda k, n=n: h2[:, k, 0:n], ["h2"], n, ev_gu, "gu", wg, psA)

            def ev_dn(ct, pst, pres, n=n, ex=ex):
                if ex == 0:
                    P.add("dve", lambda e: e.tensor_copy(out=acc[:, ct, 0:n], in_=pst[:, 0:n]), r=[pres], w=[f"acc{ct}"])
                else:
                    P.add("dve", lambda e: e.tensor_tensor(out=acc[:, ct, 0:n], in0=pst[:, 0:n], in1=acc[:, ct, 0:n], op=ALU.add),
                          r=[pres, f"acc{ct}"], w=[f"acc{ct}"])
            mm_stream_fm(P, wdn[ex], DE, D, lambda k, n=n: act[:, k, 0:n], [f"act{f}" for f in range(4)], n, ev_dn, "dn", wd, psB, NC=1024, psw="psB")

        for k in range(KT):
            P.add("dve", lambda e, k=k, n=n, cls=cls: e.scalar_tensor_tensor(out=acc[:, k, 0:n], in0=acc[:, k, 0:n], scalar=vec[:, cls * 4 + 3, k:k + 1],
                                                                          in1=xt[:, k, 0:n], op0=ALU.mult, op1=ALU.add),
                  r=[f"acc{k}", "vec", "xt"], w=[f"acc{k}", "accall"])
        if last:
            rms(acc, n, "accall")
            for k in range(KT):
                P.add("dve", lambda e, k=k, n=n: e.tensor_tensor(out=acc[:, k, 0:n], in0=acc[:, k, 0:n], in1=rstd[:, 0:n], op=ALU.mult),
                      r=["accall", "rstd", f"acc{k}"], w=[f"acc{k}"])
                P.add("pool", lambda e, k=k, n=n: e.tensor_scalar(out=acc[:, k, 0:n], in0=acc[:, k, 0:n], scalar1=vec[:, 9, k:k + 1], scalar2=None, op0=ALU.mult),
                      r=[f"acc{k}", "vec"], w=[f"acc{k}", "accall"])
        ov = out.rearrange("(k p) t -> p k t", p=128)
        P.dma(ov[:, :, t0:t0 + n], acc[:, :, 0:n], r=["accall"] + [f"acc{k}" for k in range(KT)], w=["out"])
    return P.emit()


D = 2048
KT = 16
EPS = 1e-6
NTOK = 8448
NCTX = 256


def dram_ap(t, offset, pat):
    return bass.AP(t.tensor, offset, pat)


def phase_inproj(P, xT, W, NC_ALL, ptm, vec, weff, ones, epsb, TT=384):
    xt = P.sb("ip_xt", [128, KT, TT]); hh = P.sb("ip_h", [128, KT, TT]); rstd = P.sb("ip_rstd", [128, TT])
    ws = [(P.sb(f"ip_w{i}", [128, KT, 256]), f"ip_w{i}") for i in range(2)]
    ot = [(P.sb(f"ip_o{i}", [128, 256]), f"ip_o{i}") for i in range(3)]
    psS = P.ps("ip_psS", [128, 512])
    psO = [(P.ps(f"ip_psO{i}", [128, 512]), f"ip_psO{i}") for i in range(3)]
    tiles = [(0, NCTX, 1)]
    t = NCTX
    while t < NTOK:
        n = min(TT, NTOK - t)
        tiles.append((t, n, 0)); t += n
    xv = xT.rearrange("(k p) t -> p k t", p=128)
    Wv = W.rearrange("(k p) n -> p k n", p=128)
    cnt = 0
    for (t0, n, cls) in tiles:
        P.dma(xt[:, :, 0:n], xv[:, :, t0:t0 + n], w=["ip_xt"])
        P.add("act", lambda e, n=n: e.activation(out=hh[:, :, 0:n], in_=xt[:, :, 0:n], func=AF.Square), r=["ip_xt"], w=["ip_h"])

        def mm(e, n=n):
            last = None
            for k in range(KT):
                last = e.matmul(psS[:, 0:n], lhsT=ones[:], rhs=hh[:, k, 0:n], start=(k == 0), stop=(k == KT - 1))
            return last
        P.add("pe", mm, r=["ip_h", "ones"], w=["ip_psS"])
        P.add("act", lambda e, n=n: e.activation(out=rstd[:, 0:n], in_=psS[:, 0:n], func=AF.Sqrt, scale=1.0 / D, bias=epsb[:, 0:1]),
              r=["ip_psS", "epsb"], w=["ip_rstd"])
        P.add("dve", lambda e, n=n: e.reciprocal(out=rstd[:, 0:n], in_=rstd[:, 0:n]), r=["ip_rstd"], w=["ip_rstd"])
        for k in range(KT):
            P.add("dve", lambda e, k=k, n=n: e.tensor_tensor(out=hh[:, k, 0:n], in0=xt[:, k, 0:n], in1=rstd[:, 0:n], op=ALU.mult),
                  r=["ip_xt", "ip_rstd"], w=["ip_h"])
        for k in range(KT):
            P.add("pool", lambda e, k=k, n=n, cls=cls: e.tensor_scalar(out=hh[:, k, 0:n], in0=hh[:, k, 0:n], scalar1=weff[:, cls, k:k + 1],
                                                                   scalar2=vec[:, cls * 2 + 0, k:k + 1], op0=ALU.mult, op1=ALU.add),
                  r=["ip_h", "weff", "vec"], w=["ip_h"])
        for ci, c0 in enumerate(range(0, NC_ALL, 256)):
            st, sres = ws[ci % 2]
            P.dma(st[:, :, :], Wv[:, :, c0:c0 + 256], w=[sres])
            for s in range(n // 128):
                ps, pres = psO[cnt % 3]
                o, ores = ot[cnt % 3]
                cnt += 1

                def mm2(e, st=st, s=s, ps=ps):
                    last = None
                    for k in range(KT):
                        last = e.matmul(ps[:, 0:256], lhsT=hh[:, k, s * 128:(s + 1) * 128], rhs=st[:, k, :], start=(k == 0), stop=(k == KT - 1))
                    return last
                P.add("pe", mm2, r=[sres, "ip_h"], w=[pres])
                P.add("act", lambda e, ps=ps, o=o: e.activation(out=o[:], in_=ps[:, 0:256], func=AF.Copy), r=[pres], w=[ores])
                P.dma(ptm[t0 + s * 128:t0 + (s + 1) * 128, c0:c0 + 256], o[:], r=[ores], w=["ptm"], q="pool")


def chunk_order(direction):
    if direction == 0:
        return [(c * 128, False) for c in range(NTOK // 128)]
    ctx = [(c * 128, True) for c in reversed(range(NCTX // 128))]
    lat = [(c * 128, True) for c in reversed(range(NCTX // 128, NTOK // 128))]
    return ctx + lat


def rows_ap(dr, row0, rev, c0, ncols):
    width = dr.shape[1]
    if not rev:
        return dr[row0:row0 + 128, c0:c0 + ncols]
    return dram_ap(dr, (row0 + 127) * width + c0, [[-width, 128], [1, ncols]])


def phase_retention(P, ptm, BC0, rope, rde_d, cst_d, ydir, ident, NH=4):
    qk = P.sb("rt_qk", [128, 2 * NH * 64]); v = P.sb("rt_v", [128, NH * 128]); rp = P.sb("rt_rope", [128, 64])
    qr = P.sb("rt_qr", [128, 2 * NH * 64]); tmp = P.sb("rt_tmp", [128, NH * 64])
    qs = P.sb("rt_qs", [128, NH, 128]); km = P.sb("rt_km", [128, NH, 128]); ks = P.sb("rt_ks", [128, NH, 128])
    qT = P.sb("rt_qT", [128, NH // 2, 128]); qsT = P.sb("rt_qsT", [128, NH, 128]); kmT = P.sb("rt_kmT", [128, NH, 128])
    sc = P.sb("rt_sc", [128, NH, 128]); S = P.sb("rt_S", [128, NH, 128]); o = P.sb("rt_o", [128, NH * 128])
    dm = P.sb("rt_dm", [128, NH, 128]); gt = P.sb("rt_gt", [128, 3 * NH])
    psT = P.ps("rt_psT", [128, 512]); psC = P.ps("rt_psC", [128, 512]); psO = P.ps("rt_psO", [128, 512]); psU = P.ps("rt_psU", [128, 512])
    for t_, nm_ in ((qs, "rt_qs"), (km, "rt_km"), (ks, "rt_ks")):
        P.add("dve", lambda e, t_=t_: e.memset(t_[:], 0.0), w=[nm_])
    rde = P.sb("rt_rde", [128, 2 * NH]); cst = P.sb("rt_cst", [128, 259]); lgt = P.sb("rt_lg", [128, 2 * NH])
    P.dma(rde[:], rde_d[:, :], w=["rt_rde"])
    P.add("act", lambda e: e.activation(out=lgt[:], in_=rde[:], func=AF.Exp, scale=0.6931471805599453), r=["rt_rde"], w=["rt_lg"])
    P.add("dve", lambda e: e.tensor_scalar(out=lgt[:], in0=lgt[:], scalar1=-1.0, scalar2=1.0, op0=ALU.mult, op1=ALU.add), r=["rt_lg"], w=["rt_lg"])
    P.add("act", lambda e: e.activation(out=lgt[:], in_=lgt[:], func=AF.Ln), r=["rt_lg"], w=["rt_lg"])
    for d in range(2):
        P.dma(cst[:], cst_d[d], w=["rt_cst"])
        for h in range(NH):
            li = d * NH + h
            P.add("act", lambda e, h=h, li=li: e.activation(out=dm[:, h, :], in_=cst[:, 0:128], func=AF.Exp, scale=lgt[:, li:li + 1]), r=["rt_cst", "rt_lg"], w=["rt_dm"])
            P.add("dve", lambda e, h=h: e.tensor_tensor(out=dm[:, h, :], in0=dm[:, h, :], in1=cst[:, 128:256], op=ALU.mult), r=["rt_dm", "rt_cst"], w=["rt_dm"])
            for j in range(3):
                P.add("act", lambda e, h=h, li=li, j=j: e.activation(out=gt[:, j * NH + h:j * NH + h + 1], in_=cst[:, 256 + j:257 + j], func=AF.Exp, scale=lgt[:, li:li + 1]),
                      r=["rt_cst", "rt_lg"], w=["rt_gt"])
        P.add("dve", lambda e: e.memset(S[:], 0.0), w=["rt_S"])
        for (row0, rev) in chunk_order(d):
            P.dma(qk[:], rows_ap(ptm, row0, False, BC0, 2 * NH * 64), w=["rt_qk"])
            P.dma(v[:], rows_ap(ptm, row0, False, BC0 + 2 * NH * 64, NH * 128), w=["rt_v"])
            P.dma(rp[:], rows_ap(rope, row0, False, 0, 64), w=["rt_rope"])
            x = qk[:].rearrange("p (h two f) -> p h two f", two=2, f=32)
            xo = qr[:].rearrange("p (h two f) -> p h two f", two=2, f=32)
            tv = tmp[:].rearrange("p (h f) -> p h f", f=32)
            cosb = rp[:, 0:32].unsqueeze(1).to_broadcast([128, 2 * NH, 32])
            sinb = rp[:, 32:64].unsqueeze(1).to_broadcast([128, 2 * NH, 32])
            P.add("dve", lambda e, x=x, xo=xo, cosb=cosb: e.tensor_tensor(out=xo[:, :, 0, :], in0=x[:, :, 0, :], in1=cosb, op=ALU.mult), r=["rt_qk", "rt_rope"], w=["rt_qr"])
            P.add("dve", lambda e, x=x, tv=tv, sinb=sinb: e.tensor_tensor(out=tv, in0=x[:, :, 1, :], in1=sinb, op=ALU.mult), r=["rt_qk", "rt_rope"], w=["rt_tmp"])
            P.add("dve", lambda e, xo=xo, tv=tv: e.tensor_tensor(out=xo[:, :, 0, :], in0=xo[:, :, 0, :], in1=tv, op=ALU.subtract), r=["rt_qr", "rt_tmp"], w=["rt_qr"])
            P.add("dve", lambda e, x=x, xo=xo, cosb=cosb: e.tensor_tensor(out=xo[:, :, 1, :], in0=x[:, :, 1, :], in1=cosb, op=ALU.mult), r=["rt_qk", "rt_rope", "rt_qr"], w=["rt_qr"])
            P.add("dve", lambda e, x=x, tv=tv, sinb=sinb: e.tensor_tensor(out=tv, in0=x[:, :, 0, :], in1=sinb, op=ALU.mult), r=["rt_qk", "rt_rope", "rt_qr"], w=["rt_tmp"])
            P.add("dve", lambda e, xo=xo, tv=tv: e.tensor_tensor(out=xo[:, :, 1, :], in0=xo[:, :, 1, :], in1=tv, op=ALU.add), r=["rt_qr", "rt_tmp"], w=["rt_qr"])
            for h in range(NH):
                off = (h % 2) * 64
                qh = qr[:, h * 64:(h + 1) * 64]; kh = qr[:, NH * 64 + h * 64:NH * 64 + (h + 1) * 64]
                P.add("dve", lambda e, h=h, off=off, qh=qh: e.tensor_scalar(out=qs[:, h, off:off + 64], in0=qh, scalar1=gt[:, h:h + 1], scalar2=None, op0=ALU.mult),
                      r=["rt_qr", "rt_gt"], w=["rt_qs"])
                P.add("pool", lambda e, h=h, off=off, kh=kh: e.tensor_scalar(out=km[:, h, off:off + 64], in0=kh, scalar1=0.125, scalar2=None, op0=ALU.mult),
                      r=["rt_qr"], w=["rt_km"])
                P.add("dve", lambda e, h=h, off=off, kh=kh: e.tensor_scalar(out=ks[:, h, off:off + 64], in0=kh, scalar1=gt[:, NH + h:NH + h + 1], scalar2=0.125, op0=ALU.mult, op1=ALU.mult),
                      r=["rt_qr", "rt_gt"], w=["rt_ks"])
            for hp in range(NH // 2):
                P.add("pe", lambda e, hp=hp: e.transpose(psT[:, hp * 128:(hp + 1) * 128], qr[:, hp * 128:(hp + 1) * 128], ident[:]), r=["rt_qr", "ident"], w=["rt_psT"])
            P.add("act", lambda e: e.activation(out=qT[:].rearrange("p a b -> p (a b)"), in_=psT[:, 0:NH // 2 * 128], func=AF.Copy), r=["rt_psT"], w=["rt_qT"])
            for h in range(NH):
                P.add("pe", lambda e, h=h: e.transpose(psT[:, h * 128:(h + 1) * 128], qs[:, h, :], ident[:]), r=["rt_qs", "ident"], w=["rt_psT"])
            P.add("act", lambda e: e.activation(out=qsT[:].rearrange("p a b -> p (a b)"), in_=psT[:, 0:NH * 128], func=AF.Copy), r=["rt_psT"], w=["rt_qsT"])
            for h in range(NH):
                P.add("pe", lambda e, h=h: e.transpose(psT[:, h * 128:(h + 1) * 128], km[:, h, :], ident[:]), r=["rt_km", "ident"], w=["rt_psT"])
            P.add("act", lambda e: e.activation(out=kmT[:].rearrange("p a b -> p (a b)"), in_=psT[:, 0:NH * 128], func=AF.Copy), r=["rt_psT"], w=["rt_kmT"])
            for h in range(NH):
                P.add("pe", lambda e, h=h: e.matmul(psC[:, h * 128:(h + 1) * 128], lhsT=kmT[:, h, :], rhs=qT[:, h // 2, :], start=True, stop=True),
                      r=["rt_kmT", "rt_qT"], w=["rt_psC"])
            P.add("dve", lambda e: e.tensor_tensor(out=sc[:].rearrange("p a b -> p (a b)"), in0=psC[:, 0:NH * 128], in1=dm[:].rearrange("p a b -> p (a b)"), op=ALU.mult),
                  r=["rt_psC", "rt_dm"], w=["rt_sc"])
            for h in range(NH):
                def mmo(e, h=h):
                    e.matmul(psO[:, h * 128:(h + 1) * 128], lhsT=sc[:, h, :], rhs=v[:, h * 128:(h + 1) * 128], start=True, stop=False)
                    return e.matmul(psO[:, h * 128:(h + 1) * 128], lhsT=qsT[:, h, :], rhs=S[:, h, :], start=False, stop=True)
                P.add("pe", mmo, r=["rt_sc", "rt_v", "rt_qsT", "rt_S"], w=["rt_psO"])
            P.add("act", lambda e: e.activation(out=o[:], in_=psO[:, 0:NH * 128], func=AF.Copy), r=["rt_psO"], w=["rt_o"])
            P.dma(ydir[d][row0:row0 + 128, :], o[:], r=["rt_o"], w=["ydir"], q="pool")
            for h in range(NH):
                P.add("pe", lambda e, h=h: e.matmul(psU[:, h * 128:(h + 1) * 128], lhsT=ks[:, h, :], rhs=v[:, h * 128:(h + 1) * 128], start=True, stop=True),
                      r=["rt_ks", "rt_v"], w=["rt_psU"])
            for h in range(NH):
                P.add("dve", lambda e, h=h: e.scalar_tensor_tensor(out=S[:, h, :], in0=S[:, h, :], scalar=gt[:, 2 * NH + h:2 * NH + h + 1], in1=psU[:, h * 128:(h + 1) * 128],
                                                                  op0=ALU.mult, op1=ALU.add), r=["rt_S", "rt_gt", "rt_psU"], w=["rt_S"])


def phase_ret_finish(P, ptm, GC0, ydir, yout, YC0, NH=4):
    a = P.sb("rf_a", [128, NH * 128]); b = P.sb("rf_b", [128, NH * 128]); g = P.sb("rf_g", [128, NH * 128])
    st = P.sb("rf_st", [128, NH, 8]); sq = P.sb("rf_sq", [128, NH * 128]); epsb = P.sb("rf_eps", [128, 1])
    P.add("dve", lambda e: e.memset(epsb[:], EPS), w=["rf_eps"])
    for c in range(NTOK // 128):
        r0 = c * 128
        P.dma(a[:], ydir[0][r0:r0 + 128, :], r=["ydir"], w=["rf_a"])
        P.dma(b[:], ydir[1][r0:r0 + 128, :], r=["ydir"], w=["rf_b"])
        P.dma(g[:], ptm[r0:r0 + 128, GC0:GC0 + NH * 128], r=["ptm"], w=["rf_g"])
        P.add("dve", lambda e: e.tensor_tensor(out=a[:], in0=a[:], in1=b[:], op=ALU.add), r=["rf_a", "rf_b"], w=["rf_a"])
        av = a[:].rearrange("p (h e) -> p h e", e=128)
        P.add("dve", lambda e, av=av: e.tensor_reduce(out=st[:, :, 0], in_=av, op=ALU.add, axis=AX.X), r=["rf_a"], w=["rf_st"])
        P.add("dve", lambda e: e.tensor_scalar(out=st[:, :, 1], in0=st[:, :, 0], scalar1=-1.0 / 128, scalar2=None, op0=ALU.mult), r=["rf_st"], w=["rf_st"])
        P.add("dve", lambda e, av=av: e.tensor_tensor(out=av, in0=av, in1=st[:, :, 1:2].to_broadcast([128, NH, 128]), op=ALU.add), r=["rf_a", "rf_st"], w=["rf_a"])
        P.add("act", lambda e: e.activation(out=sq[:], in_=a[:], func=AF.Square), r=["rf_a"], w=["rf_sq"])
        P.add("dve", lambda e: e.tensor_reduce(out=st[:, :, 2], in_=sq[:].rearrange("p (h e) -> p h e", e=128), op=ALU.add, axis=AX.X), r=["rf_sq"], w=["rf_st"])
        P.add("act", lambda e: e.activation(out=st[:, :, 3], in_=st[:, :, 2], func=AF.Sqrt, scale=1.0 / 128, bias=epsb[:, 0:1]), r=["rf_st", "rf_eps"], w=["rf_st"])
        P.add("dve", lambda e: e.reciprocal(out=st[:, :, 3], in_=st[:, :, 3]), r=["rf_st"], w=["rf_st"])
        P.add("dve", lambda e, av=av: e.tensor_tensor(out=av, in0=av, in1=st[:, :, 3:4].to_broadcast([128, NH, 128]), op=ALU.mult), r=["rf_a", "rf_st"], w=["rf_a"])
        P.add("act", lambda e: e.activation(out=g[:], in_=g[:], func=AF.Silu), r=["rf_g"], w=["rf_g"])
        P.add("dve", lambda e: e.tensor_tensor(out=a[:], in0=a[:], in1=g[:], op=ALU.mult), r=["rf_a", "rf_g"], w=["rf_a"])
        P.dma(yout[r0:r0 + 128, YC0:YC0 + NH * 128], a[:], r=["rf_a"], w=["yout"], q="pool")


NEG_E05 = -0.6065306597126334
GN_EPS = 64e-5


def phase_rwkv_prep(P, ptm, shmask, mu_d, cv_d, lw_d, Rd, Fd, VTd, ident):
    z = P.sb("rp_z", [128, 2048]); zs = [P.sb(f"rp_zs{j}", [128, 2048]) for j in range(4)]
    zz = P.sb("rp_zz", [128, 2048]); dd = P.sb("rp_d", [128, 512]); mk = P.sb("rp_mk", [128, 4])
    mu = P.sb("rp_mu", [128, 2048]); cv = P.sb("rp_cv", [128, 8, 512]); lw = P.sb("rp_lw", [128, 5, 512])
    lT = P.sb("rp_lT", [128, 4, 128]); Rt = P.sb("rp_R", [128, 6, 512]); Ft = P.sb("rp_F", [128, 2, 512])
    a = P.sb("rp_a", [128, 512]); t1 = P.sb("rp_t1", [128, 512]); kkr = P.sb("rp_kkr", [128, 512]); st = P.sb("rp_st", [128, 16])
    vp = P.sb("rp_vp", [128, 512]); vT = P.sb("rp_vT", [128, 512]); e12 = P.sb("rp_e12", [128, 1])
    psT = P.ps("rp_psT", [128, 512]); psA = P.ps("rp_psA", [128, 512]); psF = P.ps("rp_psF", [128, 512]); psB = P.ps("rp_psB", [128, 512])
    psG = P.ps("rp_psG", [128, 512]); psV = P.ps("rp_psV", [128, 512])
    P.dma(mu[:], mu_d[:, :], w=["rp_mu"]); P.dma(cv[:], cv_d[:, :, :], w=["rp_cv"]); P.dma(lw[:], lw_d[:, :, :], w=["rp_lw"])
    P.add("dve", lambda e: e.memset(e12[:], 1e-12), w=["rp_e12"])
    for j in range(4):
        P.add("pool", lambda e, j=j: e.memset(zs[j][:], 0.0), w=[f"rp_zs{j}"])
    for c in range(NTOK // 128):
        r0 = c * 128
        offs = (-1, 1, -1, 1) if r0 < NCTX else (-1, 1, -64, 64)
        P.dma(z[:], ptm[r0:r0 + 128, 0:2048], r=["ptm"], w=["rp_z"])
        P.dma(mk[:], shmask[r0:r0 + 128, :], w=["rp_mk"])
        for j in range(4):
            lo = max(r0 + offs[j], 0); hi = min(r0 + offs[j] + 128, NTOK)
            p0 = lo - (r0 + offs[j])
            P.dma(zs[j][p0:p0 + (hi - lo), :], ptm[lo:hi, 0:2048], r=["ptm"], w=[f"rp_zs{j}"])
        zv = z[:].rearrange("p (c four) -> p c four", four=4)
        zzv = zz[:].rearrange("p (c four) -> p c four", four=4)
        muv = mu[:].rearrange("p (c four) -> p c four", four=4)
        for j in range(4):
            zsv = zs[j][:].rearrange("p (c four) -> p c four", four=4)
            P.add("dve", lambda e, j=j, zsv=zsv, zv=zv: e.scalar_tensor_tensor(out=dd[:], in0=zsv[:, :, j], scalar=mk[:, j:j + 1], in1=zv[:, :, j], op0=ALU.mult, op1=ALU.subtract),
                  r=[f"rp_zs{j}", "rp_mk", "rp_z"], w=["rp_d"])
            P.add("dve", lambda e, j=j, muv=muv: e.tensor_tensor(out=dd[:], in0=dd[:], in1=muv[:, :, j], op=ALU.mult), r=["rp_d", "rp_mu"], w=["rp_d"])
            P.add("dve", lambda e, j=j, zv=zv, zzv=zzv: e.tensor_tensor(out=zzv[:, :, j], in0=dd[:], in1=zv[:, :, j], op=ALU.add), r=["rp_d", "rp_z"], w=["rp_zz"])
        P.add("act", lambda e: e.activation(out=zz[:, 1600:1728], in_=zz[:, 1600:1728], func=AF.Tanh), r=["rp_zz"], w=["rp_zz"])
        P.add("act", lambda e: e.activation(out=zz[:, 1792:1952], in_=zz[:, 1792:1952], func=AF.Sigmoid), r=["rp_zz"], w=["rp_zz"])
        for i in range(4):
            P.add("pe", lambda e, i=i: e.transpose(psT[:, i * 128:(i + 1) * 128], zz[:, 1536 + i * 128:1536 + (i + 1) * 128], ident[:]), r=["rp_zz", "ident"], w=["rp_psT"])
        P.add("act", lambda e: e.activation(out=lT[:].rearrange("p a b -> p (a b)"), in_=psT[:, :], func=AF.Copy), r=["rp_psT"], w=["rp_lT"])
        P.add("pe", lambda e: e.matmul(psA[:, :], lhsT=lT[:, 0, :], rhs=lw[:, 0, :], start=True, stop=True), r=["rp_lT", "rp_lw"], w=["rp_psA"])
        P.add("pe", lambda e: e.matmul(psF[:, :], lhsT=lT[:, 0, :], rhs=lw[:, 1, :], start=True, stop=True), r=["rp_lT", "rp_lw"], w=["rp_psF"])
        P.add("pe", lambda e: e.matmul(psB[:, :], lhsT=lT[:, 1, :], rhs=lw[:, 2, :], start=True, stop=True), r=["rp_lT", "rp_lw"], w=["rp_psB"])

        def mmg(e):
            e.matmul(psG[:, :], lhsT=lT[:, 2, :], rhs=lw[:, 3, :], start=True, stop=False)
            return e.matmul(psG[:, :], lhsT=lT[:, 3, :], rhs=lw[:, 4, :], start=False, stop=True)
        P.add("pe", mmg, r=["rp_lT", "rp_lw"], w=["rp_psG"])
        P.add("dve", lambda e: e.tensor_tensor(out=a[:], in0=psA[:, :], in1=cv[:, 0, :], op=ALU.add), r=["rp_psA", "rp_cv"], w=["rp_a"])
        P.add("act", lambda e: e.activation(out=a[:], in_=a[:], func=AF.Sigmoid), r=["rp_a"], w=["rp_a"])
        for (ps_, pres, ci, ri) in ((psF, "rp_psF", 1, 4), (psB, "rp_psB", 2, 5)):
            P.add("dve", lambda e, ps_=ps_, ci=ci, ri=ri: e.tensor_tensor(out=Rt[:, ri, :], in0=ps_[:, :], in1=cv[:, ci, :], op=ALU.add), r=[pres, "rp_cv"], w=[f"rp_R{ri}"])
            P.add("act", lambda e, ri=ri: e.activation(out=Rt[:, ri, :], in_=Rt[:, ri, :], func=AF.Sigmoid), r=[f"rp_R{ri}"], w=[f"rp_R{ri}"])
            P.add("act", lambda e, ri=ri: e.activation(out=Rt[:, ri, :], in_=Rt[:, ri, :], func=AF.Exp, scale=NEG_E05), r=[f"rp_R{ri}"], w=[f"rp_R{ri}"])
        P.add("act", lambda e: e.activation(out=Ft[:, 1, :], in_=psG[:, :], func=AF.Copy), r=["rp_psG"], w=["rp_F1"])
        P.add("pool", lambda e: e.tensor_copy(out=Ft[:, 0, :], in_=zz[:, 1024:1536]), r=["rp_zz"], w=["rp_F0"])
        P.add("pool", lambda e: e.tensor_copy(out=Rt[:, 3, :], in_=zz[:, 0:512]), r=["rp_zz"], w=["rp_R3"])
        P.add("dve", lambda e: e.tensor_tensor(out=kkr[:], in0=zz[:, 512:1024], in1=cv[:, 3, :], op=ALU.mult), r=["rp_zz", "rp_cv"], w=["rp_kkr"])
        P.add("act", lambda e: e.activation(out=t1[:], in_=kkr[:], func=AF.Square), r=["rp_kkr"], w=["rp_t1"])
        P.add("dve", lambda e: e.tensor_reduce(out=st[:, 0:8], in_=t1[:].rearrange("p (h k) -> p h k", k=64), op=ALU.add, axis=AX.X), r=["rp_t1"], w=["rp_st"])
        P.add("act", lambda e: e.activation(out=st[:, 8:16], in_=st[:, 0:8], func=AF.Sqrt, bias=e12[:, 0:1], scale=1.0), r=["rp_st", "rp_e12"], w=["rp_st"])
        P.add("dve", lambda e: e.reciprocal(out=st[:, 8:16], in_=st[:, 8:16]), r=["rp_st"], w=["rp_st"])
        P.add("dve", lambda e: e.tensor_tensor(out=kkr[:].rearrange("p (h k) -> p h k", k=64), in0=kkr[:].rearrange("p (h k) -> p h k", k=64),
                                               in1=st[:, 8:16].unsqueeze(2).to_broadcast([128, 8, 64]), op=ALU.mult), r=["rp_kkr", "rp_st"], w=["rp_kkr"])
        P.add("dve", lambda e: e.tensor_tensor(out=Rt[:, 1, :], in0=kkr[:], in1=a[:], op=ALU.mult), r=["rp_kkr", "rp_a"], w=["rp_R1"])
        P.add("pool", lambda e: e.tensor_scalar(out=Rt[:, 0, :], in0=kkr[:], scalar1=-1.0, scalar2=None, op0=ALU.mult), r=["rp_kkr"], w=["rp_R0"])
        P.add("dve", lambda e: e.scalar_tensor_tensor(out=t1[:], in0=a[:], scalar=-1.0, in1=cv[:, 4, :], op0=ALU.add, op1=ALU.mult), r=["rp_a", "rp_cv", "rp_st"], w=["rp_t1"])
        P.add("dve", lambda e: e.scalar_tensor_tensor(out=Rt[:, 2, :], in0=t1[:], scalar=1.0, in1=zz[:, 512:1024], op0=ALU.add, op1=ALU.mult), r=["rp_t1", "rp_zz"], w=["rp_R2"])
        allR = [f"rp_R{i}" for i in range(6)]
        P.dma(Rd[r0:r0 + 128, :, :], Rt[:], r=allR, w=["Rd"] + [], q="pool")
        P.dma(Fd[r0:r0 + 128, :, :], Ft[:], r=["rp_F0", "rp_F1"], w=["Fd"], q="pool")
        P.add("pool", lambda e: e.tensor_copy(out=vp[:].rearrange("p (hp hs e) -> p hs hp e", hs=2, e=64), in_=zz[:, 1024:1536].rearrange("p (hs hp e) -> p hs hp e", hs=2, e=64)),
              r=["rp_zz"], w=["rp_vp"])
        for hp in range(4):
            P.add("pe", lambda e, hp=hp: e.transpose(psV[:, hp * 128:(hp + 1) * 128], vp[:, hp * 128:(hp + 1) * 128], ident[:]), r=["rp_vp", "ident"], w=["rp_psV"])
        P.add("act", lambda e: e.activation(out=vT[:], in_=psV[:, :], func=AF.Copy), r=["rp_psV"], w=["rp_vT"])
        P.dma(VTd[c], vT[:], r=["rp_vT"], w=["VTd"], q="pool")


def phase_rwkv_scan(P, Rd, VTd, ydir, ident, TB=8, max_chunks=None):
    S = P.sb("rs_S", [128, 256]); tmp = P.sb("rs_tmp", [128, 256]); tmp2 = [P.sb(f"rs_tmp2{i}", [128, 256]) for i in range(2)]
    sa = P.sb("rs_sa", [128, 4]); vT = [P.sb(f"rs_vT{i}", [128, 4, 128]) for i in range(2)]; Y = [P.sb(f"rs_Y{i}", [128, 4, 128]) for i in range(2)]
    BC = [P.sb(f"rs_BC{i}", [128, 5, TB, 256]) for i in range(2)]
    yo = [P.sb(f"rs_yo{i}", [128, 512]) for i in range(2)]
    psY = [P.ps(f"rs_psY{i}", [128, 512]) for i in range(2)]
    Rt = Rd.tensor
    RW = 6 * 512
    blk = 0
    for d in range(2):
        P.add("dve", lambda e: e.memset(S[:], 0.0), w=["rs_S"])
        for ci, (row0, rev) in enumerate(chunk_order(d)[:max_chunks]):
            c = row0 // 128
            vt = vT[ci % 2]; vres = f"rs_vT{ci % 2}"; Yc = Y[ci % 2]; yres = f"rs_Y{ci % 2}"
            P.dma(vt[:].rearrange("p a b -> p (a b)"), VTd[c], r=["VTd"], w=[vres])
            for b0 in range(0, 128, TB):
                bc = BC[blk % 2]; bres = f"rs_BC{blk % 2}"; blk += 1
                tb0 = row0 + (b0 if not rev else 128 - b0 - TB)
                for hs in range(2):
                    for xi in range(5):
                        xs = xi if xi < 4 else 4 + d
                        src = bass.AP(Rt, tb0 * RW + xs * 512 + hs * 256, [[0, 64], [RW, TB], [1, 256]])
                        P.dma(bc[hs * 64:(hs + 1) * 64, xi, :, :], src, r=["Rd"], w=[bres])
                for ti in range(TB):
                    tl = ti if not rev else TB - 1 - ti
                    tc = (b0 + ti) if not rev else (127 - b0 - ti)
                    t2 = tmp2[ti % 2]; t2res = f"rs_tmp2{ti % 2}"
                    S3 = S[:].rearrange("p (h k) -> p h k", k=64)
                    tm3 = tmp[:].rearrange("p (h k) -> p h k", k=64)
                    P.add("pool", lambda e, bc=bc, tl=tl, vt=vt, tc=tc, t2=t2: e.tensor_tensor(out=t2[:].rearrange("p (h k) -> p h k", k=64),
                          in0=bc[:, 2, tl, :].rearrange("p (h k) -> p h k", k=64), in1=vt[:, :, tc:tc + 1].to_broadcast([128, 4, 64]), op=ALU.mult),
                          r=[bres, vres], w=[t2res])
                    P.add("dve", lambda e, bc=bc, tl=tl: e.tensor_tensor(out=tmp[:], in0=S[:], in1=bc[:, 0, tl, :], op=ALU.mult), r=["rs_S", bres], w=["rs_tmp"])
                    P.add("dve", lambda e, tm3=tm3: e.tensor_reduce(out=sa[:], in_=tm3, op=ALU.add, axis=AX.X), r=["rs_tmp"], w=["rs_sa"])
                    P.add("dve", lambda e, bc=bc, tl=tl: e.tensor_tensor(out=S[:], in0=S[:], in1=bc[:, 4, tl, :], op=ALU.mult), r=["rs_S", bres, "rs_tmp"], w=["rs_S"])
                    P.add("dve", lambda e, bc=bc, tl=tl, tm3=tm3: e.tensor_tensor(out=tm3, in0=bc[:, 1, tl, :].rearrange("p (h k) -> p h k", k=64),
                                                                          in1=sa[:].unsqueeze(2).to_broadcast([128, 4, 64]), op=ALU.mult), r=[bres, "rs_sa"], w=["rs_tmp"])
                    P.add("dve", lambda e: e.tensor_tensor(out=S[:], in0=S[:], in1=tmp[:], op=ALU.add), r=["rs_S", "rs_tmp"], w=["rs_S"])
                    P.add("dve", lambda e, t2=t2: e.tensor_tensor(out=S[:], in0=S[:], in1=t2[:], op=ALU.add), r=["rs_S", t2res], w=["rs_S"])
                    P.add("dve", lambda e, bc=bc, tl=tl: e.tensor_tensor(out=tmp[:], in0=S[:], in1=bc[:, 3, tl, :], op=ALU.mult), r=["rs_S", bres], w=["rs_tmp"])
                    P.add("dve", lambda e, tm3=tm3, Yc=Yc, tc=tc: e.tensor_reduce(out=Yc[:, :, tc], in_=tm3, op=ALU.add, axis=AX.X), r=["rs_tmp"], w=[yres])
            ps = psY[ci % 2]; pres = f"rs_psY{ci % 2}"; yob = yo[ci % 2]; yores = f"rs_yo{ci % 2}"
            for hp in range(4):
                P.add("pe", lambda e, hp=hp, ps=ps, Yc=Yc: e.transpose(ps[:, hp * 128:(hp + 1) * 128], Yc[:, hp, :], ident[:]), r=[yres, "ident"], w=[pres])
            P.add("act", lambda e, ps=ps, yob=yob: e.activation(out=yob[:], in_=ps[:, :], func=AF.Copy), r=[pres], w=[yores])
            P.dma(ydir[d][row0:row0 + 128, :], yob[:], r=[yores], w=["ydirA"], q="pool")


def phase_rwkv_finish(P, Rd, Fd, cv_d, ydir, yout):
    a = P.sb("wf_a", [128, 512]); b = P.sb("wf_b", [128, 512]); y = P.sb("wf_y", [128, 512]); Rt = P.sb("wf_R", [128, 2, 512]); Ft = P.sb("wf_F", [128, 2, 512])
    cv = P.sb("wf_cv", [128, 8, 512]); st = P.sb("wf_st", [128, 8, 4]); sq = P.sb("wf_sq", [128, 512]); epsb = P.sb("wf_eps", [128, 1])
    P.dma(cv[:], cv_d[:, :, :], w=["wf_cv"])
    P.add("dve", lambda e: e.memset(epsb[:], GN_EPS), w=["wf_eps"])
    h3 = lambda t: t[:].rearrange("p (h k) -> p h k", k=64)
    for c in range(NTOK // 128):
        r0 = c * 128
        P.dma(a[:], ydir[0][r0:r0 + 128, :], r=["ydirA"], w=["wf_a"])
        P.dma(b[:], ydir[1][r0:r0 + 128, :], r=["ydirA"], w=["wf_b"])
        P.dma(Rt[:], Rd[r0:r0 + 128, 2:4, :], r=["Rd"], w=["wf_R"])
        P.dma(Ft[:], Fd[r0:r0 + 128, :, :], r=["Fd"], w=["wf_F"])
        P.add("dve", lambda e: e.tensor_tensor(out=a[:], in0=a[:], in1=b[:], op=ALU.add), r=["wf_a", "wf_b"], w=["wf_a"])
        P.add("pool", lambda e: e.tensor_copy(out=y[:].rearrange("p (hs hp e) -> p hs hp e", hs=2, e=64), in_=a[:].rearrange("p (hp hs e) -> p hs hp e", hs=2, e=64)), r=["wf_a"], w=["wf_y"])
        P.add("dve", lambda e: e.tensor_reduce(out=st[:, :, 0], in_=h3(y), op=ALU.add, axis=AX.X), r=["wf_y"], w=["wf_st"])
        P.add("dve", lambda e: e.tensor_scalar(out=st[:, :, 1], in0=st[:, :, 0], scalar1=-1.0 / 64, scalar2=None, op0=ALU.mult), r=["wf_st"], w=["wf_st"])
        P.add("dve", lambda e: e.tensor_tensor(out=h3(y), in0=h3(y), in1=st[:, :, 1:2].to_broadcast([128, 8, 64]), op=ALU.add), r=["wf_y", "wf_st"], w=["wf_y"])
        P.add("act", lambda e: e.activation(out=sq[:], in_=y[:], func=AF.Square), r=["wf_y"], w=["wf_sq"])
        P.add("dve", lambda e: e.tensor_reduce(out=st[:, :, 2], in_=h3(sq), op=ALU.add, axis=AX.X), r=["wf_sq"], w=["wf_st"])
        P.add("act", lambda e: e.activation(out=st[:, :, 3], in_=st[:, :, 2], func=AF.Sqrt, scale=1.0 / 64, bias=epsb[:, 0:1]), r=["wf_st", "wf_eps"], w=["wf_st"])
        P.add("dve", lambda e: e.reciprocal(out=st[:, :, 3], in_=st[:, :, 3]), r=["wf_st"], w=["wf_st"])
        P.add("dve", lambda e: e.tensor_tensor(out=h3(y), in0=h3(y), in1=st[:, :, 3:4].to_broadcast([128, 8, 64]), op=ALU.mult), r=["wf_y", "wf_st"], w=["wf_y"])
        P.add("dve", lambda e: e.tensor_tensor(out=y[:], in0=y[:], in1=cv[:, 6, :], op=ALU.mult), r=["wf_y", "wf_cv"], w=["wf_y"])
        P.add("dve", lambda e: e.tensor_tensor(out=y[:], in0=y[:], in1=cv[:, 7, :], op=ALU.add), r=["wf_y", "wf_cv"], w=["wf_y"])
        P.add("pool", lambda e: e.tensor_tensor(out=sq[:], in0=Rt[:, 0, :], in1=Rt[:, 1, :], op=ALU.mult), r=["wf_R", "wf_st"], w=["wf_sq"])
        P.add("pool", lambda e: e.tensor_tensor(out=sq[:], in0=sq[:], in1=cv[:, 5, :], op=ALU.mult), r=["wf_sq", "wf_cv"], w=["wf_sq"])
        P.add("dve", lambda e: e.tensor_reduce(out=st[:, :, 0], in_=h3(sq), op=ALU.add, axis=AX.X), r=["wf_sq", "wf_y"], w=["wf_st"])
        P.add("dve", lambda e: e.tensor_tensor(out=h3(sq), in0=Ft[:, 0, :].rearrange("p (h k) -> p h k", k=64), in1=st[:, :, 0:1].to_broadcast([128, 8, 64]), op=ALU.mult),
              r=["wf_F", "wf_st"], w=["wf_sq"])
        P.add("dve", lambda e: e.tensor_tensor(out=y[:], in0=y[:], in1=sq[:], op=ALU.add), r=["wf_y", "wf_sq"], w=["wf_y"])
        P.add("dve", lambda e: e.tensor_tensor(out=y[:], in0=y[:], in1=Ft[:, 1, :], op=ALU.mult), r=["wf_y", "wf_F"], w=["wf_y"])
        P.dma(yout[r0:r0 + 128, 0:512], y[:], r=["wf_y"], w=["yout"], q="pool")


def build_la0(do_rwkv=True, do_ret=True, dbg=False, max_chunks=None):
    nc = bass.Bass("TRN2", target_bir_lowering=False)
    P = Prog(nc)
    P.use_arena(36000)
    ei = lambda name, shape: nc.dram_tensor(name, list(shape), F32, kind="ExternalInput").ap()
    NC_ALL = 3584
    xT = ei("xT", [D, NTOK]); W = ei("W", [D, NC_ALL])
    vecs = ei("vecs", [128, 5, KT])
    rope = ei("rope", [NTOK, 64]); rde = ei("rde", [128, 8]); rcst = ei("rcst", [2, 128, 259]); ident_d = ei("ident", [128, 128])
    if do_rwkv:
        shmask = ei("shmask", [NTOK, 4]); mu_d = ei("mu", [128, 2048]); cv_d = ei("cv", [128, 8, 512]); lw_d = ei("lw", [128, 5, 512])
    yout = nc.dram_tensor("yout", [NTOK, 1024], BF16, kind="ExternalOutput").ap()
    dk = "ExternalOutput" if dbg else "Internal"
    ptm = nc.dram_tensor("ptm", [NTOK, NC_ALL], F32, kind=dk).ap()
    ydirB = nc.dram_tensor("ydirB", [2, NTOK, 512], F32).ap()
    ydirA = nc.dram_tensor("ydirA", [2, NTOK, 512], F32).ap()
    Rd = nc.dram_tensor("Rd", [NTOK, 6, 512], F32).ap()
    Fd = nc.dram_tensor("Fd", [NTOK, 2, 512], F32).ap()
    VTd = nc.dram_tensor("VTd", [NTOK // 128, 128, 512], F32).ap()
    vec = P.sb("vec", [128, 5, KT]); weff = P.sb("weff", [128, 2, KT]); ones = P.sb("ones", [128, 128]); epsb = P.sb("epsb", [128, 1])
    ident = P.sb("ident_s", [128, 128])
    P.persist()
    P.dma(vec[:], vecs[:, :, :], w=["vec"]); P.dma(ident[:], ident_d[:, :], w=["ident"])
    P.add("dve", lambda e: e.memset(ones[:], 1.0), w=["ones"])
    P.add("dve", lambda e: e.memset(epsb[:], EPS), w=["epsb"])
    for c in range(2):
        P.add("dve", lambda e, c=c: e.scalar_tensor_tensor(out=weff[:, c, :], in0=vec[:, c * 2 + 1, :], scalar=1.0, in1=vec[:, 4, :], op0=ALU.add, op1=ALU.mult),
              r=["vec"], w=["weff"])
    phase_inproj(P, xT, W, NC_ALL, ptm, vec, weff, ones, epsb)
    if do_ret:
        P.phase()
        phase_retention(P, ptm, 2048, rope, rde, rcst, ydirB, ident)
        P.phase()
        phase_ret_finish(P, ptm, 2048 + 1024, ydirB, yout, 512)
    if do_rwkv:
        P.phase()
        phase_rwkv_prep(P, ptm, shmask, mu_d, cv_d, lw_d, Rd, Fd, VTd, ident)
        P.phase()
        phase_rwkv_scan(P, Rd, VTd, ydirA, ident, max_chunks=max_chunks)
        P.phase()
        phase_rwkv_finish(P, Rd, Fd, cv_d, ydirA, yout)
    return P.emit()


def phase_hgrn(P, ptm, QC0, lbl_d, hcst_d, ydir, ident, NH=6):
    W = NH * 128
    HG = 3
    q = P.sb("hg_q", [128, W]); f = P.sb("hg_f", [128, W]); v = P.sb("hg_v", [128, W]); k = P.sb("hg_k", [128, W]); lf = P.sb("hg_lf", [128, W])
    lb = P.sb("hg_lb", [128, 2, W]); oml = P.sb("hg_oml", [128, W]); cst = P.sb("hg_cst", [128, 516])
    cb = P.sb("hg_cb", [128, 3, HG * 128]); ta = P.sb("hg_ta", [128, HG * 128]); tb = P.sb("hg_tb", [128, HG * 128])
    eq = P.sb("hg_eq", [128, HG * 128]); ek = P.sb("hg_ek", [128, HG * 128]); eb = P.sb("hg_eb", [128, HG * 128]); el = P.sb("hg_el", [128, HG * 128])
    pre = P.sb("hg_pre", [128, HG, 4, 128])
    ks = P.sb("hg_ks", [128, HG, 2, 128]); preT = P.sb("hg_preT", [128, HG, 4, 128]); dec = P.sb("hg_dec", [128, HG, 2])
    sc = P.sb("hg_sc", [128, HG, 128]); S = P.sb("hg_S", [128, NH, 128]); o = P.sb("hg_o", [128, W])
    banks = [P.ps(f"hg_b{i}", [128, 512]) for i in range(8)]
    bn = [f"hg_b{i}" for i in range(8)]
    P.dma(lb[:], lbl_d[:, :, :], w=["hg_lb"])
    P.add("dve", lambda e: e.tensor_tensor(out=lb[:, 0, :], in0=lb[:, 1, :], in1=lb[:, 0, :], op=ALU.subtract), r=["hg_lb"], w=["hg_lb"])
    P.add("act", lambda e: e.activation(out=lb[:, 0, :], in_=lb[:, 0, :], func=AF.Sigmoid), r=["hg_lb"], w=["hg_lb"])
    P.add("dve", lambda e: e.tensor_scalar(out=oml[:], in0=lb[:, 0, :], scalar1=-1.0, scalar2=1.0, op0=ALU.mult, op1=ALU.add), r=["hg_lb"], w=["hg_oml"])
    for d in range(2):
        P.dma(cst[:], hcst_d[d], w=["hg_cst"])
        TRI = cst[:, 0:128]; MID = cst[:, 128:256]; ALLC = cst[:, 256:384]; CM = cst[:, 384:512]
        P.add("dve", lambda e: e.memset(S[:], 0.0), w=[f"hg_S{h}" for h in range(NH)])
        order = (0, 1) if d == 0 else (1, 0)
        for (row0, rev) in chunk_order(d):
            P.dma(q[:], ptm[row0:row0 + 128, QC0:QC0 + W], r=["ptm"], w=["hg_q"])
            P.dma(f[:], ptm[row0:row0 + 128, QC0 + (1 + d) * W:QC0 + (2 + d) * W], r=["ptm"], w=["hg_f"])
            P.dma(v[:], ptm[row0:row0 + 128, QC0 + 3 * W:QC0 + 4 * W], r=["ptm"], w=["hg_v"])
            P.add("act", lambda e: e.activation(out=q[:], in_=q[:], func=AF.Silu), r=["hg_q"], w=["hg_q"])
            P.add("act", lambda e: e.activation(out=f[:], in_=f[:], func=AF.Sigmoid), r=["hg_f"], w=["hg_f"])
            P.add("dve", lambda e: e.tensor_tensor(out=f[:], in0=f[:], in1=oml[:], op=ALU.mult), r=["hg_f", "hg_oml"], w=["hg_f"])
            P.add("dve", lambda e: e.tensor_tensor(out=f[:], in0=f[:], in1=lb[:, 0, :], op=ALU.add), r=["hg_f", "hg_lb"], w=["hg_f"])
            P.add("pool", lambda e: e.tensor_scalar(out=k[:], in0=f[:], scalar1=-1.0, scalar2=1.0, op0=ALU.mult, op1=ALU.add), r=["hg_f"], w=["hg_k"])
            P.add("act", lambda e: e.activation(out=lf[:], in_=f[:], func=AF.Ln), r=["hg_f"], w=["hg_lf"])
            for g0 in range(0, NH, HG):
                c0 = g0 * 128; cw = HG * 128
                for i, M in enumerate((TRI, MID, ALLC)):
                    P.add("pe", lambda e, i=i, M=M, c0=c0, cw=cw: e.matmul(banks[i][:, 0:cw], lhsT=M, rhs=lf[:, c0:c0 + cw], start=True, stop=True),
                          r=["hg_cst", "hg_lf"], w=[bn[i]])
                    P.add("act", lambda e, i=i, cw=cw: e.activation(out=cb[:, i, :], in_=banks[i][:, 0:cw], func=AF.Copy), r=[bn[i]], w=[f"hg_cb{i}"])
                for hl in range(HG):
                    P.add("pe", lambda e, hl=hl, c0=c0: e.matmul(banks[3][:, hl * 2:hl * 2 + 2], lhsT=lf[:, c0 + hl * 128:c0 + (hl + 1) * 128], rhs=cst[:, 514:516], start=True, stop=True),
                          r=["hg_cst", "hg_lf"], w=[bn[3]])
                P.add("act", lambda e: e.activation(out=dec[:].rearrange("p a b -> p (a b)"), in_=banks[3][:, 0:HG * 2], func=AF.Exp), r=[bn[3]], w=["hg_dec"])
                P.add("dve", lambda e: e.tensor_tensor(out=ta[:], in0=cb[:, 0, :], in1=cb[:, 1, :], op=ALU.subtract), r=["hg_cb0", "hg_cb1"], w=["hg_ta"])
                P.add("dve", lambda e: e.tensor_tensor(out=tb[:], in0=cb[:, 2, :], in1=cb[:, 0, :], op=ALU.subtract), r=["hg_cb2", "hg_cb0"], w=["hg_tb"])
                P.add("act", lambda e: e.activation(out=eq[:], in_=ta[:], func=AF.Exp), r=["hg_ta"], w=["hg_eq"])
                P.add("act", lambda e: e.activation(out=ek[:], in_=ta[:], func=AF.Exp, scale=-1.0), r=["hg_ta"], w=["hg_ek"])
                P.add("act", lambda e: e.activation(out=eb[:], in_=cb[:, 0, :], func=AF.Exp), r=["hg_cb0"], w=["hg_eb"])
                P.add("act", lambda e: e.activation(out=el[:], in_=tb[:], func=AF.Exp), r=["hg_tb"], w=["hg_el"])
                for hl in range(HG):
                    cs = slice(c0 + hl * 128, c0 + (hl + 1) * 128); ls = slice(hl * 128, (hl + 1) * 128)
                    P.add("dve", lambda e, hl=hl, cs=cs, ls=ls: e.tensor_tensor(out=pre[:, hl, 0, :], in0=q[:, cs], in1=eq[:, ls], op=ALU.mult), r=["hg_q", "hg_eq"], w=["hg_pre"])
                    P.add("pool", lambda e, hl=hl, cs=cs, ls=ls: e.tensor_tensor(out=pre[:, hl, 1, :], in0=k[:, cs], in1=ek[:, ls], op=ALU.mult), r=["hg_k", "hg_ek"], w=["hg_pre1"])
                    for xi, X in enumerate(order):
                        P.add("dve", lambda e, hl=hl, cs=cs, ls=ls, xi=xi, X=X: e.scalar_tensor_tensor(out=pre[:, hl, 2 + xi, :], in0=q[:, cs], scalar=cst[:, 512 + X:513 + X], in1=eb[:, ls],
                                                                                                    op0=ALU.mult, op1=ALU.mult), r=["hg_q", "hg_cst", "hg_eb"], w=["hg_pre"])
                        P.add("dve", lambda e, hl=hl, cs=cs, ls=ls, xi=xi, X=X: e.scalar_tensor_tensor(out=ks[:, hl, xi, :], in0=k[:, cs], scalar=cst[:, 512 + X:513 + X], in1=el[:, ls],
                                                                                                    op0=ALU.mult, op1=ALU.mult), r=["hg_k", "hg_cst", "hg_el"], w=["hg_ks"])
                for hl in range(HG):
                    for j in range(4):
                        P.add("pe", lambda e, hl=hl, j=j: e.transpose(banks[hl][:, j * 128:(j + 1) * 128], pre[:, hl, j, :], ident[:]), r=["hg_pre", "hg_pre1", "ident"], w=[bn[hl]])
                    P.add("act", lambda e, hl=hl: e.activation(out=preT[:, hl, :, :].rearrange("p a b -> p (a b)"), in_=banks[hl][:, :], func=AF.Copy), r=[bn[hl]], w=[f"hg_preT{hl}"])
                for hl in range(HG):
                    P.add("pe", lambda e, hl=hl: e.matmul(banks[4][:, hl * 128:(hl + 1) * 128], lhsT=preT[:, hl, 1, :], rhs=preT[:, hl, 0, :], start=True, stop=True),
                          r=[f"hg_preT{hl}"], w=[bn[4]])
                P.add("dve", lambda e: e.tensor_tensor(out=sc[:], in0=banks[4][:, 0:HG * 128].rearrange("p (a b) -> p a b", b=128), in1=CM.unsqueeze(1).to_broadcast([128, HG, 128]), op=ALU.mult),
                      r=[bn[4], "hg_cst"], w=["hg_sc"])
                for hl in range(HG):
                    h = g0 + hl; vs = v[:, h * 128:(h + 1) * 128]; os_ = banks[5][:, hl * 128:(hl + 1) * 128]

                    def mm1(e, hl=hl, h=h, vs=vs, os_=os_):
                        e.matmul(os_, lhsT=sc[:, hl, :], rhs=vs, start=True, stop=False)
                        return e.matmul(os_, lhsT=preT[:, hl, 2, :], rhs=S[:, h, :], start=False, stop=False)
                    P.add("pe", mm1, r=["hg_sc", "hg_v", f"hg_preT{hl}", f"hg_S{h}"], w=[f"hg_o{hl}"])
                    P.add("pe", lambda e, hl=hl, vs=vs: e.matmul(banks[6][:, hl * 128:(hl + 1) * 128], lhsT=ks[:, hl, 0, :], rhs=vs, start=True, stop=True), r=["hg_ks", "hg_v"], w=[f"hg_u{hl}"])
                    P.add("dve", lambda e, hl=hl, h=h: e.scalar_tensor_tensor(out=S[:, h, :], in0=S[:, h, :], scalar=dec[:, hl, order[0]:order[0] + 1], in1=banks[6][:, hl * 128:(hl + 1) * 128],
                                                                         op0=ALU.mult, op1=ALU.add), r=[f"hg_S{h}", "hg_dec", f"hg_u{hl}"], w=[f"hg_S{h}"])
                    P.add("pe", lambda e, hl=hl, h=h, os_=os_: e.matmul(os_, lhsT=preT[:, hl, 3, :], rhs=S[:, h, :], start=False, stop=True), r=[f"hg_preT{hl}", f"hg_S{h}", f"hg_o{hl}"], w=[f"hg_o{hl}"])
                    P.add("pe", lambda e, hl=hl, vs=vs: e.matmul(banks[7][:, hl * 128:(hl + 1) * 128], lhsT=ks[:, hl, 1, :], rhs=vs, start=True, stop=True), r=["hg_ks", "hg_v"], w=[f"hg_w{hl}"])
                    P.add("dve", lambda e, hl=hl, h=h: e.scalar_tensor_tensor(out=S[:, h, :], in0=S[:, h, :], scalar=dec[:, hl, order[1]:order[1] + 1], in1=banks[7][:, hl * 128:(hl + 1) * 128],
                                                                         op0=ALU.mult, op1=ALU.add), r=[f"hg_S{h}", "hg_dec", f"hg_w{hl}"], w=[f"hg_S{h}"])
                    P.add("act", lambda e, hl=hl, h=h, os_=os_: e.activation(out=o[:, h * 128:(h + 1) * 128], in_=os_, func=AF.Copy), r=[f"hg_o{hl}"], w=["hg_oo"])
            P.dma(ydir[d][row0:row0 + 128, :], o[:], r=["hg_oo"], w=["ydirH"], q="pool")


def phase_hgrn_finish(P, ptm, GC0, nw_d, ydir, yout, YC0, NH=6):
    W = NH * 128
    a = P.sb("hf_a", [128, W]); b = P.sb("hf_b", [128, W]); g = P.sb("hf_g", [128, W]); sq = P.sb("hf_sq", [128, W]); nw = P.sb("hf_nw", [128, W])
    st = P.sb("hf_st", [128, NH, 2]); epsb = P.sb("hf_eps", [128, 1])
    P.dma(nw[:], nw_d[:, :], w=["hf_nw"])
    P.add("dve", lambda e: e.memset(epsb[:], EPS), w=["hf_eps"])
    for c in range(NTOK // 128):
        r0 = c * 128
        P.dma(a[:], ydir[0][r0:r0 + 128, :], r=["ydirH"], w=["hf_a"])
        P.dma(b[:], ydir[1][r0:r0 + 128, :], r=["ydirH"], w=["hf_b"])
        P.dma(g[:], ptm[r0:r0 + 128, GC0:GC0 + W], r=["ptm"], w=["hf_g"])
        P.add("dve", lambda e: e.tensor_tensor(out=a[:], in0=a[:], in1=b[:], op=ALU.add), r=["hf_a", "hf_b"], w=["hf_a"])
        P.add("act", lambda e: e.activation(out=sq[:], in_=a[:], func=AF.Square), r=["hf_a"], w=["hf_sq"])
        P.add("dve", lambda e: e.tensor_reduce(out=st[:, :, 0], in_=sq[:].rearrange("p (h e) -> p h e", e=128), op=ALU.add, axis=AX.X), r=["hf_sq"], w=["hf_st"])
        P.add("act", lambda e: e.activation(out=st[:, :, 1], in_=st[:, :, 0], func=AF.Sqrt, scale=1.0 / 128, bias=epsb[:, 0:1]), r=["hf_st", "hf_eps"], w=["hf_st"])
        P.add("dve", lambda e: e.reciprocal(out=st[:, :, 1], in_=st[:, :, 1]), r=["hf_st"], w=["hf_st"])
        P.add("dve", lambda e: e.tensor_tensor(out=a[:].rearrange("p (h e) -> p h e", e=128), in0=a[:].rearrange("p (h e) -> p h e", e=128),
                                               in1=st[:, :, 1:2].to_broadcast([128, NH, 128]), op=ALU.mult), r=["hf_a", "hf_st"], w=["hf_a"])
        P.add("act", lambda e: e.activation(out=g[:], in_=g[:], func=AF.Silu), r=["hf_g"], w=["hf_g"])
        P.add("pool", lambda e: e.tensor_tensor(out=g[:], in0=g[:], in1=nw[:], op=ALU.mult), r=["hf_g", "hf_nw"], w=["hf_g"])
        P.add("dve", lambda e: e.tensor_tensor(out=a[:], in0=a[:], in1=g[:], op=ALU.mult), r=["hf_a", "hf_g"], w=["hf_a"])
        P.dma(yout[r0:r0 + 128, YC0:YC0 + W], a[:], r=["hf_a"], w=["yout"], q="pool")


TWO_PI = 6.283185307179586


def s5_disc(P, prm, out, n, tag):
    r = [tag]
    lre = prm[:, 0, :]; lim = prm[:, 1, :]
    mag = out[:, 0, :]; cs = out[:, 1, :]; sn = out[:, 2, :]; cre = out[:, 3, :]; cim = out[:, 4, :]; t0 = out[:, 5, :]; t1 = out[:, 6, :]; t2 = out[:, 7, :]
    A = lambda eng, fn: P.add(eng, fn, r=r, w=r)
    A("act", lambda e: e.activation(out=t0, in_=prm[:, 2, :], func=AF.Exp))
    A("dve", lambda e: e.tensor_tensor(out=mag, in0=lre, in1=t0, op=ALU.mult))
    A("act", lambda e: e.activation(out=mag, in_=mag, func=AF.Exp))
    A("dve", lambda e: e.scalar_tensor_tensor(out=t0, in0=lim, scalar=1.0 / TWO_PI, in1=t0, op0=ALU.mult, op1=ALU.mult))
    ti = out[:, 7, :].bitcast(mybir.dt.int32)
    A("dve", lambda e: e.tensor_copy(out=ti, in_=t0))
    A("dve", lambda e: e.tensor_copy(out=t1, in_=ti))
    A("dve", lambda e: e.tensor_tensor(out=t0, in0=t0, in1=t1, op=ALU.subtract))
    for (dst, shift) in ((sn, 0.0), (cs, 0.25)):
        A("dve", lambda e, dst=dst, shift=shift: e.tensor_scalar(out=dst, in0=t0, scalar1=shift, scalar2=None, op0=ALU.add))
        for _ in range(2):
            A("dve", lambda e, dst=dst: e.tensor_scalar(out=t1, in0=dst, scalar1=0.5, scalar2=None, op0=ALU.is_gt))
            A("dve", lambda e, dst=dst: e.tensor_tensor(out=dst, in0=dst, in1=t1, op=ALU.subtract))
            A("dve", lambda e, dst=dst: e.tensor_scalar(out=t1, in0=dst, scalar1=-0.5, scalar2=None, op0=ALU.is_lt))
            A("dve", lambda e, dst=dst: e.tensor_tensor(out=dst, in0=dst, in1=t1, op=ALU.add))
        A("act", lambda e, dst=dst: e.activation(out=dst, in_=dst, func=AF.Sin, scale=TWO_PI))
    A("dve", lambda e: e.tensor_tensor(out=t0, in0=mag, in1=cs, op=ALU.mult))
    A("dve", lambda e: e.tensor_scalar(out=t0, in0=t0, scalar1=-1.0, scalar2=None, op0=ALU.add))
    A("dve", lambda e: e.tensor_tensor(out=t1, in0=mag, in1=sn, op=ALU.mult))
    A("dve", lambda e: e.tensor_tensor(out=cre, in0=t0, in1=lre, op=ALU.mult))
    A("dve", lambda e: e.tensor_tensor(out=t2, in0=t1, in1=lim, op=ALU.mult))
    A("dve", lambda e: e.tensor_tensor(out=cre, in0=cre, in1=t2, op=ALU.add))
    A("dve", lambda e: e.tensor_tensor(out=cim, in0=t1, in1=lre, op=ALU.mult))
    A("dve", lambda e: e.tensor_tensor(out=t2, in0=t0, in1=lim, op=ALU.mult))
    A("dve", lambda e: e.tensor_tensor(out=cim, in0=cim, in1=t2, op=ALU.subtract))
    A("dve", lambda e: e.tensor_tensor(out=t0, in0=lre, in1=lre, op=ALU.mult))
    A("dve", lambda e: e.tensor_tensor(out=t1, in0=lim, in1=lim, op=ALU.mult))
    A("dve", lambda e: e.tensor_tensor(out=t0, in0=t0, in1=t1, op=ALU.add))
    A("dve", lambda e: e.reciprocal(out=t0, in_=t0))
    A("dve", lambda e: e.tensor_tensor(out=cre, in0=cre, in1=t0, op=ALU.mult))
    A("dve", lambda e: e.tensor_tensor(out=cim, in0=cim, in1=t0, op=ALU.mult))


def phase_s5(P, ptm, prmR_d, prmC_d, bexp_d, cexp_d, ydir, ident, jmat_d):
    L = 512
    SEG = [(0, NCTX)] + [(NCTX + i * L, L) for i in range((NTOK - NCTX) // L)]
    uT = P.sb("s5_uT", [128, NTOK]); yacc = P.sb("s5_yacc", [128, NTOK])
    prmR = P.sb("s5_prmR", [128, 3, 512]); dR = P.sb("s5_dR", [128, 8, 512]); prmC = P.sb("s5_prmC", [128, 3, 16]); dCs = [P.sb(f"s5_dC{i}", [128, 8, 16]) for i in range(2)]
    bex = P.sb("s5_bex", [128, 2, 512]); BB = P.sb("s5_BB", [128, 2, 512]); cex = P.sb("s5_cex", [128, 2, 128]); tmpb = P.sb("s5_tmpb", [128, 512])
    E = P.sb("s5_E", [128, 2, L]); rot = P.sb("s5_rot", [128, 12, 2]); cc = P.sb("s5_c", [128, 2, L]); zz = P.sb("s5_z", [128, 2, L]); xx = P.sb("s5_x", [128, 2, L])
    t1 = P.sb("s5_t1", [128, L]); carry = P.sb("s5_carry", [128, 6]); um = P.sb("s5_um", [128, 128]); uf = P.sb("s5_uf", [128, 128]); jm = P.sb("s5_J", [128, 128])
    ot = P.sb("s5_ot", [128, 128]); rho = P.sb("s5_rho", [128, L])
    psB = [P.ps(f"s5_psB{i}", [128, 512]) for i in range(2)]; psY = P.ps("s5_psY", [128, 512]); psT = P.ps("s5_psT", [128, 512]); psF = P.ps("s5_psF", [128, 512])
    P.dma(jm[:], jmat_d[:, :], w=["s5_J"])
    for d in range(2):
        P.dma(prmC[:], prmC_d[d], w=["s5_dC"])
        s5_disc(P, prmC, dCs[d], 16, "s5_dC")
    for ut in range(4):
        for d in range(2):
            dC = dCs[d]
            for c in range(NTOK // 128):
                if d == 0:
                    src_c = c
                else:
                    src_c = (NCTX // 128 - 1 - c) if c < NCTX // 128 else (NTOK // 128 - 1 - (c - NCTX // 128))
                P.dma(um[:], ptm[src_c * 128:(src_c + 1) * 128, ut * 128:(ut + 1) * 128], r=["ptm"], w=["s5_um"])
                if d == 1:
                    P.add("pe", lambda e: e.matmul(psF[:, 0:128], lhsT=jm[:], rhs=um[:], start=True, stop=True), r=["s5_J", "s5_um"], w=["s5_psF"])
                    P.add("act", lambda e: e.activation(out=uf[:], in_=psF[:, 0:128], func=AF.Copy), r=["s5_psF"], w=["s5_uf"])
                    srct, sres = uf, "s5_uf"
                else:
                    srct, sres = um, "s5_um"
                P.add("pe", lambda e, srct=srct: e.transpose(psT[:, 0:128], srct[:], ident[:]), r=[sres, "ident"], w=["s5_psT"])
                P.add("act", lambda e, c=c: e.activation(out=uT[:, c * 128:(c + 1) * 128], in_=psT[:, 0:128], func=AF.Copy), r=["s5_psT"], w=["s5_uT"])
            P.dma(prmR[:], prmR_d[d, :, :, ut * 512:(ut + 1) * 512], w=["s5_dR"])
            s5_disc(P, prmR, dR, 512, "s5_dR")
            P.dma(bex[:, 0, :], bexp_d[0, ut], w=["s5_bex"]); P.dma(bex[:, 1, :], bexp_d[1, ut], w=["s5_bex"])
            P.add("dve", lambda e: e.tensor_tensor(out=BB[:, 0, :], in0=bex[:, 0, :], in1=dR[:, 3, :], op=ALU.mult), r=["s5_bex", "s5_dR"], w=["s5_BB"])
            P.add("dve", lambda e: e.tensor_tensor(out=tmpb[:], in0=bex[:, 1, :], in1=dR[:, 4, :], op=ALU.mult), r=["s5_bex", "s5_dR"], w=["s5_tmpb"])
            P.add("dve", lambda e: e.tensor_tensor(out=BB[:, 0, :], in0=BB[:, 0, :], in1=tmpb[:], op=ALU.subtract), r=["s5_BB", "s5_tmpb"], w=["s5_BB"])
            P.add("dve", lambda e: e.tensor_tensor(out=BB[:, 1, :], in0=bex[:, 1, :], in1=dR[:, 3, :], op=ALU.mult), r=["s5_bex", "s5_dR", "s5_BB"], w=["s5_BB"])
            P.add("dve", lambda e: e.tensor_tensor(out=tmpb[:], in0=bex[:, 0, :], in1=dR[:, 4, :], op=ALU.mult), r=["s5_bex", "s5_dR", "s5_BB"], w=["s5_tmpb"])
            P.add("dve", lambda e: e.tensor_tensor(out=BB[:, 1, :], in0=BB[:, 1, :], in1=tmpb[:], op=ALU.add), r=["s5_BB", "s5_tmpb"], w=["s5_BB"])
            P.add("pool", lambda e: e.memset(yacc[:], 0.0), w=["s5_yacc"])
            for pair in range(4):
                tl = ut * 4 + pair
                P.dma(cex[:, 0, :], cexp_d[0, tl], w=["s5_cex"]); P.dma(cex[:, 1, :], cexp_d[1, tl], w=["s5_cex"])
                P.add("dve", lambda e, tl=tl, dC=dC: e.tensor_copy(out=rot[:, 0, 0:1], in_=dC[:, 1, tl:tl + 1]), r=["s5_dC"], w=["s5_rot"])
                P.add("dve", lambda e, tl=tl, dC=dC: e.tensor_copy(out=rot[:, 0, 1:2], in_=dC[:, 2, tl:tl + 1]), r=["s5_dC"], w=["s5_rot"])
                for kq in range(1, 10):
                    P.add("dve", lambda e, kq=kq: e.tensor_tensor(out=rot[:, 10, 0:2], in0=rot[:, kq - 1, 0:2], in1=rot[:, kq - 1, 0:2], op=ALU.mult), r=["s5_rot"], w=["s5_rot"])
                    P.add("dve", lambda e, kq=kq: e.tensor_tensor(out=rot[:, kq, 0:1], in0=rot[:, 10, 0:1], in1=rot[:, 10, 1:2], op=ALU.subtract), r=["s5_rot"], w=["s5_rot"])
                    P.add("dve", lambda e, kq=kq: e.scalar_tensor_tensor(out=rot[:, kq, 1:2], in0=rot[:, kq - 1, 0:1], scalar=2.0, in1=rot[:, kq - 1, 1:2], op0=ALU.mult, op1=ALU.mult), r=["s5_rot"], w=["s5_rot"])
                P.add("dve", lambda e: e.memset(E[:, 0, 0:1], 1.0), r=["s5_rot"], w=["s5_E"])
                P.add("dve", lambda e: e.memset(E[:, 1, 0:1], 0.0), w=["s5_E"])
                n = 1
                kq = 0
                while n < L:
                    P.add("dve", lambda e, n=n, kq=kq: e.tensor_scalar(out=E[:, 0, n:2 * n], in0=E[:, 0, 0:n], scalar1=rot[:, kq, 0:1], scalar2=None, op0=ALU.mult), r=["s5_E", "s5_rot"], w=["s5_E"])
                    P.add("dve", lambda e, n=n, kq=kq: e.scalar_tensor_tensor(out=E[:, 0, n:2 * n], in0=E[:, 1, 0:n], scalar=rot[:, kq, 1:2], in1=E[:, 0, n:2 * n], op0=ALU.mult, op1=ALU.add), r=["s5_E", "s5_rot"], w=["s5_E"])
                    P.add("dve", lambda e, n=n, kq=kq: e.tensor_scalar(out=E[:, 1, n:2 * n], in0=E[:, 1, 0:n], scalar1=rot[:, kq, 0:1], scalar2=None, op0=ALU.mult), r=["s5_E", "s5_rot"], w=["s5_E"])
                    P.add("dve", lambda e, n=n, kq=kq: e.tensor_scalar(out=t1[:, 0:n], in0=E[:, 0, 0:n], scalar1=rot[:, kq, 1:2], scalar2=None, op0=ALU.mult), r=["s5_E", "s5_rot"], w=["s5_t1"])
                    P.add("dve", lambda e, n=n: e.tensor_tensor(out=E[:, 1, n:2 * n], in0=E[:, 1, n:2 * n], in1=t1[:, 0:n], op=ALU.subtract), r=["s5_E", "s5_t1"], w=["s5_E"])
                    n *= 2; kq += 1
                P.add("dve", lambda e, tl=tl, dC=dC: e.tensor_scalar(out=rho[:], in0=E[:, 0, :], scalar1=0.0, scalar2=dC[:, 0, tl:tl + 1], op0=ALU.mult, op1=ALU.add), r=["s5_E", "s5_dC"], w=["s5_rho"])
                P.add("dve", lambda e: e.memset(carry[:], 0.0), w=["s5_carry"])
                for si, (s0, sn) in enumerate(SEG):
                    for ri in range(2):
                        P.add("pe", lambda e, ri=ri, s0=s0, sn=sn, pair=pair: e.matmul(psB[ri][:, 0:sn], lhsT=BB[:, ri, pair * 128:(pair + 1) * 128], rhs=uT[:, s0:s0 + sn], start=True, stop=True),
                              r=["s5_BB", "s5_uT"], w=[f"s5_psB{ri}"])
                    P.add("dve", lambda e, sn=sn: e.tensor_tensor(out=cc[:, 0, 0:sn], in0=psB[0][:, 0:sn], in1=E[:, 0, 0:sn], op=ALU.mult), r=["s5_psB0", "s5_E"], w=["s5_c"])
                    P.add("dve", lambda e, sn=sn: e.tensor_tensor(out=t1[:, 0:sn], in0=psB[1][:, 0:sn], in1=E[:, 1, 0:sn], op=ALU.mult), r=["s5_psB1", "s5_E"], w=["s5_t1"])
                    P.add("dve", lambda e, sn=sn: e.tensor_tensor(out=cc[:, 0, 0:sn], in0=cc[:, 0, 0:sn], in1=t1[:, 0:sn], op=ALU.subtract), r=["s5_c", "s5_t1"], w=["s5_c"])
                    P.add("dve", lambda e, sn=sn: e.tensor_tensor(out=cc[:, 1, 0:sn], in0=psB[1][:, 0:sn], in1=E[:, 0, 0:sn], op=ALU.mult), r=["s5_psB1", "s5_E", "s5_c"], w=["s5_c"])
                    P.add("dve", lambda e, sn=sn: e.tensor_tensor(out=t1[:, 0:sn], in0=psB[0][:, 0:sn], in1=E[:, 1, 0:sn], op=ALU.mult), r=["s5_psB0", "s5_E", "s5_c"], w=["s5_t1"])
                    P.add("dve", lambda e, sn=sn: e.tensor_tensor(out=cc[:, 1, 0:sn], in0=cc[:, 1, 0:sn], in1=t1[:, 0:sn], op=ALU.add), r=["s5_c", "s5_t1"], w=["s5_c"])
                    P.add("dve", lambda e: e.tensor_tensor(out=carry[:, 2:3], in0=carry[:, 0:1], in1=rot[:, 0, 0:1], op=ALU.mult), r=["s5_carry", "s5_rot"], w=["s5_carry"])
                    P.add("dve", lambda e: e.tensor_tensor(out=carry[:, 4:5], in0=carry[:, 1:2], in1=rot[:, 0, 1:2], op=ALU.mult), r=["s5_carry", "s5_rot"], w=["s5_carry"])
                    P.add("dve", lambda e: e.tensor_tensor(out=carry[:, 2:3], in0=carry[:, 2:3], in1=carry[:, 4:5], op=ALU.subtract), r=["s5_carry"], w=["s5_carry"])
                    P.add("dve", lambda e: e.tensor_tensor(out=carry[:, 3:4], in0=carry[:, 1:2], in1=rot[:, 0, 0:1], op=ALU.mult), r=["s5_carry", "s5_rot"], w=["s5_carry"])
                    P.add("dve", lambda e: e.tensor_tensor(out=carry[:, 4:5], in0=carry[:, 0:1], in1=rot[:, 0, 1:2], op=ALU.mult), r=["s5_carry", "s5_rot"], w=["s5_carry"])
                    P.add("dve", lambda e: e.tensor_tensor(out=carry[:, 3:4], in0=carry[:, 3:4], in1=carry[:, 4:5], op=ALU.add), r=["s5_carry"], w=["s5_carry"])
                    P.add("dve", lambda e, sn=sn: e.tensor_tensor_scan(out=zz[:, 0, 0:sn], data0=rho[:, 0:sn], data1=cc[:, 0, 0:sn], initial=carry[:, 2:3], op0=ALU.mult, op1=ALU.add), r=["s5_rho", "s5_c", "s5_carry"], w=["s5_z"])
                    P.add("dve", lambda e, sn=sn: e.tensor_tensor_scan(out=zz[:, 1, 0:sn], data0=rho[:, 0:sn], data1=cc[:, 1, 0:sn], initial=carry[:, 3:4], op0=ALU.mult, op1=ALU.add), r=["s5_rho", "s5_c", "s5_carry", "s5_z"], w=["s5_z"])
                    P.add("dve", lambda e, sn=sn: e.tensor_tensor(out=xx[:, 0, 0:sn], in0=zz[:, 0, 0:sn], in1=E[:, 0, 0:sn], op=ALU.mult), r=["s5_z", "s5_E"], w=["s5_x"])
                    P.add("dve", lambda e, sn=sn: e.tensor_tensor(out=t1[:, 0:sn], in0=zz[:, 1, 0:sn], in1=E[:, 1, 0:sn], op=ALU.mult), r=["s5_z", "s5_E"], w=["s5_t1"])
                    P.add("dve", lambda e, sn=sn: e.tensor_tensor(out=xx[:, 0, 0:sn], in0=xx[:, 0, 0:sn], in1=t1[:, 0:sn], op=ALU.add), r=["s5_x", "s5_t1"], w=["s5_x"])
                    P.add("dve", lambda e, sn=sn: e.tensor_tensor(out=xx[:, 1, 0:sn], in0=zz[:, 1, 0:sn], in1=E[:, 0, 0:sn], op=ALU.mult), r=["s5_z", "s5_E", "s5_x"], w=["s5_x"])
                    P.add("dve", lambda e, sn=sn: e.tensor_tensor(out=t1[:, 0:sn], in0=zz[:, 0, 0:sn], in1=E[:, 1, 0:sn], op=ALU.mult), r=["s5_z", "s5_E", "s5_x"], w=["s5_t1"])
                    P.add("dve", lambda e, sn=sn: e.tensor_tensor(out=xx[:, 1, 0:sn], in0=xx[:, 1, 0:sn], in1=t1[:, 0:sn], op=ALU.subtract), r=["s5_x", "s5_t1"], w=["s5_x"])
                    P.add("dve", lambda e, sn=sn: e.tensor_copy(out=carry[:, 0:1], in_=xx[:, 0, sn - 1:sn]), r=["s5_x"], w=["s5_carry"])
                    P.add("dve", lambda e, sn=sn: e.tensor_copy(out=carry[:, 1:2], in_=xx[:, 1, sn - 1:sn]), r=["s5_x"], w=["s5_carry"])
                    P.add("pool", lambda e, sn=sn: e.tensor_scalar(out=xx[:, 1, 0:sn], in0=xx[:, 1, 0:sn], scalar1=-1.0, scalar2=None, op0=ALU.mult), r=["s5_x", "s5_carry"], w=["s5_x"])

                    def mmy(e, sn=sn):
                        e.matmul(psY[:, 0:sn], lhsT=cex[:, 0, :], rhs=xx[:, 0, 0:sn], start=True, stop=False)
                        return e.matmul(psY[:, 0:sn], lhsT=cex[:, 1, :], rhs=xx[:, 1, 0:sn], start=False, stop=True)
                    P.add("pe", mmy, r=["s5_cex", "s5_x"], w=["s5_psY"])
                    P.add("dve", lambda e, s0=s0, sn=sn: e.tensor_tensor(out=yacc[:, s0:s0 + sn], in0=psY[:, 0:sn], in1=yacc[:, s0:s0 + sn], op=ALU.add), r=["s5_psY", "s5_yacc"], w=["s5_yacc"])
            for c in range(NTOK // 128):
                if d == 0:
                    dst_c = c
                else:
                    dst_c = (NCTX // 128 - 1 - c) if c < NCTX // 128 else (NTOK // 128 - 1 - (c - NCTX // 128))
                P.add("pe", lambda e, c=c: e.transpose(psT[:, 128:256], yacc[:, c * 128:(c + 1) * 128], ident[:]), r=["s5_yacc", "ident"], w=["s5_psT"])
                if d == 1:
                    P.add("act", lambda e: e.activation(out=uf[:], in_=psT[:, 128:256], func=AF.Copy), r=["s5_psT"], w=["s5_uf"])
                    P.add("pe", lambda e: e.matmul(psF[:, 128:256], lhsT=jm[:], rhs=uf[:], start=True, stop=True), r=["s5_J", "s5_uf"], w=["s5_psF"])
                    P.add("act", lambda e: e.activation(out=ot[:], in_=psF[:, 128:256], func=AF.Copy), r=["s5_psF"], w=["s5_ot"])
                else:
                    P.add("act", lambda e: e.activation(out=ot[:], in_=psT[:, 128:256], func=AF.Copy), r=["s5_psT"], w=["s5_ot"])
                P.dma(ydir[d][dst_c * 128:(dst_c + 1) * 128, ut * 128:(ut + 1) * 128], ot[:], r=["s5_ot"], w=["ydirS"], q="pool")


def phase_s5_finish(P, ptm, dvec_d, glu_d, ydir, yout, ident):
    a = P.sb("sf_a", [128, 512]); b = P.sb("sf_b", [128, 512]); u = P.sb("sf_u", [128, 512]); dv = P.sb("sf_d", [128, 512]); t = P.sb("sf_t", [128, 512])
    glu = P.sb("sf_glu", [128, 4, 512]); gT = P.sb("sf_gT", [128, 4, 128]); o = P.sb("sf_o", [128, 256]); sg = P.sb("sf_sg", [128, 256])
    psT = P.ps("sf_psT", [128, 512]); psO = P.ps("sf_psO", [128, 512])
    P.dma(dv[:], dvec_d[:, :], w=["sf_d"]); P.dma(glu[:], glu_d.rearrange("(k p) n -> p k n", p=128), w=["sf_glu"])
    for c in range(NTOK // 128):
        r0 = c * 128
        P.dma(a[:], ydir[0][r0:r0 + 128, :], r=["ydirS"], w=["sf_a"])
        P.dma(b[:], ydir[1][r0:r0 + 128, :], r=["ydirS"], w=["sf_b"])
        P.dma(u[:], ptm[r0:r0 + 128, 0:512], r=["ptm"], w=["sf_u"])
        P.add("dve", lambda e: e.tensor_tensor(out=a[:], in0=a[:], in1=b[:], op=ALU.add), r=["sf_a", "sf_b"], w=["sf_a"])
        P.add("pool", lambda e: e.tensor_tensor(out=u[:], in0=u[:], in1=dv[:], op=ALU.mult), r=["sf_u", "sf_d"], w=["sf_u"])
        P.add("dve", lambda e: e.tensor_tensor(out=a[:], in0=a[:], in1=u[:], op=ALU.add), r=["sf_a", "sf_u"], w=["sf_a"])
        P.add("dve", lambda e: e.tensor_tensor(out=t[:], in0=a[:], in1=a[:], op=ALU.mult), r=["sf_a"], w=["sf_t"])
        P.add("dve", lambda e: e.tensor_scalar(out=t[:], in0=t[:], scalar1=0.044715, scalar2=1.0, op0=ALU.mult, op1=ALU.add), r=["sf_t"], w=["sf_t"])
        P.add("dve", lambda e: e.tensor_tensor(out=t[:], in0=t[:], in1=a[:], op=ALU.mult), r=["sf_t", "sf_a"], w=["sf_t"])
        P.add("act", lambda e: e.activation(out=t[:], in_=t[:], func=AF.Tanh, scale=0.7978845608028654), r=["sf_t"], w=["sf_t"])
        P.add("dve", lambda e: e.tensor_scalar(out=t[:], in0=t[:], scalar1=1.0, scalar2=0.5, op0=ALU.add, op1=ALU.mult), r=["sf_t"], w=["sf_t"])
        P.add("dve", lambda e: e.tensor_tensor(out=t[:], in0=t[:], in1=a[:], op=ALU.mult), r=["sf_t", "sf_a"], w=["sf_t"])
        for k in range(4):
            P.add("pe", lambda e, k=k: e.transpose(psT[:, k * 128:(k + 1) * 128], t[:, k * 128:(k + 1) * 128], ident[:]), r=["sf_t", "ident"], w=["sf_psT"])
        P.add("act", lambda e: e.activation(out=gT[:].rearrange("p a b -> p (a b)"), in_=psT[:, :], func=AF.Copy), r=["sf_psT"], w=["sf_gT"])

        def mm(e):
            last = None
            for k in range(4):
                last = e.matmul(psO[:, :], lhsT=gT[:, k, :], rhs=glu[:, k, :], start=(k == 0), stop=(k == 3))
            return last
        P.add("pe", mm, r=["sf_gT", "sf_glu"], w=["sf_psO"])
        P.add("act", lambda e: e.activation(out=sg[:], in_=psO[:, 256:512], func=AF.Sigmoid), r=["sf_psO"], w=["sf_sg"])
        P.add("dve", lambda e: e.tensor_tensor(out=o[:], in0=psO[:, 0:256], in1=sg[:], op=ALU.mult), r=["sf_psO", "sf_sg"], w=["sf_o"])
        P.dma(yout[r0:r0 + 128, 0:256], o[:], r=["sf_o"], w=["yout"], q="pool")


def build_la1(do_s5=True, do_hg=True, dbg=False):
    nc = bass.Bass("TRN2", target_bir_lowering=False)
    P = Prog(nc)
    P.use_arena(36000)
    ei = lambda name, shape: nc.dram_tensor(name, list(shape), F32, kind="ExternalInput").ap()
    NC_ALL = 4352
    xT = ei("xT", [D, NTOK]); W = ei("W", [D, NC_ALL]); vecs = ei("vecs", [128, 5, KT]); ident_d = ei("ident", [128, 128])
    if do_hg:
        lbl = ei("lbl", [128, 2, 768]); hcst = ei("hcst", [2, 128, 516]); hnw = ei("hnw", [128, 768])
    if do_s5:
        prmR = ei("prmR", [2, 128, 3, 2048]); prmC = ei("prmC", [2, 128, 3, 16]); bexp = ei("bexp", [2, 4, 128, 512]); cexp = ei("cexp", [2, 16, 128, 128])
        jmat = ei("jmat", [128, 128]); dvec = ei("dvec", [128, 512]); glu = ei("glu", [512, 512])
    yout = nc.dram_tensor("yout", [NTOK, 1024], BF16, kind="ExternalOutput").ap()
    ptm = nc.dram_tensor("ptm", [NTOK, NC_ALL], F32, kind="ExternalOutput" if dbg else "Internal").ap()
    ydirH = nc.dram_tensor("ydirH", [2, NTOK, 768], F32).ap()
    ydirS = nc.dram_tensor("ydirS", [2, NTOK, 512], F32, kind="ExternalOutput" if dbg else "Internal").ap()
    vec = P.sb("vec", [128, 5, KT]); weff = P.sb("weff", [128, 2, KT]); ones = P.sb("ones", [128, 128]); epsb = P.sb("epsb", [128, 1])
    ident = P.sb("ident_s", [128, 128])
    P.persist()
    P.dma(vec[:], vecs[:, :, :], w=["vec"]); P.dma(ident[:], ident_d[:, :], w=["ident"])
    P.add("dve", lambda e: e.memset(ones[:], 1.0), w=["ones"])
    P.add("dve", lambda e: e.memset(epsb[:], EPS), w=["epsb"])
    for c in range(2):
        P.add("dve", lambda e, c=c: e.scalar_tensor_tensor(out=weff[:, c, :], in0=vec[:, c * 2 + 1, :], scalar=1.0, in1=vec[:, 4, :], op0=ALU.add, op1=ALU.mult),
              r=["vec"], w=["weff"])
    phase_inproj(P, xT, W, NC_ALL, ptm, vec, weff, ones, epsb)
    if do_hg:
        P.phase()
        phase_hgrn(P, ptm, 512, lbl, hcst, ydirH, ident)
        P.phase()
        phase_hgrn_finish(P, ptm, 512 + 4 * 768, hnw, ydirH, yout, 256)
    if do_s5:
        P.phase()
        phase_s5(P, ptm, prmR, prmC, bexp, cexp, ydirS, ident, jmat)
        P.phase()
        phase_s5_finish(P, ptm, dvec, glu, ydirS, yout, ident)
    return P.emit()


A_SPL = (1024, 1024, 1024, 64, 160, 64, 64)
def fm16(v): return np.ascontiguousarray(v.reshape(16, 128).T)
def head_order(hh):
    return [hh * 8 + 2 * hp + hs for hs in range(2) for hp in range(4)]
def a_cols(hh):
    ho = head_order(hh)
    ch = np.concatenate([np.arange(h * 64, (h + 1) * 64) for h in ho])
    r = ch; k = 1024 + ch; v = 2048 + ch
    a_lo = 3072 + np.arange(64); g_lo = 3136 + np.arange(160); wf = 3296 + np.arange(64); wb = 3360 + np.arange(64)
    pad = lambda n: -np.ones(n, np.int64)
    return np.concatenate([r, k, v, a_lo, wf, wb, pad(64), g_lo, pad(96)]), ch
def b_cols(hh):
    h0 = hh * 4
    q = 3424 + np.arange(h0 * 64, (h0 + 4) * 64); k = 3424 + 512 + np.arange(h0 * 64, (h0 + 4) * 64)
    v = 3424 + 1024 + np.arange(h0 * 128, (h0 + 4) * 128); g = 3424 + 2048 + np.arange(h0 * 128, (h0 + 4) * 128)
    return np.concatenate([q, k, v, g])
def take_cols(W, idx):
    out = np.zeros((W.shape[0], len(idx)), W.dtype)
    m = idx >= 0
    out[:, m] = W[:, idx[m]]
    return out
def rope_table():
    GRID_W = 64; half = 32; nf = 16
    t = np.arange(8192)
    row = (t // GRID_W).astype(np.float32); col = (t % GRID_W).astype(np.float32)
    inv = (10000.0 ** (-np.arange(nf, dtype=np.float32) / nf)).astype(np.float32)
    ang = np.concatenate([row[:, None] * inv, col[:, None] * inv], -1)
    tab = np.concatenate([np.cos(ang), np.sin(ang)], -1).astype(np.float32)
    ctx = np.concatenate([np.ones((256, 32), np.float32), np.zeros((256, 32), np.float32)], -1)
    return np.concatenate([ctx, tab], 0)
def ret_cst():
    i = np.arange(128)
    E1 = np.maximum(i[None, :] - i[:, None], 0).astype(np.float32)
    M1 = (i[:, None] <= i[None, :]).astype(np.float32)
    c = np.stack([i + 1.0, 127.0 - i, np.full(128, 128.0)], 1).astype(np.float32)
    f = np.concatenate([E1, M1, c], 1)
    cb = np.stack([128.0 - i, i + 0.0, np.full(128, 128.0)], 1).astype(np.float32)
    bwd = np.concatenate([E1.T, M1.T, cb], 1)
    return np.ascontiguousarray(np.stack([f, bwd], 0))
def shift_mask():
    m = np.zeros((8448, 4), np.float32)
    t = np.arange(256)
    m[:256, 0] = (t > 0); m[:256, 2] = (t > 0); m[:256, 1] = (t < 255); m[:256, 3] = (t < 255)
    t = np.arange(8192); col = t % 64; row = t // 64
    m[256:, 0] = (col > 0); m[256:, 1] = (col < 63); m[256:, 2] = (row > 0); m[256:, 3] = (row < 127)
    return m
def rwkv_consts(hh, inp):
    ac, ch = a_cols(hh)
    mu = np.zeros(2048, np.float32); mk = ac >= 0; mu[mk] = inp["rwkv_mu"][0][ac[mk]]
    mu_b = np.ascontiguousarray(np.tile(mu[None], (128, 1)))
    vs = [inp["rwkv_a0"][0], inp["rwkv_w0"][0, 0], inp["rwkv_w0"][0, 1], inp["rwkv_k_k"][0], inp["rwkv_k_a"][0],
          inp["rwkv_r_k"][0].reshape(-1), inp["rwkv_ln_w"][0], inp["rwkv_ln_b"][0]]
    cv = np.stack([np.tile(v[ch][None], (128, 1)) for v in vs], 1).astype(np.float32)
    lw = np.zeros((128, 5, 512), np.float32)
    lw[0:64, 0] = inp["rwkv_a2"][0][:, ch]; lw[64:128, 1] = inp["rwkv_w2"][0, 0][:, ch]; lw[0:64, 2] = inp["rwkv_w2"][0, 1][:, ch]
    lw[:, 3] = inp["rwkv_g2"][0][0:128][:, ch]; lw[0:32, 4] = inp["rwkv_g2"][0][128:160][:, ch]
    return mu_b, np.ascontiguousarray(cv), lw


def cd_cols(hh):
    u = np.arange(512)
    hc = np.arange(hh * 768, (hh + 1) * 768)
    return np.concatenate([u] + [512 + j * 1536 + hc for j in range(5)])
def hgrn_cst():
    i = np.arange(128); ch = i // 64; same = ch[:, None] == ch[None, :]
    out = np.zeros((2, 128, 516), np.float32)
    mid = ch * 64 + 32
    for d in range(2):
        if d == 0:
            TRI = same & (i[:, None] <= i[None, :]); MID = same & (i[:, None] <= mid[None, :])
        else:
            TRI = same & (i[:, None] >= i[None, :]); MID = same & (i[:, None] >= mid[None, :])
        out[d, :, 0:128] = TRI; out[d, :, 128:256] = MID; out[d, :, 256:384] = same; out[d, :, 384:512] = TRI
        out[d, :, 512] = (ch == 0); out[d, :, 513] = (ch == 1); out[d, :, 514] = (ch == 0); out[d, :, 515] = (ch == 1)
    return out
def s5_consts(inp, hh):
    lre = inp["s5_lambda_re"][0]; lim = inp["s5_lambda_im"][0]; ldt = inp["s5_log_dt"][0]
    prmR = np.zeros((2, 128, 3, 2048), np.float32); prmC = np.zeros((2, 128, 3, 16), np.float32)
    for d in range(2):
        flat = np.stack([lre[d].reshape(-1), lim[d].reshape(-1), np.repeat(ldt[d], 64)], 0)
        prmR[d] = flat[None]
        prmC[d] = flat.reshape(3, 16, 128).transpose(2, 0, 1)
    bre = inp["s5_b_re"][0]; bim = inp["s5_b_im"][0]
    bexp = np.zeros((2, 4, 128, 512), np.float32)
    for ut in range(4):
        for gl in range(8):
            g = ut * 8 + gl; pair = gl // 2; g2 = gl % 2
            c0 = pair * 128 + g2 * 64
            bexp[0, ut, gl * 16:(gl + 1) * 16, c0:c0 + 64] = bre[g].T
            bexp[1, ut, gl * 16:(gl + 1) * 16, c0:c0 + 64] = bim[g].T
    cre = inp["s5_c_re"][0]; cim = inp["s5_c_im"][0]
    cexp = np.zeros((2, 16, 128, 128), np.float32)
    for tl in range(16):
        for g2 in range(2):
            g = tl * 2 + g2; gl = g % 8
            cexp[0, tl, g2 * 64:(g2 + 1) * 64, gl * 16:(gl + 1) * 16] = cre[g].T
            cexp[1, tl, g2 * 64:(g2 + 1) * 64, gl * 16:(gl + 1) * 16] = cim[g].T
    dvec = np.ascontiguousarray(np.tile(inp["s5_d"][0][None], (128, 1)))
    gw = inp["s5_glu_w"][0]
    glu = np.ascontiguousarray(np.concatenate([gw[:, hh * 256:(hh + 1) * 256], gw[:, 512 + hh * 256:512 + (hh + 1) * 256]], 1))
    return dict(prmR=prmR, prmC=prmC, bexp=bexp, cexp=cexp, dvec=dvec, glu=glu, jmat=np.ascontiguousarray(np.eye(128, dtype=np.float32)[::-1]))
def la1_inputs(b, hh, x, xc, m, inp):
    xT = np.ascontiguousarray(np.concatenate([xc[b], x[b]], 0).T)
    W = take_cols(inp["cd_w_in"][0], cd_cols(hh))
    vecs = np.zeros((128, 5, 16), np.float32)
    ml = m[b].reshape(6, 2048); mc = m[4].reshape(6, 2048)
    vecs[:, 0] = fm16(ml[0]); vecs[:, 1] = fm16(ml[1]); vecs[:, 2] = fm16(mc[0]); vecs[:, 3] = fm16(mc[1]); vecs[:, 4] = fm16(inp["norm_w"][1, 0])
    hc = np.arange(hh * 768, (hh + 1) * 768)
    lbl = np.ascontiguousarray(np.tile(inp["hgrn_lb_logits"][:, hc][None], (128, 1, 1)).astype(np.float32))
    hnw = np.ascontiguousarray(np.tile(inp["hgrn_norm_w"][0][hc][None], (128, 1)).astype(np.float32))
    d = dict(xT=xT, W=np.ascontiguousarray(W), vecs=vecs, ident=np.eye(128, dtype=np.float32), lbl=lbl, hcst=hgrn_cst(), hnw=hnw)
    d.update(s5_consts(inp, hh))
    return d


def build_lm(NCOL=3072):
    nc = bass.Bass("TRN2", target_bir_lowering=False)
    P = Prog(nc)
    ei = lambda name, shape: nc.dram_tensor(name, list(shape), F32, kind="ExternalInput").ap()
    ccT = ei("ccT", [D, 128]); Wm = ei("Wm", [D, NCOL]); bm = ei("bm", [128, NCOL])
    out = nc.dram_tensor("m", [128, NCOL], F32, kind="ExternalOutput").ap()
    cs = P.sb("cs", [128, KT, 128]); bs = P.sb("bs", [128, NCOL]); ob = P.sb("ob", [128, NCOL])
    ws = [(P.sb(f"lm_w{i}", [128, KT, 256]), f"lm_w{i}") for i in range(2)]
    ps = [(P.ps(f"lm_ps{i}", [128, 512]), f"lm_ps{i}") for i in range(2)]
    P.dma(cs[:], ccT.rearrange("(k p) r -> p k r", p=128), w=["cs"]); P.dma(bs[:], bm[:, :], w=["bs"])
    P.add("act", lambda e: e.activation(out=cs[:], in_=cs[:], func=AF.Silu), r=["cs"], w=["cs"])
    Wv = Wm.rearrange("(k p) n -> p k n", p=128)
    for ci, c0 in enumerate(range(0, NCOL, 256)):
        st, sres = ws[ci % 2]; pt, pres = ps[ci % 2]
        P.dma(st[:], Wv[:, :, c0:c0 + 256], w=[sres])

        def mm(e, st=st, pt=pt):
            last = None
            for k in range(KT):
                last = e.matmul(pt[:, 0:256], lhsT=cs[:, k, :], rhs=st[:, k, :], start=(k == 0), stop=(k == KT - 1))
            return last
        P.add("pe", mm, r=[sres, "cs"], w=[pres])
        P.add("dve", lambda e, pt=pt, c0=c0: e.tensor_tensor(out=ob[:, c0:c0 + 256], in0=pt[:, 0:256], in1=bs[:, c0:c0 + 256], op=ALU.add), r=[pres, "bs"], w=["ob"])
    P.dma(out[:, :], ob[:], r=["ob"], w=["out"])
    return P.emit()


def _run(nc, in_maps):
    res = run_bass_kernel_spmd(nc, in_maps, core_ids=list(range(len(in_maps))))
    return res.results


def la0_inputs(b, hh, x, ctx, m, inp):
    xT = np.ascontiguousarray(np.concatenate([ctx[b], x[b]], 0).T)
    ac, ch = a_cols(hh)
    W = np.concatenate([take_cols(inp["ab_w_in"][0], ac), take_cols(inp["ab_w_in"][0], b_cols(hh))], 1)
    vecs = np.zeros((128, 5, 16), np.float32)
    ml = m[b].reshape(6, 2048); mc = m[4].reshape(6, 2048)
    vecs[:, 0] = fm16(ml[0]); vecs[:, 1] = fm16(ml[1]); vecs[:, 2] = fm16(mc[0]); vecs[:, 3] = fm16(mc[1]); vecs[:, 4] = fm16(inp["norm_w"][0, 0])
    rde = np.tile(inp["ret_decay_exp"][0][:, hh * 4:(hh + 1) * 4].reshape(1, 8), (128, 1)).astype(np.float32)
    mu_b, cv, lw = rwkv_consts(hh, inp)
    return dict(xT=xT, W=np.ascontiguousarray(W), vecs=vecs, rope=rope_table(), rde=np.ascontiguousarray(rde), rcst=ret_cst(), ident=np.eye(128, dtype=np.float32),
                shmask=shift_mask(), mu=mu_b, cv=cv, lw=lw)


def bcd_inputs(L, b, xs, ys, m, inp, last):
    vecs = np.zeros((128, 12, 16), np.float32)
    for c, row in enumerate([b, 4]):
        mm = m[row].reshape(6, 2048)
        for j, idx in enumerate([2, 3, 4, 5]):
            vecs[:, c * 4 + j, :] = fm16(mm[idx])
    vecs[:, 8, :] = fm16(inp["norm_w"][L, 1]); vecs[:, 9, :] = fm16(inp["final_norm_w"])
    wr = np.concatenate([inp["moe_wr_coarse"][L], inp["moe_wr_fine"][L]], 1)
    br = np.tile(np.concatenate([inp["moe_br_coarse"][L], inp["moe_br_fine"][L]])[None], (128, 1)).astype(np.float32)
    onehot = np.zeros((128, 32, 128), np.float32)
    for e in range(32):
        onehot[e, e, :] = 1
    return dict(xT=np.ascontiguousarray(xs.T), yT=np.ascontiguousarray(ys.T), wout=np.ascontiguousarray(inp["w_out"][L]), vecs=vecs, wr=np.ascontiguousarray(wr),
                br=np.ascontiguousarray(br), wgu=np.ascontiguousarray(inp["moe_w_gu"][L]), wdn=np.ascontiguousarray(inp["moe_w_down"][L]),
                ident=np.eye(128, dtype=np.float32), onehot=onehot.reshape(128, -1))


def kernel(**inp):
    inp = {k: np.asarray(v) for k, v in inp.items()}
    x = inp["x"]; ctx = inp["ctx"]
    B = x.shape[0]
    cc = np.zeros((128, 2048), np.float32); cc[0:4] = inp["c"]; cc[4] = inp["c_ctx"]
    ccT = np.ascontiguousarray(cc.T)
    mw = np.concatenate([inp["mod_w"][0], inp["mod_w"][1]], 1)
    mb = np.concatenate([inp["mod_b"][0], inp["mod_b"][1]], 0)
    ims = []
    for c in range(8):
        sl = slice(c * 3072, (c + 1) * 3072)
        ims.append(dict(ccT=ccT, Wm=np.ascontiguousarray(mw[:, sl]), bm=np.ascontiguousarray(np.tile(mb[sl][None], (128, 1)))))
    res = _run(build_lm(), ims)
    mall = np.concatenate([r["m"][0:5] for r in res], 1)
    ms = [mall[:, 0:12288], mall[:, 12288:]]
    res = _run(build_la0(), [la0_inputs(c // 2, c % 2, x, ctx, ms[0], inp) for c in range(8)])
    y0 = np.zeros((B, NTOK, 2048), ml_dtypes.bfloat16)
    for c in range(8):
        b, hh = c // 2, c % 2
        ac, ch = a_cols(hh)
        yo = res[c]["yout"]
        y0[b][:, ch] = yo[:, 0:512]
        y0[b][:, 1024 + hh * 512:1024 + (hh + 1) * 512] = yo[:, 512:1024]
    tiles0 = [(i * 384, 384, 0) for i in range(10)] + [(3840, 256, 0), (4096, 128, 1)]
    ims = []
    for c in range(8):
        b, sh = c // 2, c % 2
        xs = np.concatenate([x[b, sh * 4096:(sh + 1) * 4096], ctx[b, sh * 128:(sh + 1) * 128]], 0)
        ys = np.concatenate([y0[b, NCTX + sh * 4096:NCTX + (sh + 1) * 4096], y0[b, sh * 128:(sh + 1) * 128]], 0)
        ims.append(bcd_inputs(0, b, xs, ys, ms[0], inp, False))
    res = _run(build_bcd(tiles0, 4224, False), ims)
    x1 = np.zeros_like(x); xc1 = np.zeros_like(ctx)
    for c in range(8):
        b, sh = c // 2, c % 2
        o = res[c]["out"].T
        x1[b, sh * 4096:(sh + 1) * 4096] = o[0:4096]; xc1[b, sh * 128:(sh + 1) * 128] = o[4096:4224]
    res = _run(build_la1(), [la1_inputs(c // 2, c % 2, x1, xc1, ms[1], inp) for c in range(8)])
    y1 = np.zeros((B, 8192, 2048), ml_dtypes.bfloat16)
    for c in range(8):
        b, hh = c // 2, c % 2
        yo = res[c]["yout"][NCTX:]
        y1[b][:, hh * 256:(hh + 1) * 256] = yo[:, 0:256]
        y1[b][:, 512 + hh * 768:512 + (hh + 1) * 768] = yo[:, 256:1024]
    tiles1 = [(i * 384, 384, 0) for i in range(10)] + [(3840, 256, 0)]
    ims = []
    for c in range(8):
        b, sh = c // 2, c % 2
        ims.append(bcd_inputs(1, b, x1[b, sh * 4096:(sh + 1) * 4096], y1[b, sh * 4096:(sh + 1) * 4096], ms[1], inp, True))
    res = _run(build_bcd(tiles1, 4096, True), ims)
    out = np.zeros(x.shape, np.float32)
    for c in range(8):
        b, sh = c // 2, c % 2
        out[b, sh * 4096:(sh + 1) * 4096] = res[c]["out"].T
    return out
```

```python
import ml_dtypes
import contextlib
import numpy as np
import concourse.bass as bass
import concourse.mybir as mybir
from concourse.bass_utils import run_bass_kernel_spmd

F32 = mybir.dt.float32
BF16 = mybir.dt.bfloat16
U32 = mybir.dt.uint32
AF = mybir.ActivationFunctionType
ALU = mybir.AluOpType
AX = mybir.AxisListType

EPOCH = 20000
NDMA_SLOTS = 6


class Prog:
    ENGS = ("pe", "act", "dve", "pool", "sp")

    def __init__(self, nc):
        self.nc = nc
        self.ops = []
        self.last_w = {}
        self.readers = {}
        self.stack = contextlib.ExitStack()
        self.arena = None
        self.arena_words = 0
        self.bump = 0
        self.base = 0
        self.psbanks = None
        self.psnext = 0

    def use_arena(self, words):
        self.arena = self.stack.enter_context(self.nc.sbuf_tensor("arena", [128, words], F32))
        self.arena_words = words
        self.psbanks = [self.stack.enter_context(self.nc.psum_tensor(f"bank{i}", [128, 512], F32)) for i in range(8)]

    def sb(self, name, shape, dt=F32):
        if self.arena is None:
            return self.stack.enter_context(self.nc.sbuf_tensor(name, list(shape), dt))
        assert dt == F32 and shape[0] <= 128
        n = int(np.prod(shape[1:]))
        assert self.bump + n <= self.arena_words, (name, self.bump, n)
        v = self.arena[0:shape[0], self.bump:self.bump + n]
        self.bump += n
        if len(shape) == 3:
            v = v.rearrange("p (a b) -> p a b", b=shape[2])
        elif len(shape) == 4:
            v = v.rearrange("p (a b c) -> p a b c", b=shape[2], c=shape[3])
        return v

    def ps(self, name, shape, dt=F32):
        if self.psbanks is None:
            return self.stack.enter_context(self.nc.psum_tensor(name, list(shape), dt))
        assert self.psnext < 8, name
        t = self.psbanks[self.psnext]
        self.psnext += 1
        return t

    def persist(self):
        self.base = self.bump

    def phase(self):
        last = {}
        recent_dma = {}
        for i, op in enumerate(self.ops):
            if op["dma"]:
                recent_dma.setdefault(op["eng"], []).append(i)
            elif op["fn"] is not None:
                last[op["eng"]] = i
        deps = set(last.values())
        for q, lst in recent_dma.items():
            deps.update(lst[-NDMA_SLOTS:])
        for e in self.ENGS:
            self.ops.append(dict(eng=e, fn=None, deps=set(deps), dma=False, marked=False))
        self.bump = self.base
        self.psnext = 0

    def dram(self, name, shape, dt=F32, kind="Internal"):
        return self.nc.dram_tensor(name, list(shape), dt, kind=kind).ap()

    def add(self, eng, fn, r=(), w=(), dma=False):
        deps = set()
        for x in r:
            if x in self.last_w:
                deps.add(self.last_w[x])
        for x in w:
            if x in self.last_w:
                deps.add(self.last_w[x])
            deps.update(self.readers.get(x, ()))
        idx = len(self.ops)
        self.ops.append(dict(eng=eng, fn=fn, deps=deps, dma=dma, marked=False))
        for x in r:
            self.readers.setdefault(x, []).append(idx)
        for x in w:
            self.last_w[x] = idx
            self.readers[x] = []
        return idx

    def dma(self, out, in_, r=(), w=(), q="sp"):
        return self.add(q, lambda e: e.dma_start(out=out, in_=in_), r, w, dma=True)

    def emit(self):
        nc = self.nc
        ops = self.ops
        for op in ops:
            for d in op["deps"]:
                ops[d]["marked"] = True
        cnt = {e: 0 for e in self.ENGS}
        dcnt = {e: 0 for e in self.ENGS}
        sem_names = set()
        for op in ops:
            e = op["eng"]
            if op["dma"]:
                slot = dcnt[e] % NDMA_SLOTS
                use = dcnt[e] // NDMA_SLOTS + 1
                dcnt[e] += 1
                nm = f"d_{e}_{slot}"
                op["sem"] = (nm, 16 * use)
                op["prev"] = (nm, 16 * (use - 1)) if use > 1 else None
                sem_names.add(nm)
            elif op["marked"]:
                cnt[e] += 1
                ep = (cnt[e] - 1) // EPOCH
                nm = f"c_{e}_{ep}"
                op["sem"] = (nm, cnt[e] - ep * EPOCH)
                sem_names.add(nm)
        sems = {nm: self.stack.enter_context(nc.semaphore(nm)) for nm in sorted(sem_names)}
        final_dma = {}
        for op in ops:
            if op["dma"]:
                nm, v = op["sem"]
                final_dma[nm] = max(final_dma.get(nm, 0), v)
        block = self.stack.enter_context(nc.Block())

        def run(eng_name, e):
            waited = {}

            def wait(nm, v):
                if waited.get(nm, 0) < v:
                    e.wait_ge(sems[nm], v)
                    waited[nm] = v

            for op in ops:
                if op["eng"] != eng_name:
                    continue
                for d in sorted(op["deps"]):
                    nm, v = ops[d]["sem"]
                    wait(nm, v)
                if op["dma"]:
                    if op["prev"] is not None:
                        wait(*op["prev"])
                    inst = op["fn"](e)
                    inst.then_inc(sems[op["sem"][0]], 16)
                elif op["fn"] is not None:
                    inst = op["fn"](e)
                    if op["marked"]:
                        inst.then_inc(sems[op["sem"][0]], 1)
            if eng_name == "sp":
                for nm, v in sorted(final_dma.items()):
                    wait(nm, v)

        @block.tensor
        def _(e):
            run("pe", e)

        @block.scalar
        def _(e):
            run("act", e)

        @block.vector
        def _(e):
            run("dve", e)

        @block.gpsimd
        def _(e):
            run("pool", e)

        @block.sync
        def _(e):
            run("sp", e)

        self.stack.close()
        return nc


D = 2048
KT = 16
NE = 32
DE = 512
EPS = 1e-6


def mm_stream_fm(P, W, K, N, rhs_fn, rhs_res, n, evac, tag, stage, ps_list, NC=256, psw="psA"):
    kt_n = K // 128
    Wv = W.rearrange("(k p) n -> p k n", p=128)
    for ci, n0 in enumerate(range(0, N, NC)):
        st, sres = stage[ci % 2]
        P.dma(st[:, 0:kt_n, 0:NC], Wv[:, :, n0:n0 + NC], w=[sres])
        for j in range(NC // 128):
            ct = (n0 // 128) + j
            pi = ct % len(ps_list)
            pst = ps_list[pi]
            pres = f"{psw}{pi}"

            def mm(e, st=st, j=j, pst=pst):
                last = None
                for k in range(kt_n):
                    last = e.matmul(pst[:, 0:n], lhsT=st[:, k, j * 128:(j + 1) * 128], rhs=rhs_fn(k),
                                    start=(k == 0), stop=(k == kt_n - 1))
                return last

            P.add("pe", mm, r=[sres] + list(rhs_res), w=[pres])
            evac(ct, pst, pres)


def build_bcd(tiles, NT, last, TT=384):
    nc = bass.Bass("TRN2", target_bir_lowering=False)
    P = Prog(nc)
    ei = lambda name, shape: nc.dram_tensor(name, list(shape), F32, kind="ExternalInput").ap()
    xT = ei("xT", [D, NT]); yT = nc.dram_tensor("yT", [D, NT], BF16, kind="ExternalInput").ap(); wout = ei("wout", [D, D])
    vecs = ei("vecs", [128, 12, KT])
    wr = ei("wr", [D, 36]); br = ei("br", [128, 36])
    wgu = ei("wgu", [NE, D, 2 * DE]); wdn = ei("wdn", [NE, DE, D])
    ident_d = ei("ident", [128, 128]); onehot_d = ei("onehot", [128, 32 * 128])
    out = nc.dram_tensor("out", [D, NT], F32, kind="ExternalOutput").ap()


    xt = P.sb("xt", [128, KT, TT]); h2 = P.sb("h2", [128, KT, TT]); acc = P.sb("acc", [128, KT, TT])
    sg = P.sb("sg", [128, 4, TT]); act = P.sb("act", [128, 4, TT]); gb = P.sb("gb", [128, TT])
    sgt = P.sb("sgt", [128, TT])
    wg = [(P.sb(f"wg{i}", [128, KT, 256]), f"wg{i}") for i in range(2)]
    wd = [(P.sb(f"wd{i}", [128, 4, 1024]), f"wd{i}") for i in range(2)]
    vec = P.sb("vec", [128, 12, KT]); weff = P.sb("weff", [128, 2, KT])
    wrs = P.sb("wrs", [128, KT, 36]); brs = P.sb("brs", [128, 36])
    ident = P.sb("ident_s", [128, 128]); onehot = P.sb("onehot_s", [128, 32 * 128])
    ones = P.sb("ones", [128, 128]); epsb = P.sb("epsb", [128, 1])
    rstd = P.sb("rstd", [128, TT])
    lg = P.sb("lg", [128, 36]); sm = P.sb("sm", [128, 64]); gd = P.sb("gd", [128, 128]); gT = P.sb("gT", [128, TT])
    psA = [P.ps(f"psA{i}", [128, 512]) for i in range(3)]
    psB = [P.ps(f"psB{i}", [128, 512]) for i in range(3)]
    psS = P.ps("psS", [128, 512]); psR = P.ps("psR", [128, 512])

    P.dma(vec[:], vecs[:, :, :], w=["vec"])
    P.dma(wrs[:], wr.rearrange("(k p) n -> p k n", p=128), w=["wrs"])
    P.dma(brs[:], br[:, :], w=["brs"])
    P.dma(ident[:], ident_d[:, :], w=["ident"])
    P.dma(onehot[:], onehot_d[:, :], w=["onehot"])
    P.add("dve", lambda e: e.memset(ones[:], 1.0), w=["ones"])
    P.add("dve", lambda e: e.memset(epsb[:], EPS), w=["epsb"])
    P.add("dve", lambda e: e.memset(gd[:], 0.0), w=["gd"])
    for c in range(2):
        P.add("dve", lambda e, c=c: e.scalar_tensor_tensor(out=weff[:, c, :], in0=vec[:, c * 4 + 2, :], scalar=1.0,
                                                          in1=vec[:, 8, :], op0=ALU.add, op1=ALU.mult),
              r=["vec"], w=[f"weff{c}"])

    def rms(src, n, srcres):
        P.add("act", lambda e: e.activation(out=h2[:, :, 0:n], in_=src[:, :, 0:n], func=AF.Square), r=[srcres], w=["h2"])

        def mm(e):
            last = None
            for k in range(KT):
                last = e.matmul(psS[:, 0:n], lhsT=ones[:], rhs=h2[:, k, 0:n], start=(k == 0), stop=(k == KT - 1))
            return last
        P.add("pe", mm, r=["h2", "ones"], w=["psS"])
        P.add("act", lambda e: e.activation(out=rstd[:, 0:n], in_=psS[:, 0:n], func=AF.Sqrt, scale=1.0 / D, bias=epsb[:, 0:1]),
              r=["psS", "epsb"], w=["rstd"])
        P.add("dve", lambda e: e.reciprocal(out=rstd[:, 0:n], in_=rstd[:, 0:n]), r=["rstd"], w=["rstd"])

    for (t0, n, cls) in tiles:
        xv = xT.rearrange("(k p) t -> p k t", p=128)
        yv = yT.rearrange("(k p) t -> p k t", p=128)
        P.dma(xt[:, :, 0:n], xv[:, :, t0:t0 + n], w=["xt"])
        P.dma(h2[:, :, 0:n], yv[:, :, t0:t0 + n], w=["h2"], q="pool")

        def ev1(ct, pst, pres, n=n, cls=cls):
            P.add("dve", lambda e: e.scalar_tensor_tensor(out=xt[:, ct, 0:n], in0=pst[:, 0:n], scalar=vec[:, cls * 4 + 0, ct:ct + 1],
                                                          in1=xt[:, ct, 0:n], op0=ALU.mult, op1=ALU.add),
                  r=[pres, "vec", "xt"], w=["xt"])
        mm_stream_fm(P, wout, D, D, lambda k, n=n: h2[:, k, 0:n], ["h2"], n, ev1, "wo", wg, psA)
        rms(xt, n, "xt")
        for k in range(KT):
            P.add("dve", lambda e, k=k, n=n: e.tensor_tensor(out=h2[:, k, 0:n], in0=xt[:, k, 0:n], in1=rstd[:, 0:n], op=ALU.mult),
                  r=["xt", "rstd"], w=["h2"])
        for k in range(KT):
            P.add("pool", lambda e, k=k, n=n, cls=cls: e.tensor_scalar(out=h2[:, k, 0:n], in0=h2[:, k, 0:n], scalar1=weff[:, cls, k:k + 1],
                                                                   scalar2=vec[:, cls * 4 + 1, k:k + 1], op0=ALU.mult, op1=ALU.add),
                  r=["h2", f"weff{cls}", "vec"], w=["h2"])
        for s in range(n // 128):
            def rmm(e, s=s):
                last = None
                for k in range(KT):
                    last = e.matmul(psR[:, 0:36], lhsT=h2[:, k, s * 128:(s + 1) * 128], rhs=wrs[:, k, :], start=(k == 0), stop=(k == KT - 1))
                return last
            P.add("pe", rmm, r=["h2", "wrs"], w=["psR"])
            P.add("dve", lambda e: e.tensor_tensor(out=lg[:], in0=psR[:, 0:36], in1=brs[:], op=ALU.add), r=["psR", "brs"], w=["lg"])
            P.add("dve", lambda e: e.tensor_reduce(out=sm[:, 0:1], in_=lg[:, 0:4], op=ALU.max, axis=AX.X), r=["lg"], w=["sm"])
            P.add("dve", lambda e: e.tensor_scalar(out=sm[:, 1:2], in0=sm[:, 0:1], scalar1=-1.0, scalar2=None, op0=ALU.mult), r=["sm"], w=["sm"])
            P.add("act", lambda e: e.activation(out=sm[:, 48:52], in_=lg[:, 0:4], func=AF.Exp, bias=sm[:, 1:2], scale=1.0, accum_out=sm[:, 2:3]),
                  r=["lg", "sm"], w=["sm"])
            P.add("dve", lambda e: e.reciprocal(out=sm[:, 3:4], in_=sm[:, 2:3]), r=["sm"], w=["sm"])
            P.add("dve", lambda e: e.tensor_scalar(out=sm[:, 4:8], in0=lg[:, 0:4], scalar1=sm[:, 0:1], scalar2=None, op0=ALU.is_equal), r=["lg", "sm"], w=["sm"])
            P.add("dve", lambda e: e.tensor_scalar(out=sm[:, 8:16], in0=lg[:, 4:12], scalar1=sm[:, 4:5], scalar2=None, op0=ALU.mult), r=["lg", "sm"], w=["sm"])
            for g in range(1, 4):
                P.add("dve", lambda e, g=g: e.scalar_tensor_tensor(out=sm[:, 8:16], in0=lg[:, 4 + 8 * g:12 + 8 * g], scalar=sm[:, 4 + g:5 + g],
                                                                  in1=sm[:, 8:16], op0=ALU.mult, op1=ALU.add), r=["lg", "sm"], w=["sm"])
            P.add("dve", lambda e: e.max(out=sm[:, 16:24], in_=sm[:, 8:16]), r=["sm"], w=["sm"])
            P.add("dve", lambda e: e.tensor_scalar(out=sm[:, 24:25], in0=sm[:, 16:17], scalar1=-1.0, scalar2=None, op0=ALU.mult), r=["sm"], w=["sm"])
            P.add("act", lambda e: e.activation(out=sm[:, 25:33], in_=sm[:, 8:16], func=AF.Exp, bias=sm[:, 24:25], scale=1.0), r=["sm"], w=["sm"])
            P.add("act", lambda e: e.activation(out=sm[:, 33:35], in_=sm[:, 16:18], func=AF.Exp, bias=sm[:, 24:25], scale=1.0, accum_out=sm[:, 35:36]),
                  r=["sm"], w=["sm"])
            P.add("dve", lambda e: e.reciprocal(out=sm[:, 36:37], in_=sm[:, 35:36]), r=["sm"], w=["sm"])
            P.add("dve", lambda e: e.tensor_tensor(out=sm[:, 36:37], in0=sm[:, 36:37], in1=sm[:, 3:4], op=ALU.mult), r=["sm"], w=["sm"])
            P.add("dve", lambda e: e.tensor_scalar(out=sm[:, 40:48], in0=sm[:, 8:16], scalar1=sm[:, 17:18], scalar2=None, op0=ALU.is_ge), r=["sm"], w=["sm"])
            P.add("dve", lambda e: e.scalar_tensor_tensor(out=sm[:, 40:48], in0=sm[:, 25:33], scalar=sm[:, 36:37], in1=sm[:, 40:48],
                                                          op0=ALU.mult, op1=ALU.mult), r=["sm"], w=["sm"])
            for g in range(4):
                P.add("dve", lambda e, g=g: e.tensor_scalar(out=gd[:, 8 * g:8 * g + 8], in0=sm[:, 40:48], scalar1=sm[:, 4 + g:5 + g], scalar2=None, op0=ALU.mult),
                      r=["sm"], w=["gd"])
            P.add("pe", lambda e: e.transpose(psR[:, 128:256], gd[:, :], ident[:]), r=["gd", "ident"], w=["psR"])
            P.add("act", lambda e, s=s: e.activation(out=gT[:, s * 128:(s + 1) * 128], in_=psR[:, 128:256], func=AF.Copy), r=["psR"], w=["gT"])

        for ex in range(NE):
            P.add("pe", lambda e, ex=ex, n=n: e.matmul(psS[:, 0:n], lhsT=onehot[:, ex * 128:(ex + 1) * 128], rhs=gT[:, 0:n], start=True, stop=True),
                  r=["onehot", "gT"], w=["psS"])
            P.add("act", lambda e, n=n: e.activation(out=gb[:, 0:n], in_=psS[:, 0:n], func=AF.Copy), r=["psS"], w=["gb"])

            def ev_gu(ct, pst, pres, n=n):
                if ct < 4:
                    P.add("act", lambda e: e.activation(out=sgt[:, 0:n], in_=pst[:, 0:n], func=AF.Silu), r=[pres], w=["sgt"])
                    P.add("dve", lambda e: e.tensor_tensor(out=sg[:, ct, 0:n], in0=sgt[:, 0:n], in1=gb[:, 0:n], op=ALU.mult), r=["sgt", "gb"], w=[f"sg{ct}"])
                else:
                    f = ct - 4
                    P.add("dve", lambda e: e.tensor_tensor(out=act[:, f, 0:n], in0=pst[:, 0:n], in1=sg[:, f, 0:n], op=ALU.mult),
                          r=[pres, f"sg{f}"], w=[f"act{f}"])
            mm_stream_fm(P, wgu[ex], D, 2 * DE, lambda k, n=n: h2[:, k, 0:n], ["h2"], n, ev_gu, "gu", wg, psA)

            def ev_dn(ct, pst, pres, n=n, ex=ex):
                if ex == 0:
                    P.add("dve", lambda e: e.tensor_copy(out=acc[:, ct, 0:n], in_=pst[:, 0:n]), r=[pres], w=[f"acc{ct}"])
                else:
                    P.add("dve", lambda e: e.tensor_tensor(out=acc[:, ct, 0:n], in0=pst[:, 0:n], in1=acc[:, ct, 0:n], op=ALU.add),
                          r=[pres, f"acc{ct}"], w=[f"acc{ct}"])
            mm_stream_fm(P, wdn[ex], DE, D, lambda k, n=n: act[:, k, 0:n], [f"act{f}" for f in range(4)], n, ev_dn, "dn", wd, psB, NC=1024, psw="psB")

        for k in range(KT):
            P.add("dve", lambda e, k=k, n=n, cls=cls: e.scalar_tensor_tensor(out=acc[:, k, 0:n], in0=acc[:, k, 0:n], scalar=vec[:, cls * 4 + 3, k:k + 1],
                                                                          in1=xt[:, k, 0:n], op0=ALU.mult, op1=ALU.add),
                  r=[f"acc{k}", "vec", "xt"], w=[f"acc{k}", "accall"])
        if last:
            rms(acc, n, "accall")
            for k in range(KT):
                P.add("dve", lambda e, k=k, n=n: e.tensor_tensor(out=acc[:, k, 0:n], in0=acc[:, k, 0:n], in1=rstd[:, 0:n], op=ALU.mult),
                      r=["accall", "rstd", f"acc{k}"], w=[f"acc{k}"])
                P.add("pool", lambda e, k=k, n=n: e.tensor_scalar(out=acc[:, k, 0:n], in0=acc[:, k, 0:n], scalar1=vec[:, 9, k:k + 1], scalar2=None, op0=ALU.mult),
                      r=[f"acc{k}", "vec"], w=[f"acc{k}", "accall"])
        ov = out.rearrange("(k p) t -> p k t", p=128)
        P.dma(ov[:, :, t0:t0 + n], acc[:, :, 0:n], r=["accall"] + [f"acc{k}" for k in range(KT)], w=["out"])
    return P.emit()


D = 2048
KT = 16
EPS = 1e-6
NTOK = 8448
NCTX = 256


def dram_ap(t, offset, pat):
    return bass.AP(t.tensor, offset, pat)


def phase_inproj(P, xT, W, NC_ALL, ptm, vec, weff, ones, epsb, TT=384):
    xt = P.sb("ip_xt", [128, KT, TT]); hh = P.sb("ip_h", [128, KT, TT]); rstd = P.sb("ip_rstd", [128, TT])
    ws = [(P.sb(f"ip_w{i}", [128, KT, 256]), f"ip_w{i}") for i in range(2)]
    ot = [(P.sb(f"ip_o{i}", [128, 256]), f"ip_o{i}") for i in range(3)]
    psS = P.ps("ip_psS", [128, 512])
    psO = [(P.ps(f"ip_psO{i}", [128, 512]), f"ip_psO{i}") for i in range(3)]
    tiles = [(0, NCTX, 1)]
    t = NCTX
    while t < NTOK:
        n = min(TT, NTOK - t)
        tiles.append((t, n, 0)); t += n
    xv = xT.rearrange("(k p) t -> p k t", p=128)
    Wv = W.rearrange("(k p) n -> p k n", p=128)
    cnt = 0
    for (t0, n, cls) in tiles:
        P.dma(xt[:, :, 0:n], xv[:, :, t0:t0 + n], w=["ip_xt"])
        P.add("act", lambda e, n=n: e.activation(out=hh[:, :, 0:n], in_=xt[:, :, 0:n], func=AF.Square), r=["ip_xt"], w=["ip_h"])

        def mm(e, n=n):
            last = None
            for k in range(KT):
                last = e.matmul(psS[:, 0:n], lhsT=ones[:], rhs=hh[:, k, 0:n], start=(k == 0), stop=(k == KT - 1))
            return last
        P.add("pe", mm, r=["ip_h", "ones"], w=["ip_psS"])
        P.add("act", lambda e, n=n: e.activation(out=rstd[:, 0:n], in_=psS[:, 0:n], func=AF.Sqrt, scale=1.0 / D, bias=epsb[:, 0:1]),
              r=["ip_psS", "epsb"], w=["ip_rstd"])
        P.add("dve", lambda e, n=n: e.reciprocal(out=rstd[:, 0:n], in_=rstd[:, 0:n]), r=["ip_rstd"], w=["ip_rstd"])
        for k in range(KT):
            P.add("dve", lambda e, k=k, n=n: e.tensor_tensor(out=hh[:, k, 0:n], in0=xt[:, k, 0:n], in1=rstd[:, 0:n], op=ALU.mult),
                  r=["ip_xt", "ip_rstd"], w=["ip_h"])
        for k in range(KT):
            P.add("pool", lambda e, k=k, n=n, cls=cls: e.tensor_scalar(out=hh[:, k, 0:n], in0=hh[:, k, 0:n], scalar1=weff[:, cls, k:k + 1],
                                                                   scalar2=vec[:, cls * 2 + 0, k:k + 1], op0=ALU.mult, op1=ALU.add),
                  r=["ip_h", "weff", "vec"], w=["ip_h"])
        for ci, c0 in enumerate(range(0, NC_ALL, 256)):
            st, sres = ws[ci % 2]
            P.dma(st[:, :, :], Wv[:, :, c0:c0 + 256], w=[sres])
            for s in range(n // 128):
                ps, pres = psO[cnt % 3]
                o, ores = ot[cnt % 3]
                cnt += 1

                def mm2(e, st=st, s=s, ps=ps):
                    last = None
                    for k in range(KT):
                        last = e.matmul(ps[:, 0:256], lhsT=hh[:, k, s * 128:(s + 1) * 128], rhs=st[:, k, :], start=(k == 0), stop=(k == KT - 1))
                    return last
                P.add("pe", mm2, r=[sres, "ip_h"], w=[pres])
                P.add("act", lambda e, ps=ps, o=o: e.activation(out=o[:], in_=ps[:, 0:256], func=AF.Copy), r=[pres], w=[ores])
                P.dma(ptm[t0 + s * 128:t0 + (s + 1) * 128, c0:c0 + 256], o[:], r=[ores], w=["ptm"], q="pool")


def chunk_order(direction):
    if direction == 0:
        return [(c * 128, False) for c in range(NTOK // 128)]
    ctx = [(c * 128, True) for c in reversed(range(NCTX // 128))]
    lat = [(c * 128, True) for c in reversed(range(NCTX // 128, NTOK // 128))]
    return ctx + lat


def rows_ap(dr, row0, rev, c0, ncols):
    width = dr.shape[1]
    if not rev:
        return dr[row0:row0 + 128, c0:c0 + ncols]
    return dram_ap(dr, (row0 + 127) * width + c0, [[-width, 128], [1, ncols]])


def phase_retention(P, ptm, BC0, rope, rde_d, cst_d, ydir, ident, NH=4):
    qk = P.sb("rt_qk", [128, 2 * NH * 64]); v = P.sb("rt_v", [128, NH * 128]); rp = P.sb("rt_rope", [128, 64])
    qr = P.sb("rt_qr", [128, 2 * NH * 64]); tmp = P.sb("rt_tmp", [128, NH * 64])
    qs = P.sb("rt_qs", [128, NH, 128]); km = P.sb("rt_km", [128, NH, 128]); ks = P.sb("rt_ks", [128, NH, 128])
    qT = P.sb("rt_qT", [128, NH // 2, 128]); qsT = P.sb("rt_qsT", [128, NH, 128]); kmT = P.sb("rt_kmT", [128, NH, 128])
    sc = P.sb("rt_sc", [128, NH, 128]); S = P.sb("rt_S", [128, NH, 128]); o = P.sb("rt_o", [128, NH * 128])
    dm = P.sb("rt_dm", [128, NH, 128]); gt = P.sb("rt_gt", [128, 3 * NH])
    psT = P.ps("rt_psT", [128, 512]); psC = P.ps("rt_psC", [128, 512]); psO = P.ps("rt_psO", [128, 512]); psU = P.ps("rt_psU", [128, 512])
    for t_, nm_ in ((qs, "rt_qs"), (km, "rt_km"), (ks, "rt_ks")):
        P.add("dve", lambda e, t_=t_: e.memset(t_[:], 0.0), w=[nm_])
    rde = P.sb("rt_rde", [128, 2 * NH]); cst = P.sb("rt_cst", [128, 259]); lgt = P.sb("rt_lg", [128, 2 * NH])
    P.dma(rde[:], rde_d[:, :], w=["rt_rde"])
    P.add("act", lambda e: e.activation(out=lgt[:], in_=rde[:], func=AF.Exp, scale=0.6931471805599453), r=["rt_rde"], w=["rt_lg"])
    P.add("dve", lambda e: e.tensor_scalar(out=lgt[:], in0=lgt[:], scalar1=-1.0, scalar2=1.0, op0=ALU.mult, op1=ALU.add), r=["rt_lg"], w=["rt_lg"])
    P.add("act", lambda e: e.activation(out=lgt[:], in_=lgt[:], func=AF.Ln), r=["rt_lg"], w=["rt_lg"])
    for d in range(2):
        P.dma(cst[:], cst_d[d], w=["rt_cst"])
        for h in range(NH):
            li = d * NH + h
            P.add("act", lambda e, h=h, li=li: e.activation(out=dm[:, h, :], in_=cst[:, 0:128], func=AF.Exp, scale=lgt[:, li:li + 1]), r=["rt_cst", "rt_lg"], w=["rt_dm"])
            P.add("dve", lambda e, h=h: e.tensor_tensor(out=dm[:, h, :], in0=dm[:, h, :], in1=cst[:, 128:256], op=ALU.mult), r=["rt_dm", "rt_cst"], w=["rt_dm"])
            for j in range(3):
                P.add("act", lambda e, h=h, li=li, j=j: e.activation(out=gt[:, j * NH + h:j * NH + h + 1], in_=cst[:, 256 + j:257 + j], func=AF.Exp, scale=lgt[:, li:li + 1]),
                      r=["rt_cst", "rt_lg"], w=["rt_gt"])
        P.add("dve", lambda e: e.memset(S[:], 0.0), w=["rt_S"])
        for (row0, rev) in chunk_order(d):
            P.dma(qk[:], rows_ap(ptm, row0, False, BC0, 2 * NH * 64), w=["rt_qk"])
            P.dma(v[:], rows_ap(ptm, row0, False, BC0 + 2 * NH * 64, NH * 128), w=["rt_v"])
            P.dma(rp[:], rows_ap(rope, row0, False, 0, 64), w=["rt_rope"])
            x = qk[:].rearrange("p (h two f) -> p h two f", two=2, f=32)
            xo = qr[:].rearrange("p (h two f) -> p h two f", two=2, f=32)
            tv = tmp[:].rearrange("p (h f) -> p h f", f=32)
            cosb = rp[:, 0:32].unsqueeze(1).to_broadcast([128, 2 * NH, 32])
            sinb = rp[:, 32:64].unsqueeze(1).to_broadcast([128, 2 * NH, 32])
            P.add("dve", lambda e, x=x, xo=xo, cosb=cosb: e.tensor_tensor(out=xo[:, :, 0, :], in0=x[:, :, 0, :], in1=cosb, op=ALU.mult), r=["rt_qk", "rt_rope"], w=["rt_qr"])
            P.add("dve", lambda e, x=x, tv=tv, sinb=sinb: e.tensor_tensor(out=tv, in0=x[:, :, 1, :], in1=sinb, op=ALU.mult), r=["rt_qk", "rt_rope"], w=["rt_tmp"])
            P.add("dve", lambda e, xo=xo, tv=tv: e.tensor_tensor(out=xo[:, :, 0, :], in0=xo[:, :, 0, :], in1=tv, op=ALU.subtract), r=["rt_qr", "rt_tmp"], w=["rt_qr"])
            P.add("dve", lambda e, x=x, xo=xo, cosb=cosb: e.tensor_tensor(out=xo[:, :, 1, :], in0=x[:, :, 1, :], in1=cosb, op=ALU.mult), r=["rt_qk", "rt_rope", "rt_qr"], w=["rt_qr"])
            P.add("dve", lambda e, x=x, tv=tv, sinb=sinb: e.tensor_tensor(out=tv, in0=x[:, :, 0, :], in1=sinb, op=ALU.mult), r=["rt_qk", "rt_rope", "rt_qr"], w=["rt_tmp"])
            P.add("dve", lambda e, xo=xo, tv=tv: e.tensor_tensor(out=xo[:, :, 1, :], in0=xo[:, :, 1, :], in1=tv, op=ALU.add), r=["rt_qr", "rt_tmp"], w=["rt_qr"])
            for h in range(NH):
                off = (h % 2) * 64
                qh = qr[:, h * 64:(h + 1) * 64]; kh = qr[:, NH * 64 + h * 64:NH * 64 + (h + 1) * 64]
                P.add("dve", lambda e, h=h, off=off, qh=qh: e.tensor_scalar(out=qs[:, h, off:off + 64], in0=qh, scalar1=gt[:, h:h + 1], scalar2=None, op0=ALU.mult),
                      r=["rt_qr", "rt_gt"], w=["rt_qs"])
                P.add("pool", lambda e, h=h, off=off, kh=kh: e.tensor_scalar(out=km[:, h, off:off + 64], in0=kh, scalar1=0.125, scalar2=None, op0=ALU.mult),
                      r=["rt_qr"], w=["rt_km"])
                P.add("dve", lambda e, h=h, off=off, kh=kh: e.tensor_scalar(out=ks[:, h, off:off + 64], in0=kh, scalar1=gt[:, NH + h:NH + h + 1], scalar2=0.125, op0=ALU.mult, op1=ALU.mult),
                      r=["rt_qr", "rt_gt"], w=["rt_ks"])
            for hp in range(NH // 2):
                P.add("pe", lambda e, hp=hp: e.transpose(psT[:, hp * 128:(hp + 1) * 128], qr[:, hp * 128:(hp + 1) * 128], ident[:]), r=["rt_qr", "ident"], w=["rt_psT"])
            P.add("act", lambda e: e.activation(out=qT[:].rearrange("p a b -> p (a b)"), in_=psT[:, 0:NH // 2 * 128], func=AF.Copy), r=["rt_psT"], w=["rt_qT"])
            for h in range(NH):
                P.add("pe", lambda e, h=h: e.transpose(psT[:, h * 128:(h + 1) * 128], qs[:, h, :], ident[:]), r=["rt_qs", "ident"], w=["rt_psT"])
            P.add("act", lambda e: e.activation(out=qsT[:].rearrange("p a b -> p (a b)"), in_=psT[:, 0:NH * 128], func=AF.Copy), r=["rt_psT"], w=["rt_qsT"])
            for h in range(NH):
                P.add("pe", lambda e, h=h: e.transpose(psT[:, h * 128:(h + 1) * 128], km[:, h, :], ident[:]), r=["rt_km", "ident"], w=["rt_psT"])
            P.add("act", lambda e: e.activation(out=kmT[:].rearrange("p a b -> p (a b)"), in_=psT[:, 0:NH * 128], func=AF.Copy), r=["rt_psT"], w=["rt_kmT"])
            for h in range(NH):
                P.add("pe", lambda e, h=h: e.matmul(psC[:, h * 128:(h + 1) * 128], lhsT=kmT[:, h, :], rhs=qT[:, h // 2, :], start=True, stop=True),
                      r=["rt_kmT", "rt_qT"], w=["rt_psC"])
            P.add("dve", lambda e: e.tensor_tensor(out=sc[:].rearrange("p a b -> p (a b)"), in0=psC[:, 0:NH * 128], in1=dm[:].rearrange("p a b -> p (a b)"), op=ALU.mult),
                  r=["rt_psC", "rt_dm"], w=["rt_sc"])
            for h in range(NH):
                def mmo(e, h=h):
                    e.matmul(psO[:, h * 128:(h + 1) * 128], lhsT=sc[:, h, :], rhs=v[:, h * 128:(h + 1) * 128], start=True, stop=False)
                    return e.matmul(psO[:, h * 128:(h + 1) * 128], lhsT=qsT[:, h, :], rhs=S[:, h, :], start=False, stop=True)
                P.add("pe", mmo, r=["rt_sc", "rt_v", "rt_qsT", "rt_S"], w=["rt_psO"])
            P.add("act", lambda e: e.activation(out=o[:], in_=psO[:, 0:NH * 128], func=AF.Copy), r=["rt_psO"], w=["rt_o"])
            P.dma(ydir[d][row0:row0 + 128, :], o[:], r=["rt_o"], w=["ydir"], q="pool")
            for h in range(NH):
                P.add("pe", lambda e, h=h: e.matmul(psU[:, h * 128:(h + 1) * 128], lhsT=ks[:, h, :], rhs=v[:, h * 128:(h + 1) * 128], start=True, stop=True),
                      r=["rt_ks", "rt_v"], w=["rt_psU"])
            for h in range(NH):
                P.add("dve", lambda e, h=h: e.scalar_tensor_tensor(out=S[:, h, :], in0=S[:, h, :], scalar=gt[:, 2 * NH + h:2 * NH + h + 1], in1=psU[:, h * 128:(h + 1) * 128],
                                                                  op0=ALU.mult, op1=ALU.add), r=["rt_S", "rt_gt", "rt_psU"], w=["rt_S"])


def phase_ret_finish(P, ptm, GC0, ydir, yout, YC0, NH=4):
    a = P.sb("rf_a", [128, NH * 128]); b = P.sb("rf_b", [128, NH * 128]); g = P.sb("rf_g", [128, NH * 128])
    st = P.sb("rf_st", [128, NH, 8]); sq = P.sb("rf_sq", [128, NH * 128]); epsb = P.sb("rf_eps", [128, 1])
    P.add("dve", lambda e: e.memset(epsb[:], EPS), w=["rf_eps"])
    for c in range(NTOK // 128):
        r0 = c * 128
        P.dma(a[:], ydir[0][r0:r0 + 128, :], r=["ydir"], w=["rf_a"])
        P.dma(b[:], ydir[1][r0:r0 + 128, :], r=["ydir"], w=["rf_b"])
        P.dma(g[:], ptm[r0:r0 + 128, GC0:GC0 + NH * 128], r=["ptm"], w=["rf_g"])
        P.add("dve", lambda e: e.tensor_tensor(out=a[:], in0=a[:], in1=b[:], op=ALU.add), r=["rf_a", "rf_b"], w=["rf_a"])
        av = a[:].rearrange("p (h e) -> p h e", e=128)
        P.add("dve", lambda e, av=av: e.tensor_reduce(out=st[:, :, 0], in_=av, op=ALU.add, axis=AX.X), r=["rf_a"], w=["rf_st"])
        P.add("dve", lambda e: e.tensor_scalar(out=st[:, :, 1], in0=st[:, :, 0], scalar1=-1.0 / 128, scalar2=None, op0=ALU.mult), r=["rf_st"], w=["rf_st"])
        P.add("dve", lambda e, av=av: e.tensor_tensor(out=av, in0=av, in1=st[:, :, 1:2].to_broadcast([128, NH, 128]), op=ALU.add), r=["rf_a", "rf_st"], w=["rf_a"])
        P.add("act", lambda e: e.activation(out=sq[:], in_=a[:], func=AF.Square), r=["rf_a"], w=["rf_sq"])
        P.add("dve", lambda e: e.tensor_reduce(out=st[:, :, 2], in_=sq[:].rearrange("p (h e) -> p h e", e=128), op=ALU.add, axis=AX.X), r=["rf_sq"], w=["rf_st"])
        P.add("act", lambda e: e.activation(out=st[:, :, 3], in_=st[:, :, 2], func=AF.Sqrt, scale=1.0 / 128, bias=epsb[:, 0:1]), r=["rf_st", "rf_eps"], w=["rf_st"])
        P.add("dve", lambda e: e.reciprocal(out=st[:, :, 3], in_=st[:, :, 3]), r=["rf_st"], w=["rf_st"])
        P.add("dve", lambda e, av=av: e.tensor_tensor(out=av, in0=av, in1=st[:, :, 3:4].to_broadcast([128, NH, 128]), op=ALU.mult), r=["rf_a", "rf_st"], w=["rf_a"])
        P.add("act", lambda e: e.activation(out=g[:], in_=g[:], func=AF.Silu), r=["rf_g"], w=["rf_g"])
        P.add("dve", lambda e: e.tensor_tensor(out=a[:], in0=a[:], in1=g[:], op=ALU.mult), r=["rf_a", "rf_g"], w=["rf_a"])
        P.dma(yout[r0:r0 + 128, YC0:YC0 + NH * 128], a[:], r=["rf_a"], w=["yout"], q="pool")


NEG_E05 = -0.6065306597126334
GN_EPS = 64e-5


def phase_rwkv_prep(P, ptm, shmask, mu_d, cv_d, lw_d, Rd, Fd, VTd, ident):
    z = P.sb("rp_z", [128, 2048]); zs = [P.sb(f"rp_zs{j}", [128, 2048]) for j in range(4)]
    zz = P.sb("rp_zz", [128, 2048]); dd = P.sb("rp_d", [128, 512]); mk = P.sb("rp_mk", [128, 4])
    mu = P.sb("rp_mu", [128, 2048]); cv = P.sb("rp_cv", [128, 8, 512]); lw = P.sb("rp_lw", [128, 5, 512])
    lT = P.sb("rp_lT", [128, 4, 128]); Rt = P.sb("rp_R", [128, 6, 512]); Ft = P.sb("rp_F", [128, 2, 512])
    a = P.sb("rp_a", [128, 512]); t1 = P.sb("rp_t1", [128, 512]); kkr = P.sb("rp_kkr", [128, 512]); st = P.sb("rp_st", [128, 16])
    vp = P.sb("rp_vp", [128, 512]); vT = P.sb("rp_vT", [128, 512]); e12 = P.sb("rp_e12", [128, 1])
    psT = P.ps("rp_psT", [128, 512]); psA = P.ps("rp_psA", [128, 512]); psF = P.ps("rp_psF", [128, 512]); psB = P.ps("rp_psB", [128, 512])
    psG = P.ps("rp_psG", [128, 512]); psV = P.ps("rp_psV", [128, 512])
    P.dma(mu[:], mu_d[:, :], w=["rp_mu"]); P.dma(cv[:], cv_d[:, :, :], w=["rp_cv"]); P.dma(lw[:], lw_d[:, :, :], w=["rp_lw"])
    P.add("dve", lambda e: e.memset(e12[:], 1e-12), w=["rp_e12"])
    for j in range(4):
        P.add("pool", lambda e, j=j: e.memset(zs[j][:], 0.0), w=[f"rp_zs{j}"])
    for c in range(NTOK // 128):
        r0 = c * 128
        offs = (-1, 1, -1, 1) if r0 < NCTX else (-1, 1, -64, 64)
        P.dma(z[:], ptm[r0:r0 + 128, 0:2048], r=["ptm"], w=["rp_z"])
        P.dma(mk[:], shmask[r0:r0 + 128, :], w=["rp_mk"])
        for j in range(4):
            lo = max(r0 + offs[j], 0); hi = min(r0 + offs[j] + 128, NTOK)
            p0 = lo - (r0 + offs[j])
            P.dma(zs[j][p0:p0 + (hi - lo), :], ptm[lo:hi, 0:2048], r=["ptm"], w=[f"rp_zs{j}"])
        zv = z[:].rearrange("p (c four) -> p c four", four=4)
        zzv = zz[:].rearrange("p (c four) -> p c four", four=4)
        muv = mu[:].rearrange("p (c four) -> p c four", four=4)
        for j in range(4):
            zsv = zs[j][:].rearrange("p (c four) -> p c four", four=4)
            P.add("dve", lambda e, j=j, zsv=zsv, zv=zv: e.scalar_tensor_tensor(out=dd[:], in0=zsv[:, :, j], scalar=mk[:, j:j + 1], in1=zv[:, :, j], op0=ALU.mult, op1=ALU.subtract),
                  r=[f"rp_zs{j}", "rp_mk", "rp_z"], w=["rp_d"])
            P.add("dve", lambda e, j=j, muv=muv: e.tensor_tensor(out=dd[:], in0=dd[:], in1=muv[:, :, j], op=ALU.mult), r=["rp_d", "rp_mu"], w=["rp_d"])
            P.add("dve", lambda e, j=j, zv=zv, zzv=zzv: e.tensor_tensor(out=zzv[:, :, j], in0=dd[:], in1=zv[:, :, j], op=ALU.add), r=["rp_d", "rp_z"], w=["rp_zz"])
        P.add("act", lambda e: e.activation(out=zz[:, 1600:1728], in_=zz[:, 1600:1728], func=AF.Tanh), r=["rp_zz"], w=["rp_zz"])
        P.add("act", lambda e: e.activation(out=zz[:, 1792:1952], in_=zz[:, 1792:1952], func=AF.Sigmoid), r=["rp_zz"], w=["rp_zz"])
        for i in range(4):
            P.add("pe", lambda e, i=i: e.transpose(psT[:, i * 128:(i + 1) * 128], zz[:, 1536 + i * 128:1536 + (i + 1) * 128], ident[:]), r=["rp_zz", "ident"], w=["rp_psT"])
        P.add("act", lambda e: e.activation(out=lT[:].rearrange("p a b -> p (a b)"), in_=psT[:, :], func=AF.Copy), r=["rp_psT"], w=["rp_lT"])
        P.add("pe", lambda e: e.matmul(psA[:, :], lhsT=lT[:, 0, :], rhs=lw[:, 0, :], start=True, stop=True), r=["rp_lT", "rp_lw"], w=["rp_psA"])
        P.add("pe", lambda e: e.matmul(psF[:, :], lhsT=lT[:, 0, :], rhs=lw[:, 1, :], start=True, stop=True), r=["rp_lT", "rp_lw"], w=["rp_psF"])
        P.add("pe", lambda e: e.matmul(psB[:, :], lhsT=lT[:, 1, :], rhs=lw[:, 2, :], start=True, stop=True), r=["rp_lT", "rp_lw"], w=["rp_psB"])

        def mmg(e):
            e.matmul(psG[:, :], lhsT=lT[:, 2, :], rhs=lw[:, 3, :], start=True, stop=False)
            return e.matmul(psG[:, :], lhsT=lT[:, 3, :], rhs=lw[:, 4, :], start=False, stop=True)
        P.add("pe", mmg, r=["rp_lT", "rp_lw"], w=["rp_psG"])
        P.add("dve", lambda e: e.tensor_tensor(out=a[:], in0=psA[:, :], in1=cv[:, 0, :], op=ALU.add), r=["rp_psA", "rp_cv"], w=["rp_a"])
        P.add("act", lambda e: e.activation(out=a[:], in_=a[:], func=AF.Sigmoid), r=["rp_a"], w=["rp_a"])
        for (ps_, pres, ci, ri) in ((psF, "rp_psF", 1, 4), (psB, "rp_psB", 2, 5)):
            P.add("dve", lambda e, ps_=ps_, ci=ci, ri=ri: e.tensor_tensor(out=Rt[:, ri, :], in0=ps_[:, :], in1=cv[:, ci, :], op=ALU.add), r=[pres, "rp_cv"], w=[f"rp_R{ri}"])
            P.add("act", lambda e, ri=ri: e.activation(out=Rt[:, ri, :], in_=Rt[:, ri, :], func=AF.Sigmoid), r=[f"rp_R{ri}"], w=[f"rp_R{ri}"])
            P.add("act", lambda e, ri=ri: e.activation(out=Rt[:, ri, :], in_=Rt[:, ri, :], func=AF.Exp, scale=NEG_E05), r=[f"rp_R{ri}"], w=[f"rp_R{ri}"])
        P.add("act", lambda e: e.activation(out=Ft[:, 1, :], in_=psG[:, :], func=AF.Copy), r=["rp_psG"], w=["rp_F1"])
        P.add("pool", lambda e: e.tensor_copy(out=Ft[:, 0, :], in_=zz[:, 1024:1536]), r=["rp_zz"], w=["rp_F0"])
        P.add("pool", lambda e: e.tensor_copy(out=Rt[:, 3, :], in_=zz[:, 0:512]), r=["rp_zz"], w=["rp_R3"])
        P.add("dve", lambda e: e.tensor_tensor(out=kkr[:], in0=zz[:, 512:1024], in1=cv[:, 3, :], op=ALU.mult), r=["rp_zz", "rp_cv"], w=["rp_kkr"])
        P.add("act", lambda e: e.activation(out=t1[:], in_=kkr[:], func=AF.Square), r=["rp_kkr"], w=["rp_t1"])
        P.add("dve", lambda e: e.tensor_reduce(out=st[:, 0:8], in_=t1[:].rearrange("p (h k) -> p h k", k=64), op=ALU.add, axis=AX.X), r=["rp_t1"], w=["rp_st"])
        P.add("act", lambda e: e.activation(out=st[:, 8:16], in_=st[:, 0:8], func=AF.Sqrt, bias=e12[:, 0:1], scale=1.0), r=["rp_st", "rp_e12"], w=["rp_st"])
        P.add("dve", lambda e: e.reciprocal(out=st[:, 8:16], in_=st[:, 8:16]), r=["rp_st"], w=["rp_st"])
        P.add("dve", lambda e: e.tensor_tensor(out=kkr[:].rearrange("p (h k) -> p h k", k=64), in0=kkr[:].rearrange("p (h k) -> p h k", k=64),
                                               in1=st[:, 8:16].unsqueeze(2).to_broadcast([128, 8, 64]), op=ALU.mult), r=["rp_kkr", "rp_st"], w=["rp_kkr"])
        P.add("dve", lambda e: e.tensor_tensor(out=Rt[:, 1, :], in0=kkr[:], in1=a[:], op=ALU.mult), r=["rp_kkr", "rp_a"], w=["rp_R1"])
        P.add("pool", lambda e: e.tensor_scalar(out=Rt[:, 0, :], in0=kkr[:], scalar1=-1.0, scalar2=None, op0=ALU.mult), r=["rp_kkr"], w=["rp_R0"])
        P.add("dve", lambda e: e.scalar_tensor_tensor(out=t1[:], in0=a[:], scalar=-1.0, in1=cv[:, 4, :], op0=ALU.add, op1=ALU.mult), r=["rp_a", "rp_cv", "rp_st"], w=["rp_t1"])
        P.add("dve", lambda e: e.scalar_tensor_tensor(out=Rt[:, 2, :], in0=t1[:], scalar=1.0, in1=zz[:, 512:1024], op0=ALU.add, op1=ALU.mult), r=["rp_t1", "rp_zz"], w=["rp_R2"])
        allR = [f"rp_R{i}" for i in range(6)]
        P.dma(Rd[r0:r0 + 128, :, :], Rt[:], r=allR, w=["Rd"] + [], q="pool")
        P.dma(Fd[r0:r0 + 128, :, :], Ft[:], r=["rp_F0", "rp_F1"], w=["Fd"], q="pool")
        P.add("pool", lambda e: e.tensor_copy(out=vp[:].rearrange("p (hp hs e) -> p hs hp e", hs=2, e=64), in_=zz[:, 1024:1536].rearrange("p (hs hp e) -> p hs hp e", hs=2, e=64)),
              r=["rp_zz"], w=["rp_vp"])
        for hp in range(4):
            P.add("pe", lambda e, hp=hp: e.transpose(psV[:, hp * 128:(hp + 1) * 128], vp[:, hp * 128:(hp + 1) * 128], ident[:]), r=["rp_vp", "ident"], w=["rp_psV"])
        P.add("act", lambda e: e.activation(out=vT[:], in_=psV[:, :], func=AF.Copy), r=["rp_psV"], w=["rp_vT"])
        P.dma(VTd[c], vT[:], r=["rp_vT"], w=["VTd"], q="pool")


def phase_rwkv_scan(P, Rd, VTd, ydir, ident, TB=8, max_chunks=None):
    S = P.sb("rs_S", [128, 256]); tmp = P.sb("rs_tmp", [128, 256]); tmp2 = [P.sb(f"rs_tmp2{i}", [128, 256]) for i in range(2)]
    sa = P.sb("rs_sa", [128, 4]); vT = [P.sb(f"rs_vT{i}", [128, 4, 128]) for i in range(2)]; Y = [P.sb(f"rs_Y{i}", [128, 4, 128]) for i in range(2)]
    BC = [P.sb(f"rs_BC{i}", [128, 5, TB, 256]) for i in range(2)]
    yo = [P.sb(f"rs_yo{i}", [128, 512]) for i in range(2)]
    psY = [P.ps(f"rs_psY{i}", [128, 512]) for i in range(2)]
    Rt = Rd.tensor
    RW = 6 * 512
    blk = 0
    for d in range(2):
        P.add("dve", lambda e: e.memset(S[:], 0.0), w=["rs_S"])
        for ci, (row0, rev) in enumerate(chunk_order(d)[:max_chunks]):
            c = row0 // 128
            vt = vT[ci % 2]; vres = f"rs_vT{ci % 2}"; Yc = Y[ci % 2]; yres = f"rs_Y{ci % 2}"
            P.dma(vt[:].rearrange("p a b -> p (a b)"), VTd[c], r=["VTd"], w=[vres])
            for b0 in range(0, 128, TB):
                bc = BC[blk % 2]; bres = f"rs_BC{blk % 2}"; blk += 1
                tb0 = row0 + (b0 if not rev else 128 - b0 - TB)
                for hs in range(2):
                    for xi in range(5):
                        xs = xi if xi < 4 else 4 + d
                        src = bass.AP(Rt, tb0 * RW + xs * 512 + hs * 256, [[0, 64], [RW, TB], [1, 256]])
                        P.dma(bc[hs * 64:(hs + 1) * 64, xi, :, :], src, r=["Rd"], w=[bres], q=("sp" if hs == 0 else "act"))
                for ti in range(TB):
                    tl = ti if not rev else TB - 1 - ti
                    tc = (b0 + ti) if not rev else (127 - b0 - ti)
                    t2 = tmp2[ti % 2]; t2res = f"rs_tmp2{ti % 2}"
                    S3 = S[:].rearrange("p (h k) -> p h k", k=64)
                    tm3 = tmp[:].rearrange("p (h k) -> p h k", k=64)
                    P.add("pool", lambda e, bc=bc, tl=tl, vt=vt, tc=tc, t2=t2: e.tensor_tensor(out=t2[:].rearrange("p (h k) -> p h k", k=64),
                          in0=bc[:, 2, tl, :].rearrange("p (h k) -> p h k", k=64), in1=vt[:, :, tc:tc + 1].to_broadcast([128, 4, 64]), op=ALU.mult),
                          r=[bres, vres], w=[t2res])
                    P.add("dve", lambda e, bc=bc, tl=tl: e.tensor_tensor(out=tmp[:], in0=S[:], in1=bc[:, 0, tl, :], op=ALU.mult), r=["rs_S", bres], w=["rs_tmp"])
                    P.add("dve", lambda e, tm3=tm3: e.tensor_reduce(out=sa[:], in_=tm3, op=ALU.add, axis=AX.X), r=["rs_tmp"], w=["rs_sa"])
                    P.add("dve", lambda e, bc=bc, tl=tl: e.tensor_tensor(out=S[:], in0=S[:], in1=bc[:, 4, tl, :], op=ALU.mult), r=["rs_S", bres, "rs_tmp"], w=["rs_S"])
                    P.add("dve", lambda e, bc=bc, tl=tl, tm3=tm3: e.tensor_tensor(out=tm3, in0=bc[:, 1, tl, :].rearrange("p (h k) -> p h k", k=64),
                                                                          in1=sa[:].unsqueeze(2).to_broadcast([128, 4, 64]), op=ALU.mult), r=[bres, "rs_sa"], w=["rs_tmp"])
                    P.add("dve", lambda e: e.tensor_tensor(out=S[:], in0=S[:], in1=tmp[:], op=ALU.add), r=["rs_S", "rs_tmp"], w=["rs_S"])
                    P.add("dve", lambda e, t2=t2: e.tensor_tensor(out=S[:], in0=S[:], in1=t2[:], op=ALU.add), r=["rs_S", t2res], w=["rs_S"])
                    P.add("dve", lambda e, bc=bc, tl=tl: e.tensor_tensor(out=tmp[:], in0=S[:], in1=bc[:, 3, tl, :], op=ALU.mult), r=["rs_S", bres], w=["rs_tmp"])
                    P.add("dve", lambda e, tm3=tm3, Yc=Yc, tc=tc: e.tensor_reduce(out=Yc[:, :, tc], in_=tm3, op=ALU.add, axis=AX.X), r=["rs_tmp"], w=[yres])
            ps = psY[ci % 2]; pres = f"rs_psY{ci % 2}"; yob = yo[ci % 2]; yores = f"rs_yo{ci % 2}"
            for hp in range(4):
                P.add("pe", lambda e, hp=hp, ps=ps, Yc=Yc: e.transpose(ps[:, hp * 128:(hp + 1) * 128], Yc[:, hp, :], ident[:]), r=[yres, "ident"], w=[pres])
            P.add("act", lambda e, ps=ps, yob=yob: e.activation(out=yob[:], in_=ps[:, :], func=AF.Copy), r=[pres], w=[yores])
            P.dma(ydir[d][row0:row0 + 128, :], yob[:], r=[yores], w=["ydirA"], q="pool")


def phase_rwkv_finish(P, Rd, Fd, cv_d, ydir, yout):
    a = P.sb("wf_a", [128, 512]); b = P.sb("wf_b", [128, 512]); y = P.sb("wf_y", [128, 512]); Rt = P.sb("wf_R", [128, 2, 512]); Ft = P.sb("wf_F", [128, 2, 512])
    cv = P.sb("wf_cv", [128, 8, 512]); st = P.sb("wf_st", [128, 8, 4]); sq = P.sb("wf_sq", [128, 512]); epsb = P.sb("wf_eps", [128, 1])
    P.dma(cv[:], cv_d[:, :, :], w=["wf_cv"])
    P.add("dve", lambda e: e.memset(epsb[:], GN_EPS), w=["wf_eps"])
    h3 = lambda t: t[:].rearrange("p (h k) -> p h k", k=64)
    for c in range(NTOK // 128):
        r0 = c * 128
        P.dma(a[:], ydir[0][r0:r0 + 128, :], r=["ydirA"], w=["wf_a"])
        P.dma(b[:], ydir[1][r0:r0 + 128, :], r=["ydirA"], w=["wf_b"])
        P.dma(Rt[:], Rd[r0:r0 + 128, 2:4, :], r=["Rd"], w=["wf_R"])
        P.dma(Ft[:], Fd[r0:r0 + 128, :, :], r=["Fd"], w=["wf_F"])
        P.add("dve", lambda e: e.tensor_tensor(out=a[:], in0=a[:], in1=b[:], op=ALU.add), r=["wf_a", "wf_b"], w=["wf_a"])
        P.add("pool", lambda e: e.tensor_copy(out=y[:].rearrange("p (hs hp e) -> p hs hp e", hs=2, e=64), in_=a[:].rearrange("p (hp hs e) -> p hs hp e", hs=2, e=64)), r=["wf_a"], w=["wf_y"])
        P.add("dve", lambda e: e.tensor_reduce(out=st[:, :, 0], in_=h3(y), op=ALU.add, axis=AX.X), r=["wf_y"], w=["wf_st"])
        P.add("dve", lambda e: e.tensor_scalar(out=st[:, :, 1], in0=st[:, :, 0], scalar1=-1.0 / 64, scalar2=None, op0=ALU.mult), r=["wf_st"], w=["wf_st"])
        P.add("dve", lambda e: e.tensor_tensor(out=h3(y), in0=h3(y), in1=st[:, :, 1:2].to_broadcast([128, 8, 64]), op=ALU.add), r=["wf_y", "wf_st"], w=["wf_y"])
        P.add("act", lambda e: e.activation(out=sq[:], in_=y[:], func=AF.Square), r=["wf_y"], w=["wf_sq"])
        P.add("dve", lambda e: e.tensor_reduce(out=st[:, :, 2], in_=h3(sq), op=ALU.add, axis=AX.X), r=["wf_sq"], w=["wf_st"])
        P.add("act", lambda e: e.activation(out=st[:, :, 3], in_=st[:, :, 2], func=AF.Sqrt, scale=1.0 / 64, bias=epsb[:, 0:1]), r=["wf_st", "wf_eps"], w=["wf_st"])
        P.add("dve", lambda e: e.reciprocal(out=st[:, :, 3], in_=st[:, :, 3]), r=["wf_st"], w=["wf_st"])
        P.add("dve", lambda e: e.tensor_tensor(out=h3(y), in0=h3(y), in1=st[:, :, 3:4].to_broadcast([128, 8, 64]), op=ALU.mult), r=["wf_y", "wf_st"], w=["wf_y"])
        P.add("dve", lambda e: e.tensor_tensor(out=y[:], in0=y[:], in1=cv[:, 6, :], op=ALU.mult), r=["wf_y", "wf_cv"], w=["wf_y"])
        P.add("dve", lambda e: e.tensor_tensor(out=y[:], in0=y[:], in1=cv[:, 7, :], op=ALU.add), r=["wf_y", "wf_cv"], w=["wf_y"])
        P.add("pool", lambda e: e.tensor_tensor(out=sq[:], in0=Rt[:, 0, :], in1=Rt[:, 1, :], op=ALU.mult), r=["wf_R", "wf_st"], w=["wf_sq"])
        P.add("pool", lambda e: e.tensor_tensor(out=sq[:], in0=sq[:], in1=cv[:, 5, :], op=ALU.mult), r=["wf_sq", "wf_cv"], w=["wf_sq"])
        P.add("dve", lambda e: e.tensor_reduce(out=st[:, :, 0], in_=h3(sq), op=ALU.add, axis=AX.X), r=["wf_sq", "wf_y"], w=["wf_st"])
        P.add("dve", lambda e: e.tensor_tensor(out=h3(sq), in0=Ft[:, 0, :].rearrange("p (h k) -> p h k", k=64), in1=st[:, :, 0:1].to_broadcast([128, 8, 64]), op=ALU.mult),
              r=["wf_F", "wf_st"], w=["wf_sq"])
        P.add("dve", lambda e: e.tensor_tensor(out=y[:], in0=y[:], in1=sq[:], op=ALU.add), r=["wf_y", "wf_sq"], w=["wf_y"])
        P.add("dve", lambda e: e.tensor_tensor(out=y[:], in0=y[:], in1=Ft[:, 1, :], op=ALU.mult), r=["wf_y", "wf_F"], w=["wf_y"])
        P.dma(yout[r0:r0 + 128, 0:512], y[:], r=["wf_y"], w=["yout"], q="pool")


def build_la0(do_rwkv=True, do_ret=True, dbg=False, max_chunks=None):
    nc = bass.Bass("TRN2", target_bir_lowering=False)
    P = Prog(nc)
    P.use_arena(36000)
    ei = lambda name, shape: nc.dram_tensor(name, list(shape), F32, kind="ExternalInput").ap()
    NC_ALL = 3584
    xT = ei("xT", [D, NTOK]); W = ei("W", [D, NC_ALL])
    vecs = ei("vecs", [128, 5, KT])
    rope = ei("rope", [NTOK, 64]); rde = ei("rde", [128, 8]); rcst = ei("rcst", [2, 128, 259]); ident_d = ei("ident", [128, 128])
    if do_rwkv:
        shmask = ei("shmask", [NTOK, 4]); mu_d = ei("mu", [128, 2048]); cv_d = ei("cv", [128, 8, 512]); lw_d = ei("lw", [128, 5, 512])
    yout = nc.dram_tensor("yout", [NTOK, 1024], BF16, kind="ExternalOutput").ap()
    dk = "ExternalOutput" if dbg else "Internal"
    ptm = nc.dram_tensor("ptm", [NTOK, NC_ALL], F32, kind=dk).ap()
    ydirB = nc.dram_tensor("ydirB", [2, NTOK, 512], F32).ap()
    ydirA = nc.dram_tensor("ydirA", [2, NTOK, 512], F32).ap()
    Rd = nc.dram_tensor("Rd", [NTOK, 6, 512], F32).ap()
    Fd = nc.dram_tensor("Fd", [NTOK, 2, 512], F32).ap()
    VTd = nc.dram_tensor("VTd", [NTOK // 128, 128, 512], F32).ap()
    vec = P.sb("vec", [128, 5, KT]); weff = P.sb("weff", [128, 2, KT]); ones = P.sb("ones", [128, 128]); epsb = P.sb("epsb", [128, 1])
    ident = P.sb("ident_s", [128, 128])
    P.persist()
    P.dma(vec[:], vecs[:, :, :], w=["vec"]); P.dma(ident[:], ident_d[:, :], w=["ident"])
    P.add("dve", lambda e: e.memset(ones[:], 1.0), w=["ones"])
    P.add("dve", lambda e: e.memset(epsb[:], EPS), w=["epsb"])
    for c in range(2):
        P.add("dve", lambda e, c=c: e.scalar_tensor_tensor(out=weff[:, c, :], in0=vec[:, c * 2 + 1, :], scalar=1.0, in1=vec[:, 4, :], op0=ALU.add, op1=ALU.mult),
              r=["vec"], w=["weff"])
    phase_inproj(P, xT, W, NC_ALL, ptm, vec, weff, ones, epsb)
    if do_ret:
        P.phase()
        phase_retention(P, ptm, 2048, rope, rde, rcst, ydirB, ident)
        P.phase()
        phase_ret_finish(P, ptm, 2048 + 1024, ydirB, yout, 512)
    if do_rwkv:
        P.phase()
        phase_rwkv_prep(P, ptm, shmask, mu_d, cv_d, lw_d, Rd, Fd, VTd, ident)
        P.phase()
        phase_rwkv_scan(P, Rd, VTd, ydirA, ident, max_chunks=max_chunks)
        P.phase()
        phase_rwkv_finish(P, Rd, Fd, cv_d, ydirA, yout)
    return P.emit()


def phase_hgrn(P, ptm, QC0, lbl_d, hcst_d, ydir, ident, NH=6):
    W = NH * 128
    HG = 3
    q = P.sb("hg_q", [128, W]); f = P.sb("hg_f", [128, W]); v = P.sb("hg_v", [128, W]); k = P.sb("hg_k", [128, W]); lf = P.sb("hg_lf", [128, W])
    lb = P.sb("hg_lb", [128, 2, W]); oml = P.sb("hg_oml", [128, W]); cst = P.sb("hg_cst", [128, 516])
    cb = P.sb("hg_cb", [128, 3, HG * 128]); ta = P.sb("hg_ta", [128, HG * 128]); tb = P.sb("hg_tb", [128, HG * 128])
    eq = P.sb("hg_eq", [128, HG * 128]); ek = P.sb("hg_ek", [128, HG * 128]); eb = P.sb("hg_eb", [128, HG * 128]); el = P.sb("hg_el", [128, HG * 128])
    pre = P.sb("hg_pre", [128, HG, 4, 128])
    ks = P.sb("hg_ks", [128, HG, 2, 128]); preT = P.sb("hg_preT", [128, HG, 4, 128]); dec = P.sb("hg_dec", [128, HG, 2])
    sc = P.sb("hg_sc", [128, HG, 128]); S = P.sb("hg_S", [128, NH, 128]); o = P.sb("hg_o", [128, W])
    banks = [P.ps(f"hg_b{i}", [128, 512]) for i in range(8)]
    bn = [f"hg_b{i}" for i in range(8)]
    P.dma(lb[:], lbl_d[:, :, :], w=["hg_lb"])
    P.add("dve", lambda e: e.tensor_tensor(out=lb[:, 0, :], in0=lb[:, 1, :], in1=lb[:, 0, :], op=ALU.subtract), r=["hg_lb"], w=["hg_lb"])
    P.add("act", lambda e: e.activation(out=lb[:, 0, :], in_=lb[:, 0, :], func=AF.Sigmoid), r=["hg_lb"], w=["hg_lb"])
    P.add("dve", lambda e: e.tensor_scalar(out=oml[:], in0=lb[:, 0, :], scalar1=-1.0, scalar2=1.0, op0=ALU.mult, op1=ALU.add), r=["hg_lb"], w=["hg_oml"])
    for d in range(2):
        P.dma(cst[:], hcst_d[d], w=["hg_cst"])
        TRI = cst[:, 0:128]; MID = cst[:, 128:256]; ALLC = cst[:, 256:384]; CM = cst[:, 384:512]
        P.add("dve", lambda e: e.memset(S[:], 0.0), w=[f"hg_S{h}" for h in range(NH)])
        order = (0, 1) if d == 0 else (1, 0)
        for (row0, rev) in chunk_order(d):
            P.dma(q[:], ptm[row0:row0 + 128, QC0:QC0 + W], r=["ptm"], w=["hg_q"])
            P.dma(f[:], ptm[row0:row0 + 128, QC0 + (1 + d) * W:QC0 + (2 + d) * W], r=["ptm"], w=["hg_f"])
            P.dma(v[:], ptm[row0:row0 + 128, QC0 + 3 * W:QC0 + 4 * W], r=["ptm"], w=["hg_v"])
            P.add("act", lambda e: e.activation(out=q[:], in_=q[:], func=AF.Silu), r=["hg_q"], w=["hg_q"])
            P.add("act", lambda e: e.activation(out=f[:], in_=f[:], func=AF.Sigmoid), r=["hg_f"], w=["hg_f"])
            P.add("dve", lambda e: e.tensor_tensor(out=f[:], in0=f[:], in1=oml[:], op=ALU.mult), r=["hg_f", "hg_oml"], w=["hg_f"])
            P.add("dve", lambda e: e.tensor_tensor(out=f[:], in0=f[:], in1=lb[:, 0, :], op=ALU.add), r=["hg_f", "hg_lb"], w=["hg_f"])
            P.add("pool", lambda e: e.tensor_scalar(out=k[:], in0=f[:], scalar1=-1.0, scalar2=1.0, op0=ALU.mult, op1=ALU.add), r=["hg_f"], w=["hg_k"])
            P.add("act", lambda e: e.activation(out=lf[:], in_=f[:], func=AF.Ln), r=["hg_f"], w=["hg_lf"])
            for g0 in range(0, NH, HG):
                c0 = g0 * 128; cw = HG * 128
                for i, M in enumerate((TRI, MID, ALLC)):
                    P.add("pe", lambda e, i=i, M=M, c0=c0, cw=cw: e.matmul(banks[i][:, 0:cw], lhsT=M, rhs=lf[:, c0:c0 + cw], start=True, stop=True),
                          r=["hg_cst", "hg_lf"], w=[bn[i]])
                    P.add("act", lambda e, i=i, cw=cw: e.activation(out=cb[:, i, :], in_=banks[i][:, 0:cw], func=AF.Copy), r=[bn[i]], w=[f"hg_cb{i}"])
                for hl in range(HG):
                    P.add("pe", lambda e, hl=hl, c0=c0: e.matmul(banks[3][:, hl * 2:hl * 2 + 2], lhsT=lf[:, c0 + hl * 128:c0 + (hl + 1) * 128], rhs=cst[:, 514:516], start=True, stop=True),
                          r=["hg_cst", "hg_lf"], w=[bn[3]])
                P.add("act", lambda e: e.activation(out=dec[:].rearrange("p a b -> p (a b)"), in_=banks[3][:, 0:HG * 2], func=AF.Exp), r=[bn[3]], w=["hg_dec"])
                P.add("dve", lambda e: e.tensor_tensor(out=ta[:], in0=cb[:, 0, :], in1=cb[:, 1, :], op=ALU.subtract), r=["hg_cb0", "hg_cb1"], w=["hg_ta"])
                P.add("dve", lambda e: e.tensor_tensor(out=tb[:], in0=cb[:, 2, :], in1=cb[:, 0, :], op=ALU.subtract), r=["hg_cb2", "hg_cb0"], w=["hg_tb"])
                P.add("act", lambda e: e.activation(out=eq[:], in_=ta[:], func=AF.Exp), r=["hg_ta"], w=["hg_eq"])
                P.add("act", lambda e: e.activation(out=ek[:], in_=ta[:], func=AF.Exp, scale=-1.0), r=["hg_ta"], w=["hg_ek"])
                P.add("act", lambda e: e.activation(out=eb[:], in_=cb[:, 0, :], func=AF.Exp), r=["hg_cb0"], w=["hg_eb"])
                P.add("act", lambda e: e.activation(out=el[:], in_=tb[:], func=AF.Exp), r=["hg_tb"], w=["hg_el"])
                for hl in range(HG):
                    cs = slice(c0 + hl * 128, c0 + (hl + 1) * 128); ls = slice(hl * 128, (hl + 1) * 128)
                    P.add("dve", lambda e, hl=hl, cs=cs, ls=ls: e.tensor_tensor(out=pre[:, hl, 0, :], in0=q[:, cs], in1=eq[:, ls], op=ALU.mult), r=["hg_q", "hg_eq"], w=["hg_pre"])
                    P.add("pool", lambda e, hl=hl, cs=cs, ls=ls: e.tensor_tensor(out=pre[:, hl, 1, :], in0=k[:, cs], in1=ek[:, ls], op=ALU.mult), r=["hg_k", "hg_ek"], w=["hg_pre1"])
                    for xi, X in enumerate(order):
                        P.add("dve", lambda e, hl=hl, cs=cs, ls=ls, xi=xi, X=X: e.scalar_tensor_tensor(out=pre[:, hl, 2 + xi, :], in0=q[:, cs], scalar=cst[:, 512 + X:513 + X], in1=eb[:, ls],
                                                                                                    op0=ALU.mult, op1=ALU.mult), r=["hg_q", "hg_cst", "hg_eb"], w=["hg_pre"])
                        P.add("dve", lambda e, hl=hl, cs=cs, ls=ls, xi=xi, X=X: e.scalar_tensor_tensor(out=ks[:, hl, xi, :], in0=k[:, cs], scalar=cst[:, 512 + X:513 + X], in1=el[:, ls],
                                                                                                    op0=ALU.mult, op1=ALU.mult), r=["hg_k", "hg_cst", "hg_el"], w=["hg_ks"])
                for hl in range(HG):
                    for j in range(4):
                        P.add("pe", lambda e, hl=hl, j=j: e.transpose(banks[hl][:, j * 128:(j + 1) * 128], pre[:, hl, j, :], ident[:]), r=["hg_pre", "hg_pre1", "ident"], w=[bn[hl]])
                    P.add("act", lambda e, hl=hl: e.activation(out=preT[:, hl, :, :].rearrange("p a b -> p (a b)"), in_=banks[hl][:, :], func=AF.Copy), r=[bn[hl]], w=[f"hg_preT{hl}"])
                for hl in range(HG):
                    P.add("pe", lambda e, hl=hl: e.matmul(banks[4][:, hl * 128:(hl + 1) * 128], lhsT=preT[:, hl, 1, :], rhs=preT[:, hl, 0, :], start=True, stop=True),
                          r=[f"hg_preT{hl}"], w=[bn[4]])
                P.add("dve", lambda e: e.tensor_tensor(out=sc[:], in0=banks[4][:, 0:HG * 128].rearrange("p (a b) -> p a b", b=128), in1=CM.unsqueeze(1).to_broadcast([128, HG, 128]), op=ALU.mult),
                      r=[bn[4], "hg_cst"], w=["hg_sc"])
                for hl in range(HG):
                    h = g0 + hl; vs = v[:, h * 128:(h + 1) * 128]; os_ = banks[5][:, hl * 128:(hl + 1) * 128]

                    def mm1(e, hl=hl, h=h, vs=vs, os_=os_):
                        e.matmul(os_, lhsT=sc[:, hl, :], rhs=vs, start=True, stop=False)
                        return e.matmul(os_, lhsT=preT[:, hl, 2, :], rhs=S[:, h, :], start=False, stop=False)
                    P.add("pe", mm1, r=["hg_sc", "hg_v", f"hg_preT{hl}", f"hg_S{h}"], w=[f"hg_o{hl}"])
                    P.add("pe", lambda e, hl=hl, vs=vs: e.matmul(banks[6][:, hl * 128:(hl + 1) * 128], lhsT=ks[:, hl, 0, :], rhs=vs, start=True, stop=True), r=["hg_ks", "hg_v"], w=[f"hg_u{hl}"])
                    P.add("dve", lambda e, hl=hl, h=h: e.scalar_tensor_tensor(out=S[:, h, :], in0=S[:, h, :], scalar=dec[:, hl, order[0]:order[0] + 1], in1=banks[6][:, hl * 128:(hl + 1) * 128],
                                                                         op0=ALU.mult, op1=ALU.add), r=[f"hg_S{h}", "hg_dec", f"hg_u{hl}"], w=[f"hg_S{h}"])
                    P.add("pe", lambda e, hl=hl, h=h, os_=os_: e.matmul(os_, lhsT=preT[:, hl, 3, :], rhs=S[:, h, :], start=False, stop=True), r=[f"hg_preT{hl}", f"hg_S{h}", f"hg_o{hl}"], w=[f"hg_o{hl}"])
                    P.add("pe", lambda e, hl=hl, vs=vs: e.matmul(banks[7][:, hl * 128:(hl + 1) * 128], lhsT=ks[:, hl, 1, :], rhs=vs, start=True, stop=True), r=["hg_ks", "hg_v"], w=[f"hg_w{hl}"])
                    P.add("dve", lambda e, hl=hl, h=h: e.scalar_tensor_tensor(out=S[:, h, :], in0=S[:, h, :], scalar=dec[:, hl, order[1]:order[1] + 1], in1=banks[7][:, hl * 128:(hl + 1) * 128],
                                                                         op0=ALU.mult, op1=ALU.add), r=[f"hg_S{h}", "hg_dec", f"hg_w{hl}"], w=[f"hg_S{h}"])
                    P.add("act", lambda e, hl=hl, h=h, os_=os_: e.activation(out=o[:, h * 128:(h + 1) * 128], in_=os_, func=AF.Copy), r=[f"hg_o{hl}"], w=["hg_oo"])
            P.dma(ydir[d][row0:row0 + 128, :], o[:], r=["hg_oo"], w=["ydirH"], q="pool")


def phase_hgrn_finish(P, ptm, GC0, nw_d, ydir, yout, YC0, NH=6):
    W = NH * 128
    a = P.sb("hf_a", [128, W]); b = P.sb("hf_b", [128, W]); g = P.sb("hf_g", [128, W]); sq = P.sb("hf_sq", [128, W]); nw = P.sb("hf_nw", [128, W])
    st = P.sb("hf_st", [128, NH, 2]); epsb = P.sb("hf_eps", [128, 1])
    P.dma(nw[:], nw_d[:, :], w=["hf_nw"])
    P.add("dve", lambda e: e.memset(epsb[:], EPS), w=["hf_eps"])
    for c in range(NTOK // 128):
        r0 = c * 128
        P.dma(a[:], ydir[0][r0:r0 + 128, :], r=["ydirH"], w=["hf_a"])
        P.dma(b[:], ydir[1][r0:r0 + 128, :], r=["ydirH"], w=["hf_b"])
        P.dma(g[:], ptm[r0:r0 + 128, GC0:GC0 + W], r=["ptm"], w=["hf_g"])
        P.add("dve", lambda e: e.tensor_tensor(out=a[:], in0=a[:], in1=b[:], op=ALU.add), r=["hf_a", "hf_b"], w=["hf_a"])
        P.add("act", lambda e: e.activation(out=sq[:], in_=a[:], func=AF.Square), r=["hf_a"], w=["hf_sq"])
        P.add("dve", lambda e: e.tensor_reduce(out=st[:, :, 0], in_=sq[:].rearrange("p (h e) -> p h e", e=128), op=ALU.add, axis=AX.X), r=["hf_sq"], w=["hf_st"])
        P.add("act", lambda e: e.activation(out=st[:, :, 1], in_=st[:, :, 0], func=AF.Sqrt, scale=1.0 / 128, bias=epsb[:, 0:1]), r=["hf_st", "hf_eps"], w=["hf_st"])
        P.add("dve", lambda e: e.reciprocal(out=st[:, :, 1], in_=st[:, :, 1]), r=["hf_st"], w=["hf_st"])
        P.add("dve", lambda e: e.tensor_tensor(out=a[:].rearrange("p (h e) -> p h e", e=128), in0=a[:].rearrange("p (h e) -> p h e", e=128),
                                               in1=st[:, :, 1:2].to_broadcast([128, NH, 128]), op=ALU.mult), r=["hf_a", "hf_st"], w=["hf_a"])
        P.add("act", lambda e: e.activation(out=g[:], in_=g[:], func=AF.Silu), r=["hf_g"], w=["hf_g"])
        P.add("pool", lambda e: e.tensor_tensor(out=g[:], in0=g[:], in1=nw[:], op=ALU.mult), r=["hf_g", "hf_nw"], w=["hf_g"])
        P.add("dve", lambda e: e.tensor_tensor(out=a[:], in0=a[:], in1=g[:], op=ALU.mult), r=["hf_a", "hf_g"], w=["hf_a"])
        P.dma(yout[r0:r0 + 128, YC0:YC0 + W], a[:], r=["hf_a"], w=["yout"], q="pool")


TWO_PI = 6.283185307179586


def s5_disc(P, prm, out, n, tag):
    r = [tag]
    lre = prm[:, 0, :]; lim = prm[:, 1, :]
    mag = out[:, 0, :]; cs = out[:, 1, :]; sn = out[:, 2, :]; cre = out[:, 3, :]; cim = out[:, 4, :]; t0 = out[:, 5, :]; t1 = out[:, 6, :]; t2 = out[:, 7, :]
    A = lambda eng, fn: P.add(eng, fn, r=r, w=r)
    A("act", lambda e: e.activation(out=t0, in_=prm[:, 2, :], func=AF.Exp))
    A("dve", lambda e: e.tensor_tensor(out=mag, in0=lre, in1=t0, op=ALU.mult))
    A("act", lambda e: e.activation(out=mag, in_=mag, func=AF.Exp))
    A("dve", lambda e: e.scalar_tensor_tensor(out=t0, in0=lim, scalar=1.0 / TWO_PI, in1=t0, op0=ALU.mult, op1=ALU.mult))
    ti = out[:, 7, :].bitcast(mybir.dt.int32)
    A("dve", lambda e: e.tensor_copy(out=ti, in_=t0))
    A("dve", lambda e: e.tensor_copy(out=t1, in_=ti))
    A("dve", lambda e: e.tensor_tensor(out=t0, in0=t0, in1=t1, op=ALU.subtract))
    for (dst, shift) in ((sn, 0.0), (cs, 0.25)):
        A("dve", lambda e, dst=dst, shift=shift: e.tensor_scalar(out=dst, in0=t0, scalar1=shift, scalar2=None, op0=ALU.add))
        for _ in range(2):
            A("dve", lambda e, dst=dst: e.tensor_scalar(out=t1, in0=dst, scalar1=0.5, scalar2=None, op0=ALU.is_gt))
            A("dve", lambda e, dst=dst: e.tensor_tensor(out=dst, in0=dst, in1=t1, op=ALU.subtract))
            A("dve", lambda e, dst=dst: e.tensor_scalar(out=t1, in0=dst, scalar1=-0.5, scalar2=None, op0=ALU.is_lt))
            A("dve", lambda e, dst=dst: e.tensor_tensor(out=dst, in0=dst, in1=t1, op=ALU.add))
        A("act", lambda e, dst=dst: e.activation(out=dst, in_=dst, func=AF.Sin, scale=TWO_PI))
    A("dve", lambda e: e.tensor_tensor(out=t0, in0=mag, in1=cs, op=ALU.mult))
    A("dve", lambda e: e.tensor_scalar(out=t0, in0=t0, scalar1=-1.0, scalar2=None, op0=ALU.add))
    A("dve", lambda e: e.tensor_tensor(out=t1, in0=mag, in1=sn, op=ALU.mult))
    A("dve", lambda e: e.tensor_tensor(out=cre, in0=t0, in1=lre, op=ALU.mult))
    A("dve", lambda e: e.tensor_tensor(out=t2, in0=t1, in1=lim, op=ALU.mult))
    A("dve", lambda e: e.tensor_tensor(out=cre, in0=cre, in1=t2, op=ALU.add))
    A("dve", lambda e: e.tensor_tensor(out=cim, in0=t1, in1=lre, op=ALU.mult))
    A("dve", lambda e: e.tensor_tensor(out=t2, in0=t0, in1=lim, op=ALU.mult))
    A("dve", lambda e: e.tensor_tensor(out=cim, in0=cim, in1=t2, op=ALU.subtract))
    A("dve", lambda e: e.tensor_tensor(out=t0, in0=lre, in1=lre, op=ALU.mult))
    A("dve", lambda e: e.tensor_tensor(out=t1, in0=lim, in1=lim, op=ALU.mult))
    A("dve", lambda e: e.tensor_tensor(out=t0, in0=t0, in1=t1, op=ALU.add))
    A("dve", lambda e: e.reciprocal(out=t0, in_=t0))
    A("dve", lambda e: e.tensor_tensor(out=cre, in0=cre, in1=t0, op=ALU.mult))
    A("dve", lambda e: e.tensor_tensor(out=cim, in0=cim, in1=t0, op=ALU.mult))


def phase_s5(P, ptm, prmR_d, prmC_d, bexp_d, cexp_d, ydir, ident, jmat_d):
    L = 512
    SEG = [(0, NCTX)] + [(NCTX + i * L, L) for i in range((NTOK - NCTX) // L)]
    uT = P.sb("s5_uT", [128, NTOK]); yacc = P.sb("s5_yacc", [128, NTOK])
    prmR = P.sb("s5_prmR", [128, 3, 512]); dR = P.sb("s5_dR", [128, 8, 512]); prmC = P.sb("s5_prmC", [128, 3, 16]); dCs = [P.sb(f"s5_dC{i}", [128, 8, 16]) for i in range(2)]
    bex = P.sb("s5_bex", [128, 2, 512]); BB = P.sb("s5_BB", [128, 2, 512]); cex = P.sb("s5_cex", [128, 2, 128]); tmpb = P.sb("s5_tmpb", [128, 512])
    E = P.sb("s5_E", [128, 2, L]); rot = P.sb("s5_rot", [128, 12, 2]); cc = P.sb("s5_c", [128, 2, L]); zz = P.sb("s5_z", [128, 2, L]); xx = P.sb("s5_x", [128, 2, L])
    t1 = P.sb("s5_t1", [128, L]); carry = P.sb("s5_carry", [128, 6]); um = P.sb("s5_um", [128, 128]); uf = P.sb("s5_uf", [128, 128]); jm = P.sb("s5_J", [128, 128])
    ot = P.sb("s5_ot", [128, 128]); rho = P.sb("s5_rho", [128, L])
    psB = [P.ps(f"s5_psB{i}", [128, 512]) for i in range(2)]; psY = P.ps("s5_psY", [128, 512]); psT = P.ps("s5_psT", [128, 512]); psF = P.ps("s5_psF", [128, 512])
    P.dma(jm[:], jmat_d[:, :], w=["s5_J"])
    for d in range(2):
        P.dma(prmC[:], prmC_d[d], w=["s5_dC"])
        s5_disc(P, prmC, dCs[d], 16, "s5_dC")
    for ut in range(4):
        for d in range(2):
            dC = dCs[d]
            for c in range(NTOK // 128):
                if d == 0:
                    src_c = c
                else:
                    src_c = (NCTX // 128 - 1 - c) if c < NCTX // 128 else (NTOK // 128 - 1 - (c - NCTX // 128))
                P.dma(um[:], ptm[src_c * 128:(src_c + 1) * 128, ut * 128:(ut + 1) * 128], r=["ptm"], w=["s5_um"])
                if d == 1:
                    P.add("pe", lambda e: e.matmul(psF[:, 0:128], lhsT=jm[:], rhs=um[:], start=True, stop=True), r=["s5_J", "s5_um"], w=["s5_psF"])
                    P.add("act", lambda e: e.activation(out=uf[:], in_=psF[:, 0:128], func=AF.Copy), r=["s5_psF"], w=["s5_uf"])
                    srct, sres = uf, "s5_uf"
                else:
                    srct, sres = um, "s5_um"
                P.add("pe", lambda e, srct=srct: e.transpose(psT[:, 0:128], srct[:], ident[:]), r=[sres, "ident"], w=["s5_psT"])
                P.add("act", lambda e, c=c: e.activation(out=uT[:, c * 128:(c + 1) * 128], in_=psT[:, 0:128], func=AF.Copy), r=["s5_psT"], w=["s5_uT"])
            P.dma(prmR[:], prmR_d[d, :, :, ut * 512:(ut + 1) * 512], w=["s5_dR"])
            s5_disc(P, prmR, dR, 512, "s5_dR")
            P.dma(bex[:, 0, :], bexp_d[0, ut], w=["s5_bex"]); P.dma(bex[:, 1, :], bexp_d[1, ut], w=["s5_bex"])
            P.add("dve", lambda e: e.tensor_tensor(out=BB[:, 0, :], in0=bex[:, 0, :], in1=dR[:, 3, :], op=ALU.mult), r=["s5_bex", "s5_dR"], w=["s5_BB"])
            P.add("dve", lambda e: e.tensor_tensor(out=tmpb[:], in0=bex[:, 1, :], in1=dR[:, 4, :], op=ALU.mult), r=["s5_bex", "s5_dR"], w=["s5_tmpb"])
            P.add("dve", lambda e: e.tensor_tensor(out=BB[:, 0, :], in0=BB[:, 0, :], in1=tmpb[:], op=ALU.subtract), r=["s5_BB", "s5_tmpb"], w=["s5_BB"])
            P.add("dve", lambda e: e.tensor_tensor(out=BB[:, 1, :], in0=bex[:, 1, :], in1=dR[:, 3, :], op=ALU.mult), r=["s5_bex", "s5_dR", "s5_BB"], w=["s5_BB"])
            P.add("dve", lambda e: e.tensor_tensor(out=tmpb[:], in0=bex[:, 0, :], in1=dR[:, 4, :], op=ALU.mult), r=["s5_bex", "s5_dR", "s5_BB"], w=["s5_tmpb"])
            P.add("dve", lambda e: e.tensor_tensor(out=BB[:, 1, :], in0=BB[:, 1, :], in1=tmpb[:], op=ALU.add), r=["s5_BB", "s5_tmpb"], w=["s5_BB"])
            P.add("pool", lambda e: e.memset(yacc[:], 0.0), w=["s5_yacc"])
            for pair in range(4):
                tl = ut * 4 + pair
                P.dma(cex[:, 0, :], cexp_d[0, tl], w=["s5_cex"]); P.dma(cex[:, 1, :], cexp_d[1, tl], w=["s5_cex"])
                P.add("dve", lambda e, tl=tl, dC=dC: e.tensor_copy(out=rot[:, 0, 0:1], in_=dC[:, 1, tl:tl + 1]), r=["s5_dC"], w=["s5_rot"])
                P.add("dve", lambda e, tl=tl, dC=dC: e.tensor_copy(out=rot[:, 0, 1:2], in_=dC[:, 2, tl:tl + 1]), r=["s5_dC"], w=["s5_rot"])
                for kq in range(1, 10):
                    P.add("dve", lambda e, kq=kq: e.tensor_tensor(out=rot[:, 10, 0:2], in0=rot[:, kq - 1, 0:2], in1=rot[:, kq - 1, 0:2], op=ALU.mult), r=["s5_rot"], w=["s5_rot"])
                    P.add("dve", lambda e, kq=kq: e.tensor_tensor(out=rot[:, kq, 0:1], in0=rot[:, 10, 0:1], in1=rot[:, 10, 1:2], op=ALU.subtract), r=["s5_rot"], w=["s5_rot"])
                    P.add("dve", lambda e, kq=kq: e.scalar_tensor_tensor(out=rot[:, kq, 1:2], in0=rot[:, kq - 1, 0:1], scalar=2.0, in1=rot[:, kq - 1, 1:2], op0=ALU.mult, op1=ALU.mult), r=["s5_rot"], w=["s5_rot"])
                P.add("dve", lambda e: e.memset(E[:, 0, 0:1], 1.0), r=["s5_rot"], w=["s5_E"])
                P.add("dve", lambda e: e.memset(E[:, 1, 0:1], 0.0), w=["s5_E"])
                n = 1
                kq = 0
                while n < L:
                    P.add("dve", lambda e, n=n, kq=kq: e.tensor_scalar(out=E[:, 0, n:2 * n], in0=E[:, 0, 0:n], scalar1=rot[:, kq, 0:1], scalar2=None, op0=ALU.mult), r=["s5_E", "s5_rot"], w=["s5_E"])
                    P.add("dve", lambda e, n=n, kq=kq: e.scalar_tensor_tensor(out=E[:, 0, n:2 * n], in0=E[:, 1, 0:n], scalar=rot[:, kq, 1:2], in1=E[:, 0, n:2 * n], op0=ALU.mult, op1=ALU.add), r=["s5_E", "s5_rot"], w=["s5_E"])
                    P.add("dve", lambda e, n=n, kq=kq: e.tensor_scalar(out=E[:, 1, n:2 * n], in0=E[:, 1, 0:n], scalar1=rot[:, kq, 0:1], scalar2=None, op0=ALU.mult), r=["s5_E", "s5_rot"], w=["s5_E"])
                    P.add("dve", lambda e, n=n, kq=kq: e.tensor_scalar(out=t1[:, 0:n], in0=E[:, 0, 0:n], scalar1=rot[:, kq, 1:2], scalar2=None, op0=ALU.mult), r=["s5_E", "s5_rot"], w=["s5_t1"])
                    P.add("dve", lambda e, n=n: e.tensor_tensor(out=E[:, 1, n:2 * n], in0=E[:, 1, n:2 * n], in1=t1[:, 0:n], op=ALU.subtract), r=["s5_E", "s5_t1"], w=["s5_E"])
                    n *= 2; kq += 1
                P.add("dve", lambda e, tl=tl, dC=dC: e.tensor_scalar(out=rho[:], in0=E[:, 0, :], scalar1=0.0, scalar2=dC[:, 0, tl:tl + 1], op0=ALU.mult, op1=ALU.add), r=["s5_E", "s5_dC"], w=["s5_rho"])
                P.add("dve", lambda e: e.memset(carry[:], 0.0), w=["s5_carry"])
                for si, (s0, sn) in enumerate(SEG):
                    for ri in range(2):
                        P.add("pe", lambda e, ri=ri, s0=s0, sn=sn, pair=pair: e.matmul(psB[ri][:, 0:sn], lhsT=BB[:, ri, pair * 128:(pair + 1) * 128], rhs=uT[:, s0:s0 + sn], start=True, stop=True),
                              r=["s5_BB", "s5_uT"], w=[f"s5_psB{ri}"])
                    P.add("dve", lambda e, sn=sn: e.tensor_tensor(out=cc[:, 0, 0:sn], in0=psB[0][:, 0:sn], in1=E[:, 0, 0:sn], op=ALU.mult), r=["s5_psB0", "s5_E"], w=["s5_c"])
                    P.add("dve", lambda e, sn=sn: e.tensor_tensor(out=t1[:, 0:sn], in0=psB[1][:, 0:sn], in1=E[:, 1, 0:sn], op=ALU.mult), r=["s5_psB1", "s5_E"], w=["s5_t1"])
                    P.add("dve", lambda e, sn=sn: e.tensor_tensor(out=cc[:, 0, 0:sn], in0=cc[:, 0, 0:sn], in1=t1[:, 0:sn], op=ALU.subtract), r=["s5_c", "s5_t1"], w=["s5_c"])
                    P.add("dve", lambda e, sn=sn: e.tensor_tensor(out=cc[:, 1, 0:sn], in0=psB[1][:, 0:sn], in1=E[:, 0, 0:sn], op=ALU.mult), r=["s5_psB1", "s5_E", "s5_c"], w=["s5_c"])
                    P.add("dve", lambda e, sn=sn: e.tensor_tensor(out=t1[:, 0:sn], in0=psB[0][:, 0:sn], in1=E[:, 1, 0:sn], op=ALU.mult), r=["s5_psB0", "s5_E", "s5_c"], w=["s5_t1"])
                    P.add("dve", lambda e, sn=sn: e.tensor_tensor(out=cc[:, 1, 0:sn], in0=cc[:, 1, 0:sn], in1=t1[:, 0:sn], op=ALU.add), r=["s5_c", "s5_t1"], w=["s5_c"])
                    P.add("dve", lambda e: e.tensor_tensor(out=carry[:, 2:3], in0=carry[:, 0:1], in1=rot[:, 0, 0:1], op=ALU.mult), r=["s5_carry", "s5_rot"], w=["s5_carry"])
                    P.add("dve", lambda e: e.tensor_tensor(out=carry[:, 4:5], in0=carry[:, 1:2], in1=rot[:, 0, 1:2], op=ALU.mult), r=["s5_carry", "s5_rot"], w=["s5_carry"])
                    P.add("dve", lambda e: e.tensor_tensor(out=carry[:, 2:3], in0=carry[:, 2:3], in1=carry[:, 4:5], op=ALU.subtract), r=["s5_carry"], w=["s5_carry"])
                    P.add("dve", lambda e: e.tensor_tensor(out=carry[:, 3:4], in0=carry[:, 1:2], in1=rot[:, 0, 0:1], op=ALU.mult), r=["s5_carry", "s5_rot"], w=["s5_carry"])
                    P.add("dve", lambda e: e.tensor_tensor(out=carry[:, 4:5], in0=carry[:, 0:1], in1=rot[:, 0, 1:2], op=ALU.mult), r=["s5_carry", "s5_rot"], w=["s5_carry"])
                    P.add("dve", lambda e: e.tensor_tensor(out=carry[:, 3:4], in0=carry[:, 3:4], in1=carry[:, 4:5], op=ALU.add), r=["s5_carry"], w=["s5_carry"])
                    P.add("dve", lambda e, sn=sn: e.tensor_tensor_scan(out=zz[:, 0, 0:sn], data0=rho[:, 0:sn], data1=cc[:, 0, 0:sn], initial=carry[:, 2:3], op0=ALU.mult, op1=ALU.add), r=["s5_rho", "s5_c", "s5_carry"], w=["s5_z"])
                    P.add("dve", lambda e, sn=sn: e.tensor_tensor_scan(out=zz[:, 1, 0:sn], data0=rho[:, 0:sn], data1=cc[:, 1, 0:sn], initial=carry[:, 3:4], op0=ALU.mult, op1=ALU.add), r=["s5_rho", "s5_c", "s5_carry", "s5_z"], w=["s5_z"])
                    P.add("dve", lambda e, sn=sn: e.tensor_tensor(out=xx[:, 0, 0:sn], in0=zz[:, 0, 0:sn], in1=E[:, 0, 0:sn], op=ALU.mult), r=["s5_z", "s5_E"], w=["s5_x"])
                    P.add("dve", lambda e, sn=sn: e.tensor_tensor(out=t1[:, 0:sn], in0=zz[:, 1, 0:sn], in1=E[:, 1, 0:sn], op=ALU.mult), r=["s5_z", "s5_E"], w=["s5_t1"])
                    P.add("dve", lambda e, sn=sn: e.tensor_tensor(out=xx[:, 0, 0:sn], in0=xx[:, 0, 0:sn], in1=t1[:, 0:sn], op=ALU.add), r=["s5_x", "s5_t1"], w=["s5_x"])
                    P.add("dve", lambda e, sn=sn: e.tensor_tensor(out=xx[:, 1, 0:sn], in0=zz[:, 1, 0:sn], in1=E[:, 0, 0:sn], op=ALU.mult), r=["s5_z", "s5_E", "s5_x"], w=["s5_x"])
                    P.add("dve", lambda e, sn=sn: e.tensor_tensor(out=t1[:, 0:sn], in0=zz[:, 0, 0:sn], in1=E[:, 1, 0:sn], op=ALU.mult), r=["s5_z", "s5_E", "s5_x"], w=["s5_t1"])
                    P.add("dve", lambda e, sn=sn: e.tensor_tensor(out=xx[:, 1, 0:sn], in0=xx[:, 1, 0:sn], in1=t1[:, 0:sn], op=ALU.subtract), r=["s5_x", "s5_t1"], w=["s5_x"])
                    P.add("dve", lambda e, sn=sn: e.tensor_copy(out=carry[:, 0:1], in_=xx[:, 0, sn - 1:sn]), r=["s5_x"], w=["s5_carry"])
                    P.add("dve", lambda e, sn=sn: e.tensor_copy(out=carry[:, 1:2], in_=xx[:, 1, sn - 1:sn]), r=["s5_x"], w=["s5_carry"])
                    P.add("pool", lambda e, sn=sn: e.tensor_scalar(out=xx[:, 1, 0:sn], in0=xx[:, 1, 0:sn], scalar1=-1.0, scalar2=None, op0=ALU.mult), r=["s5_x", "s5_carry"], w=["s5_x"])

                    def mmy(e, sn=sn):
                        e.matmul(psY[:, 0:sn], lhsT=cex[:, 0, :], rhs=xx[:, 0, 0:sn], start=True, stop=False)
                        return e.matmul(psY[:, 0:sn], lhsT=cex[:, 1, :], rhs=xx[:, 1, 0:sn], start=False, stop=True)
                    P.add("pe", mmy, r=["s5_cex", "s5_x"], w=["s5_psY"])
                    P.add("dve", lambda e, s0=s0, sn=sn: e.tensor_tensor(out=yacc[:, s0:s0 + sn], in0=psY[:, 0:sn], in1=yacc[:, s0:s0 + sn], op=ALU.add), r=["s5_psY", "s5_yacc"], w=["s5_yacc"])
            for c in range(NTOK // 128):
                if d == 0:
                    dst_c = c
                else:
                    dst_c = (NCTX // 128 - 1 - c) if c < NCTX // 128 else (NTOK // 128 - 1 - (c - NCTX // 128))
                P.add("pe", lambda e, c=c: e.transpose(psT[:, 128:256], yacc[:, c * 128:(c + 1) * 128], ident[:]), r=["s5_yacc", "ident"], w=["s5_psT"])
                if d == 1:
                    P.add("act", lambda e: e.activation(out=uf[:], in_=psT[:, 128:256], func=AF.Copy), r=["s5_psT"], w=["s5_uf"])
                    P.add("pe", lambda e: e.matmul(psF[:, 128:256], lhsT=jm[:], rhs=uf[:], start=True, stop=True), r=["s5_J", "s5_uf"], w=["s5_psF"])
                    P.add("act", lambda e: e.activation(out=ot[:], in_=psF[:, 128:256], func=AF.Copy), r=["s5_psF"], w=["s5_ot"])
                else:
                    P.add("act", lambda e: e.activation(out=ot[:], in_=psT[:, 128:256], func=AF.Copy), r=["s5_psT"], w=["s5_ot"])
                P.dma(ydir[d][dst_c * 128:(dst_c + 1) * 128, ut * 128:(ut + 1) * 128], ot[:], r=["s5_ot"], w=["ydirS"], q="pool")


def phase_s5_finish(P, ptm, dvec_d, glu_d, ydir, yout, ident):
    a = P.sb("sf_a", [128, 512]); b = P.sb("sf_b", [128, 512]); u = P.sb("sf_u", [128, 512]); dv = P.sb("sf_d", [128, 512]); t = P.sb("sf_t", [128, 512])
    glu = P.sb("sf_glu", [128, 4, 512]); gT = P.sb("sf_gT", [128, 4, 128]); o = P.sb("sf_o", [128, 256]); sg = P.sb("sf_sg", [128, 256])
    psT = P.ps("sf_psT", [128, 512]); psO = P.ps("sf_psO", [128, 512])
    P.dma(dv[:], dvec_d[:, :], w=["sf_d"]); P.dma(glu[:], glu_d.rearrange("(k p) n -> p k n", p=128), w=["sf_glu"])
    for c in range(NTOK // 128):
        r0 = c * 128
        P.dma(a[:], ydir[0][r0:r0 + 128, :], r=["ydirS"], w=["sf_a"])
        P.dma(b[:], ydir[1][r0:r0 + 128, :], r=["ydirS"], w=["sf_b"])
        P.dma(u[:], ptm[r0:r0 + 128, 0:512], r=["ptm"], w=["sf_u"])
        P.add("dve", lambda e: e.tensor_tensor(out=a[:], in0=a[:], in1=b[:], op=ALU.add), r=["sf_a", "sf_b"], w=["sf_a"])
        P.add("pool", lambda e: e.tensor_tensor(out=u[:], in0=u[:], in1=dv[:], op=ALU.mult), r=["sf_u", "sf_d"], w=["sf_u"])
        P.add("dve", lambda e: e.tensor_tensor(out=a[:], in0=a[:], in1=u[:], op=ALU.add), r=["sf_a", "sf_u"], w=["sf_a"])
        P.add("dve", lambda e: e.tensor_tensor(out=t[:], in0=a[:], in1=a[:], op=ALU.mult), r=["sf_a"], w=["sf_t"])
        P.add("dve", lambda e: e.tensor_scalar(out=t[:], in0=t[:], scalar1=0.044715, scalar2=1.0, op0=ALU.mult, op1=ALU.add), r=["sf_t"], w=["sf_t"])
        P.add("dve", lambda e: e.tensor_tensor(out=t[:], in0=t[:], in1=a[:], op=ALU.mult), r=["sf_t", "sf_a"], w=["sf_t"])
        P.add("act", lambda e: e.activation(out=t[:], in_=t[:], func=AF.Tanh, scale=0.7978845608028654), r=["sf_t"], w=["sf_t"])
        P.add("dve", lambda e: e.tensor_scalar(out=t[:], in0=t[:], scalar1=1.0, scalar2=0.5, op0=ALU.add, op1=ALU.mult), r=["sf_t"], w=["sf_t"])
        P.add("dve", lambda e: e.tensor_tensor(out=t[:], in0=t[:], in1=a[:], op=ALU.mult), r=["sf_t", "sf_a"], w=["sf_t"])
        for k in range(4):
            P.add("pe", lambda e, k=k: e.transpose(psT[:, k * 128:(k + 1) * 128], t[:, k * 128:(k + 1) * 128], ident[:]), r=["sf_t", "ident"], w=["sf_psT"])
        P.add("act", lambda e: e.activation(out=gT[:].rearrange("p a b -> p (a b)"), in_=psT[:, :], func=AF.Copy), r=["sf_psT"], w=["sf_gT"])

        def mm(e):
            last = None
            for k in range(4):
                last = e.matmul(psO[:, :], lhsT=gT[:, k, :], rhs=glu[:, k, :], start=(k == 0), stop=(k == 3))
            return last
        P.add("pe", mm, r=["sf_gT", "sf_glu"], w=["sf_psO"])
        P.add("act", lambda e: e.activation(out=sg[:], in_=psO[:, 256:512], func=AF.Sigmoid), r=["sf_psO"], w=["sf_sg"])
        P.add("dve", lambda e: e.tensor_tensor(out=o[:], in0=psO[:, 0:256], in1=sg[:], op=ALU.mult), r=["sf_psO", "sf_sg"], w=["sf_o"])
        P.dma(yout[r0:r0 + 128, 0:256], o[:], r=["sf_o"], w=["yout"], q="pool")


def build_la1(do_s5=True, do_hg=True, dbg=False):
    nc = bass.Bass("TRN2", target_bir_lowering=False)
    P = Prog(nc)
    P.use_arena(36000)
    ei = lambda name, shape: nc.dram_tensor(name, list(shape), F32, kind="ExternalInput").ap()
    NC_ALL = 4352
    xT = ei("xT", [D, NTOK]); W = ei("W", [D, NC_ALL]); vecs = ei("vecs", [128, 5, KT]); ident_d = ei("ident", [128, 128])
    if do_hg:
        lbl = ei("lbl", [128, 2, 768]); hcst = ei("hcst", [2, 128, 516]); hnw = ei("hnw", [128, 768])
    if do_s5:
        prmR = ei("prmR", [2, 128, 3, 2048]); prmC = ei("prmC", [2, 128, 3, 16]); bexp = ei("bexp", [2, 4, 128, 512]); cexp = ei("cexp", [2, 16, 128, 128])
        jmat = ei("jmat", [128, 128]); dvec = ei("dvec", [128, 512]); glu = ei("glu", [512, 512])
    yout = nc.dram_tensor("yout", [NTOK, 1024], BF16, kind="ExternalOutput").ap()
    ptm = nc.dram_tensor("ptm", [NTOK, NC_ALL], F32, kind="ExternalOutput" if dbg else "Internal").ap()
    ydirH = nc.dram_tensor("ydirH", [2, NTOK, 768], F32).ap()
    ydirS = nc.dram_tensor("ydirS", [2, NTOK, 512], F32, kind="ExternalOutput" if dbg else "Internal").ap()
    vec = P.sb("vec", [128, 5, KT]); weff = P.sb("weff", [128, 2, KT]); ones = P.sb("ones", [128, 128]); epsb = P.sb("epsb", [128, 1])
    ident = P.sb("ident_s", [128, 128])
    P.persist()
    P.dma(vec[:], vecs[:, :, :], w=["vec"]); P.dma(ident[:], ident_d[:, :], w=["ident"])
    P.add("dve", lambda e: e.memset(ones[:], 1.0), w=["ones"])
    P.add("dve", lambda e: e.memset(epsb[:], EPS), w=["epsb"])
    for c in range(2):
        P.add("dve", lambda e, c=c: e.scalar_tensor_tensor(out=weff[:, c, :], in0=vec[:, c * 2 + 1, :], scalar=1.0, in1=vec[:, 4, :], op0=ALU.add, op1=ALU.mult),
              r=["vec"], w=["weff"])
    phase_inproj(P, xT, W, NC_ALL, ptm, vec, weff, ones, epsb)
    if do_hg:
        P.phase()
        phase_hgrn(P, ptm, 512, lbl, hcst, ydirH, ident)
        P.phase()
        phase_hgrn_finish(P, ptm, 512 + 4 * 768, hnw, ydirH, yout, 256)
    if do_s5:
        P.phase()
        phase_s5(P, ptm, prmR, prmC, bexp, cexp, ydirS, ident, jmat)
        P.phase()
        phase_s5_finish(P, ptm, dvec, glu, ydirS, yout, ident)
    return P.emit()


A_SPL = (1024, 1024, 1024, 64, 160, 64, 64)
def fm16(v): return np.ascontiguousarray(v.reshape(16, 128).T)
def head_order(hh):
    return [hh * 8 + 2 * hp + hs for hs in range(2) for hp in range(4)]
def a_cols(hh):
    ho = head_order(hh)
    ch = np.concatenate([np.arange(h * 64, (h + 1) * 64) for h in ho])
    r = ch; k = 1024 + ch; v = 2048 + ch
    a_lo = 3072 + np.arange(64); g_lo = 3136 + np.arange(160); wf = 3296 + np.arange(64); wb = 3360 + np.arange(64)
    pad = lambda n: -np.ones(n, np.int64)
    return np.concatenate([r, k, v, a_lo, wf, wb, pad(64), g_lo, pad(96)]), ch
def b_cols(hh):
    h0 = hh * 4
    q = 3424 + np.arange(h0 * 64, (h0 + 4) * 64); k = 3424 + 512 + np.arange(h0 * 64, (h0 + 4) * 64)
    v = 3424 + 1024 + np.arange(h0 * 128, (h0 + 4) * 128); g = 3424 + 2048 + np.arange(h0 * 128, (h0 + 4) * 128)
    return np.concatenate([q, k, v, g])
def take_cols(W, idx):
    out = np.zeros((W.shape[0], len(idx)), W.dtype)
    m = idx >= 0
    out[:, m] = W[:, idx[m]]
    return out
def rope_table():
    GRID_W = 64; half = 32; nf = 16
    t = np.arange(8192)
    row = (t // GRID_W).astype(np.float32); col = (t % GRID_W).astype(np.float32)
    inv = (10000.0 ** (-np.arange(nf, dtype=np.float32) / nf)).astype(np.float32)
    ang = np.concatenate([row[:, None] * inv, col[:, None] * inv], -1)
    tab = np.concatenate([np.cos(ang), np.sin(ang)], -1).astype(np.float32)
    ctx = np.concatenate([np.ones((256, 32), np.float32), np.zeros((256, 32), np.float32)], -1)
    return np.concatenate([ctx, tab], 0)
def ret_cst():
    i = np.arange(128)
    E1 = np.maximum(i[None, :] - i[:, None], 0).astype(np.float32)
    M1 = (i[:, None] <= i[None, :]).astype(np.float32)
    c = np.stack([i + 1.0, 127.0 - i, np.full(128, 128.0)], 1).astype(np.float32)
    f = np.concatenate([E1, M1, c], 1)
    cb = np.stack([128.0 - i, i + 0.0, np.full(128, 128.0)], 1).astype(np.float32)
    bwd = np.concatenate([E1.T, M1.T, cb], 1)
    return np.ascontiguousarray(np.stack([f, bwd], 0))
def shift_mask():
    m = np.zeros((8448, 4), np.float32)
    t = np.arange(256)
    m[:256, 0] = (t > 0); m[:256, 2] = (t > 0); m[:256, 1] = (t < 255); m[:256, 3] = (t < 255)
    t = np.arange(8192); col = t % 64; row = t // 64
    m[256:, 0] = (col > 0); m[256:, 1] = (col < 63); m[256:, 2] = (row > 0); m[256:, 3] = (row < 127)
    return m
def rwkv_consts(hh, inp):
    ac, ch = a_cols(hh)
    mu = np.zeros(2048, np.float32); mk = ac >= 0; mu[mk] = inp["rwkv_mu"][0][ac[mk]]
    mu_b = np.ascontiguousarray(np.tile(mu[None], (128, 1)))
    vs = [inp["rwkv_a0"][0], inp["rwkv_w0"][0, 0], inp["rwkv_w0"][0, 1], inp["rwkv_k_k"][0], inp["rwkv_k_a"][0],
          inp["rwkv_r_k"][0].reshape(-1), inp["rwkv_ln_w"][0], inp["rwkv_ln_b"][0]]
    cv = np.stack([np.tile(v[ch][None], (128, 1)) for v in vs], 1).astype(np.float32)
    lw = np.zeros((128, 5, 512), np.float32)
    lw[0:64, 0] = inp["rwkv_a2"][0][:, ch]; lw[64:128, 1] = inp["rwkv_w2"][0, 0][:, ch]; lw[0:64, 2] = inp["rwkv_w2"][0, 1][:, ch]
    lw[:, 3] = inp["rwkv_g2"][0][0:128][:, ch]; lw[0:32, 4] = inp["rwkv_g2"][0][128:160][:, ch]
    return mu_b, np.ascontiguousarray(cv), lw


def cd_cols(hh):
    u = np.arange(512)
    hc = np.arange(hh * 768, (hh + 1) * 768)
    return np.concatenate([u] + [512 + j * 1536 + hc for j in range(5)])
def hgrn_cst():
    i = np.arange(128); ch = i // 64; same = ch[:, None] == ch[None, :]
    out = np.zeros((2, 128, 516), np.float32)
    mid = ch * 64 + 32
    for d in range(2):
        if d == 0:
            TRI = same & (i[:, None] <= i[None, :]); MID = same & (i[:, None] <= mid[None, :])
        else:
            TRI = same & (i[:, None] >= i[None, :]); MID = same & (i[:, None] >= mid[None, :])
        out[d, :, 0:128] = TRI; out[d, :, 128:256] = MID; out[d, :, 256:384] = same; out[d, :, 384:512] = TRI
        out[d, :, 512] = (ch == 0); out[d, :, 513] = (ch == 1); out[d, :, 514] = (ch == 0); out[d, :, 515] = (ch == 1)
    return out
def s5_consts(inp, hh):
    lre = inp["s5_lambda_re"][0]; lim = inp["s5_lambda_im"][0]; ldt = inp["s5_log_dt"][0]
    prmR = np.zeros((2, 128, 3, 2048), np.float32); prmC = np.zeros((2, 128, 3, 16), np.float32)
    for d in range(2):
        flat = np.stack([lre[d].reshape(-1), lim[d].reshape(-1), np.repeat(ldt[d], 64)], 0)
        prmR[d] = flat[None]
        prmC[d] = flat.reshape(3, 16, 128).transpose(2, 0, 1)
    bre = inp["s5_b_re"][0]; bim = inp["s5_b_im"][0]
    bexp = np.zeros((2, 4, 128, 512), np.float32)
    for ut in range(4):
        for gl in range(8):
            g = ut * 8 + gl; pair = gl // 2; g2 = gl % 2
            c0 = pair * 128 + g2 * 64
            bexp[0, ut, gl * 16:(gl + 1) * 16, c0:c0 + 64] = bre[g].T
            bexp[1, ut, gl * 16:(gl + 1) * 16, c0:c0 + 64] = bim[g].T
    cre = inp["s5_c_re"][0]; cim = inp["s5_c_im"][0]
    cexp = np.zeros((2, 16, 128, 128), np.float32)
    for tl in range(16):
        for g2 in range(2):
            g = tl * 2 + g2; gl = g % 8
            cexp[0, tl, g2 * 64:(g2 + 1) * 64, gl * 16:(gl + 1) * 16] = cre[g].T
            cexp[1, tl, g2 * 64:(g2 + 1) * 64, gl * 16:(gl + 1) * 16] = cim[g].T
    dvec = np.ascontiguousarray(np.tile(inp["s5_d"][0][None], (128, 1)))
    gw = inp["s5_glu_w"][0]
    glu = np.ascontiguousarray(np.concatenate([gw[:, hh * 256:(hh + 1) * 256], gw[:, 512 + hh * 256:512 + (hh + 1) * 256]], 1))
    return dict(prmR=prmR, prmC=prmC, bexp=bexp, cexp=cexp, dvec=dvec, glu=glu, jmat=np.ascontiguousarray(np.eye(128, dtype=np.float32)[::-1]))
def la1_inputs(b, hh, x, xc, m, inp):
    xT = np.ascontiguousarray(np.concatenate([xc[b], x[b]], 0).T)
    W = take_cols(inp["cd_w_in"][0], cd_cols(hh))
    vecs = np.zeros((128, 5, 16), np.float32)
    ml = m[b].reshape(6, 2048); mc = m[4].reshape(6, 2048)
    vecs[:, 0] = fm16(ml[0]); vecs[:, 1] = fm16(ml[1]); vecs[:, 2] = fm16(mc[0]); vecs[:, 3] = fm16(mc[1]); vecs[:, 4] = fm16(inp["norm_w"][1, 0])
    hc = np.arange(hh * 768, (hh + 1) * 768)
    lbl = np.ascontiguousarray(np.tile(inp["hgrn_lb_logits"][:, hc][None], (128, 1, 1)).astype(np.float32))
    hnw = np.ascontiguousarray(np.tile(inp["hgrn_norm_w"][0][hc][None], (128, 1)).astype(np.float32))
    d = dict(xT=xT, W=np.ascontiguousarray(W), vecs=vecs, ident=np.eye(128, dtype=np.float32), lbl=lbl, hcst=hgrn_cst(), hnw=hnw)
    d.update(s5_consts(inp, hh))
    return d


def build_lm(NCOL=3072):
    nc = bass.Bass("TRN2", target_bir_lowering=False)
    P = Prog(nc)
    ei = lambda name, shape: nc.dram_tensor(name, list(shape), F32, kind="ExternalInput").ap()
    ccT = ei("ccT", [D, 128]); Wm = ei("Wm", [D, NCOL]); bm = ei("bm", [128, NCOL])
    out = nc.dram_tensor("m", [128, NCOL], F32, kind="ExternalOutput").ap()
    cs = P.sb("cs", [128, KT, 128]); bs = P.sb("bs", [128, NCOL]); ob = P.sb("ob", [128, NCOL])
    ws = [(P.sb(f"lm_w{i}", [128, KT, 256]), f"lm_w{i}") for i in range(2)]
    ps = [(P.ps(f"lm_ps{i}", [128, 512]), f"lm_ps{i}") for i in range(2)]
    P.dma(cs[:], ccT.rearrange("(k p) r -> p k r", p=128), w=["cs"]); P.dma(bs[:], bm[:, :], w=["bs"])
    P.add("act", lambda e: e.activation(out=cs[:], in_=cs[:], func=AF.Silu), r=["cs"], w=["cs"])
    Wv = Wm.rearrange("(k p) n -> p k n", p=128)
    for ci, c0 in enumerate(range(0, NCOL, 256)):
        st, sres = ws[ci % 2]; pt, pres = ps[ci % 2]
        P.dma(st[:], Wv[:, :, c0:c0 + 256], w=[sres])

        def mm(e, st=st, pt=pt):
            last = None
            for k in range(KT):
                last = e.matmul(pt[:, 0:256], lhsT=cs[:, k, :], rhs=st[:, k, :], start=(k == 0), stop=(k == KT - 1))
            return last
        P.add("pe", mm, r=[sres, "cs"], w=[pres])
        P.add("dve", lambda e, pt=pt, c0=c0: e.tensor_tensor(out=ob[:, c0:c0 + 256], in0=pt[:, 0:256], in1=bs[:, c0:c0 + 256], op=ALU.add), r=[pres, "bs"], w=["ob"])
    P.dma(out[:, :], ob[:], r=["ob"], w=["out"])
    return P.emit()


def _run(nc, in_maps):
    res = run_bass_kernel_spmd(nc, in_maps, core_ids=list(range(len(in_maps))))
    return res.results


def la0_inputs(b, hh, x, ctx, m, inp):
    xT = np.ascontiguousarray(np.concatenate([ctx[b], x[b]], 0).T)
    ac, ch = a_cols(hh)
    W = np.concatenate([take_cols(inp["ab_w_in"][0], ac), take_cols(inp["ab_w_in"][0], b_cols(hh))], 1)
    vecs = np.zeros((128, 5, 16), np.float32)
    ml = m[b].reshape(6, 2048); mc = m[4].reshape(6, 2048)
    vecs[:, 0] = fm16(ml[0]); vecs[:, 1] = fm16(ml[1]); vecs[:, 2] = fm16(mc[0]); vecs[:, 3] = fm16(mc[1]); vecs[:, 4] = fm16(inp["norm_w"][0, 0])
    rde = np.tile(inp["ret_decay_exp"][0][:, hh * 4:(hh + 1) * 4].reshape(1, 8), (128, 1)).astype(np.float32)
    mu_b, cv, lw = rwkv_consts(hh, inp)
    return dict(xT=xT, W=np.ascontiguousarray(W), vecs=vecs, rope=rope_table(), rde=np.ascontiguousarray(rde), rcst=ret_cst(), ident=np.eye(128, dtype=np.float32),
                shmask=shift_mask(), mu=mu_b, cv=cv, lw=lw)


def bcd_inputs(L, b, xs, ys, m, inp, last):
    vecs = np.zeros((128, 12, 16), np.float32)
    for c, row in enumerate([b, 4]):
        mm = m[row].reshape(6, 2048)
        for j, idx in enumerate([2, 3, 4, 5]):
            vecs[:, c * 4 + j, :] = fm16(mm[idx])
    vecs[:, 8, :] = fm16(inp["norm_w"][L, 1]); vecs[:, 9, :] = fm16(inp["final_norm_w"])
    wr = np.concatenate([inp["moe_wr_coarse"][L], inp["moe_wr_fine"][L]], 1)
    br = np.tile(np.concatenate([inp["moe_br_coarse"][L], inp["moe_br_fine"][L]])[None], (128, 1)).astype(np.float32)
    onehot = np.zeros((128, 32, 128), np.float32)
    for e in range(32):
        onehot[e, e, :] = 1
    return dict(xT=np.ascontiguousarray(xs.T), yT=np.ascontiguousarray(ys.T), wout=np.ascontiguousarray(inp["w_out"][L]), vecs=vecs, wr=np.ascontiguousarray(wr),
                br=np.ascontiguousarray(br), wgu=np.ascontiguousarray(inp["moe_w_gu"][L]), wdn=np.ascontiguousarray(inp["moe_w_down"][L]),
                ident=np.eye(128, dtype=np.float32), onehot=onehot.reshape(128, -1))


def kernel(**inp):
    inp = {k: np.asarray(v) for k, v in inp.items()}
    x = inp["x"]; ctx = inp["ctx"]
    B = x.shape[0]
    cc = np.zeros((128, 2048), np.float32); cc[0:4] = inp["c"]; cc[4] = inp["c_ctx"]
    ccT = np.ascontiguousarray(cc.T)
    mw = np.concatenate([inp["mod_w"][0], inp["mod_w"][1]], 1)
    mb = np.concatenate([inp["mod_b"][0], inp["mod_b"][1]], 0)
    ims = []
    for c in range(8):
        sl = slice(c * 3072, (c + 1) * 3072)
        ims.append(dict(ccT=ccT, Wm=np.ascontiguousarray(mw[:, sl]), bm=np.ascontiguousarray(np.tile(mb[sl][None], (128, 1)))))
    res = _run(build_lm(), ims)
    mall = np.concatenate([r["m"][0:5] for r in res], 1)
    ms = [mall[:, 0:12288], mall[:, 12288:]]
    res = _run(build_la0(), [la0_inputs(c // 2, c % 2, x, ctx, ms[0], inp) for c in range(8)])
    y0 = np.zeros((B, NTOK, 2048), ml_dtypes.bfloat16)
    for c in range(8):
        b, hh = c // 2, c % 2
        ac, ch = a_cols(hh)
        yo = res[c]["yout"]
        y0[b][:, ch] = yo[:, 0:512]
        y0[b][:, 1024 + hh * 512:1024 + (hh + 1) * 512] = yo[:, 512:1024]
    tiles0 = [(i * 384, 384, 0) for i in range(10)] + [(3840, 256, 0), (4096, 128, 1)]
    ims = []
    for c in range(8):
        b, sh = c // 2, c % 2
        xs = np.concatenate([x[b, sh * 4096:(sh + 1) * 4096], ctx[b, sh * 128:(sh + 1) * 128]], 0)
        ys = np.concatenate([y0[b, NCTX + sh * 4096:NCTX + (sh + 1) * 4096], y0[b, sh * 128:(sh + 1) * 128]], 0)
        ims.append(bcd_inputs(0, b, xs, ys, ms[0], inp, False))
    res = _run(build_bcd(tiles0, 4224, False), ims)
    x1 = np.zeros_like(x); xc1 = np.zeros_like(ctx)
    for c in range(8):
        b, sh = c // 2, c % 2
        o = res[c]["out"].T
        x1[b, sh * 4096:(sh + 1) * 4096] = o[0:4096]; xc1[b, sh * 128:(sh + 1) * 128] = o[4096:4224]
    res = _run(build_la1(), [la1_inputs(c // 2, c % 2, x1, xc1, ms[1], inp) for c in range(8)])
    y1 = np.zeros((B, 8192, 2048), ml_dtypes.bfloat16)
    for c in range(8):
        b, hh = c // 2, c % 2
        yo = res[c]["yout"][NCTX:]
        y1[b][:, hh * 256:(hh + 1) * 256] = yo[:, 0:256]
        y1[b][:, 512 + hh * 768:512 + (hh + 1) * 768] = yo[:, 256:1024]
    tiles1 = [(i * 384, 384, 0) for i in range(10)] + [(3840, 256, 0)]
    ims = []
    for c in range(8):
        b, sh = c // 2, c % 2
        ims.append(bcd_inputs(1, b, x1[b, sh * 4096:(sh + 1) * 4096], y1[b, sh * 4096:(sh + 1) * 4096], ms[1], inp, True))
    res = _run(build_bcd(tiles1, 4096, True), ims)
    out = np.zeros(x.shape, np.float32)
    for c in range(8):
        b, sh = c // 2, c % 2
        out[b, sh * 4096:(sh + 1) * 4096] = res[c]["out"].T
    return out
```
